# Optimizing a Trainium2 kernel written in Bass

```python
import math
import jax, jax.numpy as jnp
from jax import lax
import numpy as np

D_MODEL = 2048
BATCH = 4
SEQ = 2048
DEPTH = 2

CHUNK = 64
N_PREV_CHUNKS = 8
BAND = (N_PREV_CHUNKS + 1) * CHUNK
ATT_HEADS = 8
ATT_HEAD_DIM = D_MODEL // 16
ATT_WIDTH = ATT_HEADS * ATT_HEAD_DIM
REL_CLIP = 128
N_REL = 2 * REL_CLIP + 1
SGU_GROUPS = 8
SGU_GROUP_DIM = D_MODEL // 16
SGU_WIDTH = SGU_GROUPS * SGU_GROUP_DIM
SGU_WINDOW = 128
MIX_WIDTH = ATT_WIDTH + SGU_WIDTH
IN_WIDTH = 3 * ATT_WIDTH + 2 * SGU_WIDTH
N_EXPERTS = 16
N_EXPERT_GROUPS = 4
EXPERTS_PER_GROUP = N_EXPERTS // N_EXPERT_GROUPS
TOP_K = 2
D_FF_EXPERT = D_MODEL // 2
DEEPNORM_ALPHA = (2.0 * DEPTH) ** 0.25
DEEPNORM_BETA = (8.0 * DEPTH) ** -0.25
LN_EPS = 1e-5
NEG_INF = -1e30

kernel_name = "chunk_hybrid_relattn_sgu_grouped_moe_deepnorm"


def layer_norm(x, g, b):
    xf = x.astype(jnp.float32)
    mu = jnp.mean(xf, axis=-1, keepdims=True)
    xc = xf - mu
    var = jnp.mean(jnp.square(xc), axis=-1, keepdims=True)
    y = xc * lax.rsqrt(var + LN_EPS) * g.astype(jnp.float32) + b.astype(jnp.float32)
    return y.astype(x.dtype)


def _rel_index():
    i = np.arange(CHUNK)[:, None]
    key_pos = np.arange(BAND)[None, :] - N_PREV_CHUNKS * CHUNK
    return np.clip(i - key_pos, -REL_CLIP, REL_CLIP) + REL_CLIP


def _band_valid(n_chunks):
    j = np.arange(BAND) // CHUNK
    return (np.arange(n_chunks)[:, None] - N_PREV_CHUNKS + j[None, :]) >= 0


def chunk_band_attention(q, k, v, rel_table):
    b, s, h, dh = q.shape
    nc = s // CHUNK
    qc = q.reshape(b, nc, CHUNK, h, dh)

    def band(t):
        tc = t.reshape(b, nc, CHUNK, h, dh)
        tp = jnp.pad(tc, ((0, 0), (N_PREV_CHUNKS, 0), (0, 0), (0, 0), (0, 0)))
        return jnp.concatenate([tp[:, j:j + nc] for j in range(N_PREV_CHUNKS + 1)], axis=2)

    kb, vb = band(k), band(v)
    scores = jnp.einsum('bnqhd,bnkhd->bnhqk', qc.astype(jnp.float32), kb.astype(jnp.float32))
    scores = scores * (1.0 / math.sqrt(dh))
    bias = rel_table.astype(jnp.float32)[:, _rel_index()]
    scores = scores + bias[None, None]
    valid = jnp.asarray(_band_valid(nc))[None, :, None, None, :]
    scores = jnp.where(valid, scores, NEG_INF)
    p = jax.nn.softmax(scores, axis=-1).astype(v.dtype)
    out = jnp.einsum('bnhqk,bnkhd->bnqhd', p, vb)
    return out.reshape(b, s, h * dh)


def spatial_gating(u, v, ln_g, ln_b, w_s, b_s):
    b, s, _ = u.shape
    nb = s // SGU_WINDOW
    v = layer_norm(v, ln_g, ln_b)
    vb = v.reshape(b, nb, SGU_WINDOW, SGU_GROUPS, SGU_GROUP_DIM)
    causal = jnp.asarray(np.tril(np.ones((SGU_WINDOW, SGU_WINDOW), dtype=bool)))
    w_m = jnp.where(causal[None], w_s, jnp.zeros((), w_s.dtype))
    mixed = jnp.einsum('gts,bnsgc->bntgc', w_m, vb) + b_s.T[None, None, :, :, None]
    return u * mixed.reshape(b, s, SGU_WIDTH)


def grouped_top2_moe(h, router_w, router_b, w_gate, w_up, w_down):
    b, s, d = h.shape
    t = h.reshape(b * s, d)
    logits = t.astype(jnp.float32) @ router_w.astype(jnp.float32) + router_b.astype(jnp.float32)
    probs = jax.nn.softmax(logits, axis=-1)
    pg = probs.reshape(-1, N_EXPERT_GROUPS, EXPERTS_PER_GROUP)
    group_score = lax.top_k(pg, TOP_K)[0].sum(-1)
    sel_group = jnp.argmax(group_score, axis=-1)
    in_group = (jnp.arange(N_EXPERT_GROUPS)[None, :] == sel_group[:, None])[:, :, None]
    masked = jnp.where(in_group, pg, -1.0).reshape(-1, N_EXPERTS)
    top_p, top_i = lax.top_k(masked, TOP_K)
    gates = top_p / jnp.sum(top_p, axis=-1, keepdims=True)
    combine = jnp.sum(jax.nn.one_hot(top_i, N_EXPERTS, dtype=jnp.float32) * gates[..., None], axis=1)
    out = jnp.zeros((b * s, d), jnp.float32)
    for e in range(N_EXPERTS):
        hid = jax.nn.silu(t @ w_gate[e]) * (t @ w_up[e])
        out = out + combine[:, e:e + 1] * (hid @ w_down[e]).astype(jnp.float32)
    return out.astype(h.dtype).reshape(b, s, d)


def setup_inputs(seed: int = 0) -> dict:
    key = jax.random.key(seed)
    ks = jax.random.split(key, 20)
    f32 = jnp.float32
    L = DEPTH
    col_scale = jnp.concatenate([
        jnp.ones((2 * ATT_WIDTH,), f32),
        jnp.full((ATT_WIDTH + 2 * SGU_WIDTH,), DEEPNORM_BETA, f32)])
    x = jax.random.normal(ks[0], (BATCH, SEQ, D_MODEL), f32)
    ln_in_g = 1.0 + 0.02 * jax.random.normal(ks[1], (D_MODEL,), f32)
    ln_in_b = 0.02 * jax.random.normal(ks[2], (D_MODEL,), f32)
    w_in = jax.random.normal(ks[3], (L, D_MODEL, IN_WIDTH), f32) * (D_MODEL ** -0.5) * col_scale
    rel_bias = 0.1 * jax.random.normal(ks[4], (L, ATT_HEADS, N_REL), f32)
    sgu_ln_g = 1.0 + 0.02 * jax.random.normal(ks[5], (L, SGU_WIDTH), f32)
    sgu_ln_b = 0.02 * jax.random.normal(ks[6], (L, SGU_WIDTH), f32)
    sgu_w = jax.random.normal(ks[7], (L, SGU_GROUPS, SGU_WINDOW, SGU_WINDOW), f32) * (0.5 * SGU_WINDOW ** -0.5)
    sgu_b = 1.0 + 0.02 * jax.random.normal(ks[8], (L, SGU_GROUPS, SGU_WINDOW), f32)
    w_out = jax.random.normal(ks[9], (L, MIX_WIDTH, D_MODEL), f32) * (DEEPNORM_BETA * MIX_WIDTH ** -0.5)
    ln1_g = 1.0 + 0.02 * jax.random.normal(ks[10], (L, D_MODEL), f32)
    ln1_b = 0.02 * jax.random.normal(ks[11], (L, D_MODEL), f32)
    router_w = jax.random.normal(ks[12], (D_MODEL, N_EXPERTS), f32) * (D_MODEL ** -0.5)
    router_b = 0.01 * jax.random.normal(ks[13], (N_EXPERTS,), f32)
    w_gate = jax.random.normal(ks[14], (L, N_EXPERTS, D_MODEL, D_FF_EXPERT), f32) * (DEEPNORM_BETA * D_MODEL ** -0.5)
    w_up = jax.random.normal(ks[15], (L, N_EXPERTS, D_MODEL, D_FF_EXPERT), f32) * (DEEPNORM_BETA * D_MODEL ** -0.5)
    w_down = jax.random.normal(ks[16], (L, N_EXPERTS, D_FF_EXPERT, D_MODEL), f32) * (DEEPNORM_BETA * D_FF_EXPERT ** -0.5)
    ln2_g = 1.0 + 0.02 * jax.random.normal(ks[17], (L, D_MODEL), f32)
    ln2_b = 0.02 * jax.random.normal(ks[18], (L, D_MODEL), f32)
    return {"x": x, "ln_in_g": ln_in_g, "ln_in_b": ln_in_b, "w_in": w_in, "rel_bias": rel_bias,
            "sgu_ln_g": sgu_ln_g, "sgu_ln_b": sgu_ln_b, "sgu_w": sgu_w, "sgu_b": sgu_b,
            "w_out": w_out, "ln1_g": ln1_g, "ln1_b": ln1_b, "router_w": router_w,
            "router_b": router_b, "w_gate": w_gate, "w_up": w_up, "w_down": w_down,
            "ln2_g": ln2_g, "ln2_b": ln2_b}


def reference(x, ln_in_g, ln_in_b, w_in, rel_bias, sgu_ln_g, sgu_ln_b, sgu_w, sgu_b,
              w_out, ln1_g, ln1_b, router_w, router_b, w_gate, w_up, w_down, ln2_g, ln2_b):
    b, s, _ = x.shape
    h = layer_norm(x, ln_in_g, ln_in_b)
    splits = [ATT_WIDTH, 2 * ATT_WIDTH, 3 * ATT_WIDTH, 3 * ATT_WIDTH + SGU_WIDTH]
    for l in range(DEPTH):
        z = h @ w_in[l]
        q, k, v, u_s, v_s = jnp.split(z, splits, axis=-1)
        hd = (b, s, ATT_HEADS, ATT_HEAD_DIM)
        y_att = chunk_band_attention(q.reshape(hd), k.reshape(hd), v.reshape(hd), rel_bias[l])
        y_sgu = spatial_gating(jax.nn.gelu(u_s, approximate=False), jax.nn.gelu(v_s, approximate=False),
                               sgu_ln_g[l], sgu_ln_b[l], sgu_w[l], sgu_b[l])
        mix = jnp.concatenate([y_att, y_sgu], axis=-1) @ w_out[l]
        h = layer_norm(DEEPNORM_ALPHA * h + mix, ln1_g[l], ln1_b[l])
        ffn = grouped_top2_moe(h, router_w, router_b, w_gate[l], w_up[l], w_down[l])
        h = layer_norm(DEEPNORM_ALPHA * h + ffn, ln2_g[l], ln2_b[l])
    return h
```

```python
from contextlib import ExitStack

import numpy as np
import concourse.bass as bass
import concourse.mybir as mybir
from concourse.bass_utils import run_bass_kernel_spmd

F32 = mybir.dt.float32
BF16 = mybir.dt.bfloat16
AF = mybir.ActivationFunctionType
ALU = mybir.AluOpType
AX = mybir.AxisListType

ENGS = ("pe", "act", "dve", "pool", "sp")

D = 2048
NCH = 16
DEPTH = 2
ALPHA = (2.0 * DEPTH) ** 0.25
EPS = 1e-5
NE = 16
FF = 1024
N_CORES = 8


class Box:
    __slots__ = ("key", "val")

    def __init__(self, key, val=None):
        self.key = key
        self.val = val


class Slot:
    __slots__ = ("name", "w", "r")

    def __init__(self, name):
        self.name = name
        self.w = None
        self.r = {}


class DSem:
    def __init__(self, prog, name):
        self.name = f"{name}_{len(prog.dsems)}"
        self.count = 0
        self.key = "d:" + self.name
        prog.dsems.append(self)


class _Rec:
    def __init__(self):
        self.call = None

    def __getattr__(self, name):
        def f(*a, **k):
            self.call = (name, a, k)
        return f


class Prog:
    def __init__(self):
        self.ops = {e: [] for e in ENGS}
        self.count = {e: 0 for e in ENGS}
        self.pending = {e: [] for e in ENGS}
        self.seen = {e: {} for e in ENGS}
        self.dsems = []
        self.same_engine_sync = True

    def dsem(self, name):
        return DSem(self, name)

    def _need(self, eng, box, waits):
        if box is None:
            return
        if box.key == eng and (eng == "pe" or not self.same_engine_sync):
            return
        assert box.val is not None, f"dependency on unsignalled op ({box.key})"
        k, v = box.key, box.val
        if self.seen[eng].get(k, 0) >= v:
            return
        self.seen[eng][k] = v
        waits[k] = max(waits.get(k, 0), v)

    def _deps(self, eng, reads, writes):
        waits = {}
        for s in reads:
            self._need(eng, s.w, waits)
        for s in writes:
            self._need(eng, s.w, waits)
            for b in s.r.values():
                self._need(eng, b, waits)
        return waits

    def _mark(self, box, reads, writes):
        for s in reads:
            s.r[box.key] = box
        for s in writes:
            s.w = box
            s.r = {}

    def op(self, eng, fn, reads=(), writes=(), sig=True):
        waits = self._deps(eng, reads, writes)
        box = Box(eng)
        if sig:
            self.count[eng] += 1
            box.val = self.count[eng]
            for b in self.pending[eng]:
                b.val = box.val
            self.pending[eng] = []
        else:
            self.pending[eng].append(box)
        self._mark(box, reads, writes)
        rec = _Rec()
        fn(rec)
        assert rec.call is not None
        self.ops[eng].append(("op", rec.call, waits, sig))
        return box

    def dma(self, eng, out, in_, dsem, reads=(), writes=()):
        waits = self._deps(eng, reads, writes)
        dsem.count += 16
        box = Box(dsem.key, dsem.count)
        self._mark(box, reads, writes)
        self.ops[eng].append(("dma", (out, in_, dsem), waits, True))
        return box

    def barrier(self):
        for e in ENGS:
            assert not self.pending[e], f"barrier with unsignalled ops on {e}"
        for e in ENGS:
            waits = {}
            for k in ENGS:
                if k != e and self.count[k] > self.seen[e].get(k, 0):
                    waits[k] = self.count[k]
                    self.seen[e][k] = self.count[k]
            for d in self.dsems:
                if d.count > self.seen[e].get(d.key, 0):
                    waits[d.key] = d.count
                    self.seen[e][d.key] = d.count
            if waits:
                self.ops[e].append(("wait", None, waits, False))

    def emit(self, nc, stack):
        sems = {}
        for e in ENGS:
            sems[e] = stack.enter_context(nc.semaphore("s_" + e))
        for d in self.dsems:
            sems[d.key] = stack.enter_context(nc.semaphore("d_" + d.name))
        for e in ENGS:
            assert self.count[e] < 60000, (e, self.count[e])
            assert not self.pending[e], f"trailing unsignalled ops on {e}"
        block = stack.enter_context(nc.Block())
        hand = {"pe": block.tensor, "act": block.scalar, "dve": block.vector,
                "pool": block.gpsimd, "sp": block.sync}

        def make(e):
            def body(engine):
                for kind, fn, waits, sig in self.ops[e]:
                    for k, v in waits.items():
                        engine.wait_ge(sems[k], v)
                    if kind == "op":
                        name, a, k = fn
                        ins = getattr(engine, name)(*a, **k)
                        if sig:
                            ins.then_inc(sems[e], 1)
                    elif kind == "dma":
                        out, in_, dsem = fn
                        engine.dma_start(out=out, in_=in_).then_inc(sems[dsem.key], 16)
            return body

        for e in ENGS:
            hand[e](make(e))


class Mem:
    def __init__(self, nc, base=16512, top=229376):
        self.nc, self.p, self.top, self.n = nc, base, top, 0
        self.peak = base

    def alloc(self, shape, dt):
        esz = 4 if dt == F32 else 2
        nbytes = esz * int(np.prod(shape[1:]))
        off = self.p
        self.p += (nbytes + 63) // 64 * 64
        assert self.p <= self.top, f"SBUF overflow: {self.p} > {self.top}"
        self.peak = max(self.peak, self.p)
        self.n += 1
        return self.nc.alloc_sbuf_tensor_at(f"sb{self.n}", list(shape), dt, offset=off)

    def alloc_at(self, off, shape, dt):
        self.n += 1
        return self.nc.alloc_sbuf_tensor_at(f"sb{self.n}", list(shape), dt, offset=off)

    def mark(self):
        return self.p

    def release(self, m):
        self.p = m


class _Stop(Exception):
    pass


def build_program(stop=None):
    nc = bass.Bass("TRN2", target_bir_lowering=False)
    dram = lambda name, shape, dt=F32, kind="ExternalInput": nc.dram_tensor(name, shape, dt, kind=kind).ap()
    xin = dram("xin", [2048, D])
    hvin = dram("hv", [128, 1])
    colsin = dram("cols", [128, 160])
    w_in = dram("w_in", [DEPTH, D, 5120])
    relT = dram("relT", [DEPTH, 8, 128, 640])
    sgu_g = dram("sgu_ln_g", [DEPTH, 1024])
    sgu_bb = dram("sgu_ln_b", [DEPTH, 1024])
    sgu_w = dram("sgu_w", [DEPTH, 8, 128, 128])
    sgu_b = dram("sgu_b", [DEPTH, 8, 128])
    w_out = dram("w_out", [DEPTH, D, D])
    router_w = dram("router_w", [D, NE])
    router_b = dram("router_b", [NE])
    w_gate = dram("w_gate", [DEPTH, NE, D, FF])
    w_up = dram("w_up", [DEPTH, NE, D, FF])
    w_down = dram("w_down", [DEPTH, NE, FF, D])
    out = dram("out", [1024, D], kind="ExternalOutput")
    cbsc = dram("cbsc", [128, NCH * 512], BF16, kind="Internal")

    P = Prog()
    M = Mem(nc)
    st = ExitStack()
    dbg = dram("dbg", [128, NCH, 1024], kind="ExternalOutput") if stop else None

    def stage(name, Tq):
        if stop != name:
            return
        P.barrier()
        dd = P.dsem("dbg")
        P.dma("sp", dbg[:, :, 0:Tq], X[:, :, 0:Tq], dd, writes=[Slot("dbgo")])
        raise _Stop()

    X = M.alloc([128, NCH, 1024], F32)
    HB = M.alloc([128, NCH, 1024], BF16)
    ident = M.alloc([128, 128], F32)
    ones_f = M.alloc([128, 128], F32)
    ones_b = M.alloc([128, 128], BF16)
    sel = M.alloc([16, NE, 128], F32)
    cols = M.alloc([128, 160], F32)
    rw = M.alloc([128, NCH, NE], F32)
    rb = M.alloc([128, NE], F32)
    hv = M.alloc([128, 1], F32)
    epsc = M.alloc([128, 1], F32)
    combT = M.alloc([16, 1024], F32)
    small = M.alloc([128, 256], F32)
    LN_OFF = M.mark()
    LN_T = ([M.alloc([128, 512], F32), M.alloc([128, 512], F32)], M.alloc([128, 512], F32),
            M.alloc([128, 512], F32), M.alloc([128, 512], F32),
            [M.alloc([128, 512], F32), M.alloc([128, 512], F32)])
    LN_S = ([Slot("sq0"), Slot("sq1")], Slot("stat"), [Slot("tmp0"), Slot("tmp1")])
    ARENA = M.mark()

    PS = [st.enter_context(nc.psum_tensor(f"ps{i}", [128, 512], F32)) for i in range(8)]
    sPS = [Slot(f"ps{i}") for i in range(8)]

    sX = [[Slot(f"X{c}_{tb}") for tb in range(2)] for c in range(NCH)]
    sHB = [Slot(f"HB{tb}") for tb in range(2)]
    sConst = Slot("const")
    sCombT = Slot("combT")
    sSmall = Slot("small")

    dconst = P.dsem("const")
    dmisc = P.dsem("misc")

    CB_OFF = [None]
    GCOL = lambda k, c: cols[:, k * 16 + c: k * 16 + c + 1]

    P.dma("sp", cols[:], colsin, dconst, writes=[sConst])
    P.dma("sp", hv[:], hvin, dconst, writes=[sConst])
    P.dma("sp", rw[:], router_w.rearrange("(c p) e -> p c e", p=128), dconst, writes=[sConst])
    P.dma("sp", rb[:], router_b.partition_broadcast(128), dconst, writes=[sConst])
    P.op("pool", lambda e: e.memset(ones_f[:], 1.0), writes=[sConst])
    P.op("pool", lambda e: e.memset(ones_b[:], 1.0), writes=[sConst])
    P.op("pool", lambda e: e.memset(epsc[:], EPS), writes=[sConst])
    P.op("pool", lambda e: e.affine_select(out=ident[:], in_=ones_f[:], pattern=[[-1, 128]],
                                           compare_op=ALU.is_equal, fill=0.0, base=0, channel_multiplier=1),
         reads=[sConst], writes=[sConst])
    P.op("pool", lambda e: e.affine_select(out=sel[:], in_=ones_f[0:16, :].unsqueeze(1).to_broadcast([16, NE, 128]),
                                           pattern=[[-1, NE], [0, 128]], compare_op=ALU.is_equal, fill=0.0,
                                           base=0, channel_multiplier=1),
         reads=[sConst], writes=[sConst])
    P.barrier()

    def ln_fm(src, nch, Tq, gk, bk, out_f32, out_bf, s_src, s_outb, psA, psB, tmps):
        sq, mean, rstd, nmr, tmp = tmps
        s_sq, s_stat, s_tmp = LN_S
        inv = 1.0 / (nch * 128)
        for tb in range(Tq // 512):
            blk = slice(tb * 512, tb * 512 + 512)
            for c in range(nch):
                P.op("act", lambda e, c=c: e.activation(out=sq[c % 2][:], in_=src[:, c, blk], func=AF.Square),
                     reads=[s_src[c][tb]], writes=[s_sq[c % 2]])
                P.op("pe", lambda e, c=c: e.matmul(PS[psA][:], lhsT=ones_f[:], rhs=src[:, c, blk],
                                                   start=(c == 0), stop=(c == nch - 1)),
                     reads=[s_src[c][tb]], writes=[sPS[psA]], sig=(c == nch - 1))
                P.op("pe", lambda e, c=c: e.matmul(PS[psB][:], lhsT=ones_f[:], rhs=sq[c % 2][:],
                                                   start=(c == 0), stop=(c == nch - 1)),
                     reads=[s_sq[c % 2]], writes=[sPS[psB]], sig=True)
            P.op("dve", lambda e: e.tensor_scalar(out=mean[:], in0=PS[psA][:], scalar1=inv, scalar2=None, op0=ALU.mult),
                 reads=[sPS[psA]], writes=[s_stat])
            P.op("dve", lambda e: e.tensor_tensor(out=nmr[:], in0=mean[:], in1=mean[:], op=ALU.mult),
                 reads=[s_stat], writes=[s_stat])
            P.op("dve", lambda e: e.scalar_tensor_tensor(out=rstd[:], in0=PS[psB][:], scalar=inv, in1=nmr[:],
                                                         op0=ALU.mult, op1=ALU.subtract),
                 reads=[sPS[psB], s_stat], writes=[s_stat])
            P.op("act", lambda e: e.activation(out=rstd[:], in_=rstd[:], func=AF.Sqrt, bias=epsc[:], scale=1.0),
                 reads=[s_stat], writes=[s_stat])
            P.op("dve", lambda e: e.reciprocal(out=rstd[:], in_=rstd[:]), reads=[s_stat], writes=[s_stat])
            P.op("dve", lambda e: e.scalar_tensor_tensor(out=nmr[:], in0=mean[:], scalar=-1.0, in1=rstd[:],
                                                         op0=ALU.mult, op1=ALU.mult),
                 reads=[s_stat], writes=[s_stat])
            for c in range(nch):
                t = tmp[c % 2]
                P.op("dve", lambda e, c=c, t=t: e.tensor_tensor(out=t[:], in0=src[:, c, blk], in1=rstd[:], op=ALU.mult),
                     reads=[s_src[c][tb], s_stat], writes=[s_tmp[c % 2]])
                P.op("dve", lambda e, t=t: e.tensor_tensor(out=t[:], in0=t[:], in1=nmr[:], op=ALU.add),
                     reads=[s_stat], writes=[s_tmp[c % 2]])
                if out_f32 is not None:
                    P.op("act", lambda e, c=c, t=t: e.activation(out=out_f32[:, c, blk], in_=t[:], func=AF.Identity,
                                                                 scale=GCOL(gk, c), bias=GCOL(bk, c)),
                         reads=[s_tmp[c % 2]], writes=[s_src[c][tb]])
                P.op("act", lambda e, c=c, t=t: e.activation(out=out_bf[:, c, blk], in_=t[:], func=AF.Identity,
                                                             scale=GCOL(gk, c), bias=GCOL(bk, c)),
                     reads=[s_tmp[c % 2]], writes=[s_outb[tb]])

    def ln_tmps():
        return LN_T

    def ln_in(row0, ntile, dest_f32, dest_bf, tile0, s_dst_f, s_dst_b):
        xt = [M.alloc([128, D], F32), M.alloc([128, D], F32)]
        junk = M.alloc([128, D], BF16)
        st_ = M.alloc([128, 8], F32)
        s_xt = [Slot("xt0"), Slot("xt1")]
        s_junk, s_st = Slot("junk"), Slot("st")
        dx = [P.dsem(f"x{row0}_0"), P.dsem(f"x{row0}_1")]
        for i in range(ntile):
            b = i % 2
            x_t = xt[b]
            P.dma("sp", x_t[:], xin[row0 + i * 128: row0 + (i + 1) * 128, :], dx[b], writes=[s_xt[b]])
            P.op("dve", lambda e, x_t=x_t: e.tensor_reduce(out=st_[:, 0:1], in_=x_t[:], axis=AX.X, op=ALU.add),
                 reads=[s_xt[b]], writes=[s_st])
            P.op("act", lambda e, x_t=x_t: e.activation(out=junk[:], in_=x_t[:], func=AF.Square, accum_out=st_[:, 1:2]),
                 reads=[s_xt[b], s_st], writes=[s_junk, s_st])
            P.op("dve", lambda e: e.tensor_scalar(out=st_[:, 2:3], in0=st_[:, 0:1], scalar1=1.0 / D, scalar2=None, op0=ALU.mult),
                 reads=[s_st], writes=[s_st])
            P.op("dve", lambda e: e.tensor_tensor(out=st_[:, 3:4], in0=st_[:, 2:3], in1=st_[:, 2:3], op=ALU.mult),
                 reads=[s_st], writes=[s_st])
            P.op("dve", lambda e: e.scalar_tensor_tensor(out=st_[:, 4:5], in0=st_[:, 1:2], scalar=1.0 / D, in1=st_[:, 3:4],
                                                         op0=ALU.mult, op1=ALU.subtract),
                 reads=[s_st], writes=[s_st])
            P.op("act", lambda e: e.activation(out=st_[:, 5:6], in_=st_[:, 4:5], func=AF.Sqrt, bias=epsc[:], scale=1.0),
                 reads=[s_st], writes=[s_st])
            P.op("dve", lambda e: e.reciprocal(out=st_[:, 6:7], in_=st_[:, 5:6]), reads=[s_st], writes=[s_st])
            P.op("dve", lambda e, x_t=x_t: e.tensor_scalar(out=x_t[:], in0=x_t[:], scalar1=st_[:, 2:3], scalar2=st_[:, 6:7],
                                                           op0=ALU.subtract, op1=ALU.mult),
                 reads=[s_st], writes=[s_xt[b]])
            tcol = slice((tile0 + i) * 128, (tile0 + i + 1) * 128)
            tb = (tile0 + i) // 4
            for c4 in range(4):
                bank = c4 % 2
                for cc in range(4):
                    c = c4 * 4 + cc
                    P.op("pe", lambda e, c=c, cc=cc, bank=bank, x_t=x_t: e.transpose(
                        PS[bank][:, cc * 128:(cc + 1) * 128], x_t[:, c * 128:(c + 1) * 128], ident[:]),
                        reads=[s_xt[b]], writes=[sPS[bank]])
                for cc in range(4):
                    c = c4 * 4 + cc
                    if dest_f32 is not None:
                        P.op("act", lambda e, c=c, cc=cc, bank=bank: e.activation(
                            out=dest_f32[:, c, tcol], in_=PS[bank][:, cc * 128:(cc + 1) * 128], func=AF.Identity,
                            scale=GCOL(0, c), bias=GCOL(1, c)),
                            reads=[sPS[bank]], writes=[s_dst_f[c][tb]])
                    P.op("act", lambda e, c=c, cc=cc, bank=bank: e.activation(
                        out=dest_bf[:, c, tcol], in_=PS[bank][:, cc * 128:(cc + 1) * 128], func=AF.Identity,
                        scale=GCOL(0, c), bias=GCOL(1, c)),
                        reads=[sPS[bank]], writes=[s_dst_b[tb]])

    def mixer(l, Tq, CB, sCB, tag):
        nblk = Tq // 512
        ntq = Tq // 128
        ntk = ntq + 4
        m0 = M.mark()
        ymix = M.alloc([128, 8, Tq], BF16)
        s_ymix = [[Slot(f"ymix{c}_{tb}") for tb in range(nblk)] for c in range(8)]
        wq = [M.alloc([128, NCH, 128], BF16) for _ in range(3)]
        s_wq = [Slot(f"wq{i}") for i in range(3)]
        d_wq = [P.dsem(f"wq{tag}{i}") for i in range(3)]
        ring = [0]

        def load_cols(c0):
            i = ring[0] % 3
            ring[0] += 1
            P.dma("pool", wq[i][:], w_in[l, :, c0:c0 + 128].rearrange("(c p) n -> p c n", p=128), d_wq[i],
                  writes=[s_wq[i]])
            return wq[i], s_wq[i]

        def tok_src(tbk):
            if tbk == 0:
                return (lambda c: CB[:, c, :]), sCB
            return (lambda c, tbk=tbk: HB[:, c, (tbk - 1) * 512: tbk * 512]), sHB[tbk - 1]

        def tok_tile(j):
            if j < 4:
                return (lambda c: CB[:, c, j * 128:(j + 1) * 128]), sCB
            return (lambda c: HB[:, c, (j - 4) * 128:(j - 3) * 128]), sHB[(j - 4) // 4]

        m1 = M.mark()
        E = M.alloc([128, 8, 640], BF16)
        etmp = [M.alloc([128, 640], F32), M.alloc([128, 640], F32)]
        s_E, s_etmp = Slot("E"), [Slot("et0"), Slot("et1")]
        d_et = [P.dsem(f"et{tag}0"), P.dsem(f"et{tag}1")]
        kT = [M.alloc([128, 512 + Tq], BF16) for _ in range(2)]
        qT = [M.alloc([128, Tq], BF16) for _ in range(2)]
        Vh = [M.alloc([128, ntk, 128], BF16) for _ in range(2)]
        Pt = [M.alloc([128, 640], BF16) for _ in range(2)]
        rec = [M.alloc([128, 128], F32) for _ in range(2)]
        s_kT, s_qT, s_Vh = [Slot("kT0"), Slot("kT1")], [Slot("qT0"), Slot("qT1")], [Slot("Vh0"), Slot("Vh1")]
        s_Pt, s_rec = [Slot("Pt0"), Slot("Pt1")], [Slot("rec0"), Slot("rec1")]
        for h in range(8):
            b = h % 2
            P.dma("sp", etmp[b][:], relT[l, h], d_et[b], writes=[s_etmp[b]])
            P.op("act", lambda e, h=h, b=b: e.activation(out=E[:, h, :], in_=etmp[b][:], func=AF.Exp),
                 reads=[s_etmp[b]], writes=[s_E])
            P.op("pool", lambda e, h=h: e.memset(E[0:64, h, 64:128], 0.0), writes=[s_E])
            P.op("pool", lambda e, h=h: e.memset(E[64:128, h, 512:576], 0.0), writes=[s_E])
        scale = 1.0 / float(np.sqrt(128.0))
        pending = [load_cols(0), load_cols(1024), load_cols(2048)]
        for h in range(8):
            b = h % 2
            (wqh, s_wqh), (wkh, s_wkh), (wvh, s_wvh) = pending
            for tbk in range(nblk + 1):
                src, s_src = tok_src(tbk)
                bank = tbk % 2
                for c in range(NCH):
                    P.op("pe", lambda e, c=c, src=src, bank=bank, wkh=wkh: e.matmul(
                        PS[bank][:], lhsT=wkh[:, c, :], rhs=src(c), start=(c == 0), stop=(c == NCH - 1)),
                        reads=[s_wkh, s_src], writes=[sPS[bank]], sig=(c == NCH - 1))
                P.op("act", lambda e, b=b, bank=bank, tbk=tbk: e.copy(out=kT[b][:, tbk * 512:(tbk + 1) * 512], in_=PS[bank][:]),
                     reads=[sPS[bank]], writes=[s_kT[b]])
            for tb in range(nblk):
                src, s_src = tok_src(tb + 1)
                bank = 2 + tb % 2
                for c in range(NCH):
                    P.op("pe", lambda e, c=c, src=src, bank=bank, wqh=wqh: e.matmul(
                        PS[bank][:], lhsT=wqh[:, c, :], rhs=src(c), start=(c == 0), stop=(c == NCH - 1)),
                        reads=[s_wqh, s_src], writes=[sPS[bank]], sig=(c == NCH - 1))
                P.op("act", lambda e, b=b, bank=bank, tb=tb: e.copy(out=qT[b][:, tb * 512:(tb + 1) * 512], in_=PS[bank][:]),
                     reads=[sPS[bank]], writes=[s_qT[b]])
            for j4 in range(ntk // 4):
                bank = 4 + j4 % 2
                for jj in range(4):
                    j = j4 * 4 + jj
                    src, s_src = tok_tile(j)
                    for c in range(NCH):
                        P.op("pe", lambda e, c=c, src=src, bank=bank, jj=jj, wvh=wvh: e.matmul(
                            PS[bank][:, jj * 128:(jj + 1) * 128], lhsT=src(c), rhs=wvh[:, c, :],
                            start=(c == 0), stop=(c == NCH - 1)),
                            reads=[s_wvh, s_src], writes=[sPS[bank]], sig=(c == NCH - 1))
                P.op("dve", lambda e, b=b, bank=bank, j4=j4: e.tensor_copy(
                    out=Vh[b][:, j4 * 4:(j4 + 1) * 4, :], in_=PS[bank][:].rearrange("p (j d) -> p j d", d=128)),
                    reads=[sPS[bank]], writes=[s_Vh[b]])
            if h < 7:
                pending = [load_cols((h + 1) * 128), load_cols(1024 + (h + 1) * 128), load_cols(2048 + (h + 1) * 128)]
            def scores(i):
                pb = i % 2
                sa, sb_ = (6, 7) if i % 2 == 0 else (0, 1)
                for j in range(5):
                    dst = PS[sa][:, j * 128:(j + 1) * 128] if j < 4 else PS[sb_][:, 0:128]
                    P.op("pe", lambda e: e.matmul(
                        dst, lhsT=kT[b][:, (i + j) * 128:(i + j + 1) * 128], rhs=qT[b][:, i * 128:(i + 1) * 128],
                        start=True, stop=True),
                        reads=[s_kT[b], s_qT[b]], writes=[sPS[sa] if j < 4 else sPS[sb_]], sig=(j >= 3))
                P.op("act", lambda e: e.activation(out=Pt[pb][:, 0:512], in_=PS[sa][:], func=AF.Exp, scale=scale),
                     reads=[sPS[sa]], writes=[s_Pt[pb]])
                P.op("act", lambda e: e.activation(out=Pt[pb][:, 512:640], in_=PS[sb_][:, 0:128], func=AF.Exp, scale=scale),
                     reads=[sPS[sb_]], writes=[s_Pt[pb]])
                nctx = max(0, 4 - i)
                if nctx > 0:
                    P.op("dve", lambda e: e.scalar_tensor_tensor(
                        out=Pt[pb][:, 0:nctx * 128], in0=Pt[pb][:, 0:nctx * 128], scalar=hv[:, 0:1],
                        in1=E[:, h, 0:nctx * 128], op0=ALU.mult, op1=ALU.mult),
                        reads=[s_E, sConst], writes=[s_Pt[pb]])
                P.op("dve", lambda e: e.tensor_tensor(
                    out=Pt[pb][:, nctx * 128:640], in0=Pt[pb][:, nctx * 128:640], in1=E[:, h, nctx * 128:640], op=ALU.mult),
                    reads=[s_E], writes=[s_Pt[pb]])

            def pv(i):
                pb = i % 2
                od = 2 + (i % 2)
                for j in range(5):
                    P.op("pe", lambda e: e.matmul(
                        PS[od][:, 0:128], lhsT=Vh[b][:, i + j, :], rhs=Pt[pb][:, j * 128:(j + 1) * 128],
                        start=(j == 0), stop=(j == 4)),
                        reads=[s_Vh[b], s_Pt[pb]], writes=[sPS[od]], sig=False)
                for j in range(5):
                    P.op("pe", lambda e: e.matmul(
                        PS[od][:, 128:256], lhsT=ones_b[:], rhs=Pt[pb][:, j * 128:(j + 1) * 128],
                        start=(j == 0), stop=(j == 4)),
                        reads=[s_Pt[pb], sConst], writes=[sPS[od]], sig=(j == 4))
                P.op("dve", lambda e: e.reciprocal(out=rec[pb][:], in_=PS[od][:, 128:256]),
                     reads=[sPS[od]], writes=[s_rec[pb]])
                P.op("dve", lambda e: e.tensor_tensor(
                    out=ymix[:, h, i * 128:(i + 1) * 128], in0=PS[od][:, 0:128], in1=rec[pb][:], op=ALU.mult),
                    reads=[sPS[od], s_rec[pb]], writes=[s_ymix[h][i // 4]])

            for i in range(ntq + 1):
                if i < ntq:
                    scores(i)
                if i >= 1:
                    pv(i - 1)
        P.barrier()
        M.release(m1)

        def out_proj(row0, first):
            wo = [M.alloc([128, 8, 128], BF16) for _ in range(3)]
            s_wo = [Slot(f"wo{i}") for i in range(3)]
            d_wo = [P.dsem(f"wo{tag}{row0}_{i}") for i in range(3)]

            def load(db):
                i = db % 3
                P.dma("pool", wo[i][:], w_out[l, row0:row0 + 1024, db * 128:(db + 1) * 128].rearrange("(c p) n -> p c n", p=128),
                      d_wo[i], writes=[s_wo[i]])
            load(0); load(1)
            for db in range(NCH):
                if db + 2 < NCH:
                    load(db + 2)
                i = db % 3
                for tb in range(nblk):
                    bank = (db * nblk + tb) % 4
                    blk = slice(tb * 512, tb * 512 + 512)
                    for c in range(8):
                        P.op("pe", lambda e, c=c, i=i, bank=bank, blk=blk: e.matmul(
                            PS[bank][:], lhsT=wo[i][:, c, :], rhs=ymix[:, c, blk], start=(c == 0), stop=(c == 7)),
                            reads=[s_wo[i], s_ymix[c][tb]], writes=[sPS[bank]], sig=(c == 7))
                    if first:
                        P.op("dve", lambda e, db=db, bank=bank, blk=blk: e.scalar_tensor_tensor(
                            out=X[:, db, blk], in0=X[:, db, blk], scalar=ALPHA, in1=PS[bank][:], op0=ALU.mult, op1=ALU.add),
                            reads=[sPS[bank]], writes=[sX[db][tb]])
                    else:
                        P.op("dve", lambda e, db=db, bank=bank, blk=blk: e.tensor_tensor(
                            out=X[:, db, blk], in0=X[:, db, blk], in1=PS[bank][:], op=ALU.add),
                            reads=[sPS[bank]], writes=[sX[db][tb]])

        m2 = M.mark()
        out_proj(0, True)
        P.barrier()
        M.release(m2)

        gbc = M.alloc([128, 1024], F32)
        bbc = M.alloc([128, 1024], F32)
        bsb = M.alloc([128, 8, 128], F32)
        WmT = M.alloc([128, 8, 128], BF16)
        vln = M.alloc_at(CB_OFF[0], [128, ntq, 1024], BF16)
        wvs = M.alloc([128, NCH, 256], BF16)
        vtmp_off = M.mark()
        vtmp = M.alloc([128, 1024], F32)
        wtmp = M.alloc_at(vtmp_off, [128, 8, 128], F32)
        junk = M.alloc([128, 1024], BF16)
        stt = M.alloc([128, ntq, 12], F32)
        ug = [M.alloc_at(LN_OFF + k * 4096, [128, Tq], F32) for k in range(2)]
        mtmp = [M.alloc([128, 512], F32) for _ in range(2)]
        s_g, s_wtmp, s_WmT, s_wvs, s_junk = Slot("g"), Slot("wtmp"), Slot("WmT"), Slot("wvs"), Slot("junk")
        s_vtmp = s_wtmp
        s_vln = [Slot(f"vln{i}") for i in range(ntq)]
        s_stt = [Slot(f"stt{i}") for i in range(ntq)]
        s_ug, s_mtmp = [Slot("ug0"), Slot("ug1")], [Slot("mt0"), Slot("mt1")]
        d_g, d_wvs = P.dsem(f"sg{tag}"), P.dsem(f"wvs{tag}")
        P.dma("sp", gbc[:], sgu_g[l].partition_broadcast(128), d_g, writes=[s_g])
        P.dma("sp", bbc[:], sgu_bb[l].partition_broadcast(128), d_g, writes=[s_g])
        P.dma("sp", bsb[:].rearrange("p g t -> p (g t)"), sgu_b[l].rearrange("g t -> (g t)").partition_broadcast(128), d_g, writes=[s_g])
        P.dma("sp", wtmp[:], sgu_w[l].rearrange("g t s -> t g s"), d_g, writes=[s_wtmp])
        for g in range(8):
            P.op("pool", lambda e, g=g: e.affine_select(out=wtmp[:, g, :], in_=wtmp[:, g, :], pattern=[[-1, 128]],
                                                        compare_op=ALU.is_ge, fill=0.0, base=0, channel_multiplier=1),
                 reads=[s_g], writes=[s_wtmp])
        for g4 in range(2):
            for gg in range(4):
                g = g4 * 4 + gg
                P.op("pe", lambda e, g=g, gg=gg, g4=g4: e.transpose(PS[g4][:, gg * 128:(gg + 1) * 128], wtmp[:, g, :], ident[:]),
                     reads=[s_wtmp, sConst], writes=[sPS[g4]])
            P.op("act", lambda e, g4=g4: e.copy(out=WmT[:, g4 * 4:(g4 + 1) * 4, :], in_=PS[g4][:].rearrange("p (g t) -> p g t", t=128)),
                 reads=[sPS[g4]], writes=[s_WmT])
        for qt in range(4):
            P.dma("pool", wvs[:], w_in[l, :, 4096 + qt * 256: 4096 + (qt + 1) * 256].rearrange("(c p) n -> p c n", p=128),
                  d_wvs, writes=[s_wvs])
            for i in range(ntq):
                bank = 2 + i % 4
                for c in range(NCH):
                    P.op("pe", lambda e, c=c, i=i, bank=bank: e.matmul(
                        PS[bank][:, 0:256], lhsT=HB[:, c, i * 128:(i + 1) * 128], rhs=wvs[:, c, :], start=(c == 0), stop=(c == NCH - 1)),
                        reads=[s_wvs, sHB[i // 4]], writes=[sPS[bank]], sig=(c == NCH - 1))
                P.op("act", lambda e, i=i, bank=bank, qt=qt: e.activation(
                    out=vln[:, i, qt * 256:(qt + 1) * 256], in_=PS[bank][:, 0:256], func=AF.Gelu, accum_out=stt[:, i, qt:qt + 1]),
                    reads=[sPS[bank]], writes=[s_vln[i], s_stt[i]])
        for i in range(ntq):
            S_ = lambda k, i=i: stt[:, i, k:k + 1]
            P.op("act", lambda e, i=i: e.activation(out=junk[:], in_=vln[:, i, :], func=AF.Square, accum_out=stt[:, i, 4:5]),
                 reads=[s_vln[i]], writes=[s_junk, s_stt[i]])
            P.op("dve", lambda e, S_=S_, i=i: e.tensor_reduce(out=S_(5), in_=stt[:, i, 0:4], axis=AX.X, op=ALU.add), reads=[s_stt[i]], writes=[s_stt[i]])
            P.op("dve", lambda e, S_=S_: e.tensor_scalar(out=S_(5), in0=S_(5), scalar1=1.0 / 1024, scalar2=None, op0=ALU.mult),
                 reads=[s_stt[i]], writes=[s_stt[i]])
            P.op("dve", lambda e, S_=S_: e.tensor_tensor(out=S_(6), in0=S_(5), in1=S_(5), op=ALU.mult), reads=[s_stt[i]], writes=[s_stt[i]])
            P.op("dve", lambda e, S_=S_: e.scalar_tensor_tensor(out=S_(7), in0=S_(4), scalar=1.0 / 1024, in1=S_(6),
                                                                op0=ALU.mult, op1=ALU.subtract), reads=[s_stt[i]], writes=[s_stt[i]])
            P.op("act", lambda e, S_=S_: e.activation(out=S_(8), in_=S_(7), func=AF.Sqrt, bias=epsc[:], scale=1.0),
                 reads=[s_stt[i]], writes=[s_stt[i]])
            P.op("dve", lambda e, S_=S_: e.reciprocal(out=S_(9), in_=S_(8)), reads=[s_stt[i]], writes=[s_stt[i]])
            P.op("dve", lambda e, S_=S_, i=i: e.tensor_scalar(out=vtmp[:], in0=vln[:, i, :], scalar1=S_(5), scalar2=S_(9),
                                                              op0=ALU.subtract, op1=ALU.mult),
                 reads=[s_vln[i], s_stt[i]], writes=[s_vtmp])
            P.op("dve", lambda e: e.tensor_tensor(out=vtmp[:], in0=vtmp[:], in1=gbc[:], op=ALU.mult), reads=[s_g], writes=[s_vtmp])
            P.op("dve", lambda e, i=i: e.tensor_tensor(out=vln[:, i, :], in0=vtmp[:], in1=bbc[:], op=ALU.add),
                 reads=[s_g, s_vtmp], writes=[s_vln[i]])
        pend = load_cols(3072)
        for g in range(8):
            wug, s_wug = pend
            if g < 7:
                pend = load_cols(3072 + (g + 1) * 128)
            ub = g % 2
            for tb in range(nblk):
                bank = tb % 2
                blk = slice(tb * 512, tb * 512 + 512)
                for c in range(NCH):
                    P.op("pe", lambda e, c=c, bank=bank, blk=blk, wug=wug: e.matmul(
                        PS[bank][:], lhsT=wug[:, c, :], rhs=HB[:, c, blk], start=(c == 0), stop=(c == NCH - 1)),
                        reads=[s_wug, sHB[tb]], writes=[sPS[bank]], sig=(c == NCH - 1))
                P.op("act", lambda e, ub=ub, bank=bank, blk=blk: e.activation(out=ug[ub][:, blk], in_=PS[bank][:], func=AF.Gelu),
                     reads=[sPS[bank]], writes=[s_ug[ub]])
            for tb in range(nblk):
                bank = 6 + tb % 2
                blk = slice(tb * 512, tb * 512 + 512)
                for ii in range(4):
                    i = tb * 4 + ii
                    P.op("pe", lambda e, g=g, i=i, ii=ii, bank=bank: e.matmul(
                        PS[bank][:, ii * 128:(ii + 1) * 128], lhsT=vln[:, i, g * 128:(g + 1) * 128], rhs=WmT[:, g, :],
                        start=True, stop=True),
                        reads=[s_vln[i], s_WmT], writes=[sPS[bank]], sig=(ii == 3))
                mt = mtmp[tb % 2]
                P.op("dve", lambda e, g=g, bank=bank, mt=mt: e.tensor_tensor(
                    out=mt[:].rearrange("p (r t) -> p r t", t=128), in0=PS[bank][:].rearrange("p (r t) -> p r t", t=128),
                    in1=bsb[:, g, :].unsqueeze(1).to_broadcast([128, 4, 128]), op=ALU.add),
                    reads=[sPS[bank], s_g], writes=[s_mtmp[tb % 2]])
                P.op("dve", lambda e, g=g, ub=ub, blk=blk, mt=mt: e.tensor_tensor(
                    out=ymix[:, g, blk], in0=mt[:], in1=ug[ub][:, blk], op=ALU.mult),
                    reads=[s_mtmp[tb % 2], s_ug[ub]], writes=[s_ymix[g][tb]])
        P.barrier()
        M.release(m2)
        out_proj(1024, False)
        P.barrier()
        M.release(m0)

    def router(Tq):
        ntq = Tq // 128
        sm = lambda a, b: small[:, a:b]
        for i in range(ntq):
            tcol = slice(i * 128, (i + 1) * 128)
            tb = i // 4
            for c in range(NCH):
                P.op("pe", lambda e, c=c, tcol=tcol: e.matmul(PS[0][:, 0:NE], lhsT=X[:, c, tcol], rhs=rw[:, c, :],
                                                             start=(c == 0), stop=(c == NCH - 1)),
                     reads=[sX[c][tb], sConst], writes=[sPS[0]], sig=(c == NCH - 1))
            R, W = [sSmall], [sSmall]
            lg, ex, pr, gm, m, eq, m2, sl, cb = (sm(0, 16), sm(16, 32), sm(32, 48), sm(48, 52), sm(64, 80), sm(80, 96),
                                                sm(96, 112), sm(112, 128), sm(128, 144))
            pairs, gs = sm(160, 184), sm(184, 188)
            c1 = lambda k: small[:, 200 + k: 201 + k]
            P.op("dve", lambda e: e.tensor_tensor(out=lg, in0=PS[0][:, 0:NE], in1=rb[:], op=ALU.add), reads=[sPS[0], sConst], writes=W)
            P.op("dve", lambda e: e.tensor_reduce(out=c1(0), in_=lg, axis=AX.X, op=ALU.max), reads=R, writes=W)
            P.op("dve", lambda e: e.tensor_scalar(out=c1(1), in0=c1(0), scalar1=-1.0, scalar2=None, op0=ALU.mult), reads=R, writes=W)
            P.op("act", lambda e: e.activation(out=ex, in_=lg, func=AF.Exp, bias=c1(1), scale=1.0, accum_out=c1(2)), reads=R, writes=W)
            P.op("dve", lambda e: e.reciprocal(out=c1(3), in_=c1(2)), reads=R, writes=W)
            P.op("dve", lambda e: e.tensor_scalar(out=pr, in0=ex, scalar1=c1(3), scalar2=None, op0=ALU.mult), reads=R, writes=W)
            pv = pr.rearrange("p (g k) -> p g k", k=4)
            pw = pairs.rearrange("p (g k) -> p g k", k=6)
            for idx, (a, b_) in enumerate([(0, 1), (0, 2), (0, 3), (1, 2), (1, 3), (2, 3)]):
                P.op("dve", lambda e, idx=idx, a=a, b_=b_: e.tensor_tensor(out=pw[:, :, idx:idx + 1], in0=pv[:, :, a:a + 1],
                                                                             in1=pv[:, :, b_:b_ + 1], op=ALU.add), reads=R, writes=W)
            P.op("dve", lambda e: e.tensor_reduce(out=gs, in_=pw, axis=AX.X, op=ALU.max), reads=R, writes=W)
            P.op("dve", lambda e: e.tensor_reduce(out=c1(4), in_=gs, axis=AX.X, op=ALU.max), reads=R, writes=W)
            P.op("dve", lambda e: e.tensor_scalar(out=gm, in0=gs, scalar1=c1(4), scalar2=None, op0=ALU.is_ge), reads=R, writes=W)
            mv = m.rearrange("p (g k) -> p g k", k=4)
            P.op("dve", lambda e: e.tensor_tensor(out=mv, in0=pv, in1=gm.unsqueeze(2).to_broadcast([128, 4, 4]), op=ALU.mult), reads=R, writes=W)
            P.op("dve", lambda e: e.tensor_scalar(out=gs, in0=gm, scalar1=-1.0, scalar2=None, op0=ALU.add), reads=R, writes=W)
            P.op("dve", lambda e: e.tensor_tensor(out=mv, in0=mv, in1=gs.unsqueeze(2).to_broadcast([128, 4, 4]), op=ALU.add), reads=R, writes=W)
            P.op("dve", lambda e: e.tensor_reduce(out=c1(5), in_=m, axis=AX.X, op=ALU.max), reads=R, writes=W)
            P.op("dve", lambda e: e.tensor_scalar(out=eq, in0=m, scalar1=c1(5), scalar2=None, op0=ALU.is_equal), reads=R, writes=W)
            P.op("dve", lambda e: e.scalar_tensor_tensor(out=m2, in0=eq, scalar=-2.0, in1=m, op0=ALU.mult, op1=ALU.add), reads=R, writes=W)
            P.op("dve", lambda e: e.tensor_reduce(out=c1(6), in_=m2, axis=AX.X, op=ALU.max), reads=R, writes=W)
            P.op("dve", lambda e: e.tensor_scalar(out=sl, in0=m, scalar1=c1(6), scalar2=None, op0=ALU.is_ge), reads=R, writes=W)
            P.op("dve", lambda e: e.tensor_tensor(out=c1(7), in0=c1(5), in1=c1(6), op=ALU.add), reads=R, writes=W)
            P.op("dve", lambda e: e.reciprocal(out=c1(8), in_=c1(7)), reads=R, writes=W)
            P.op("dve", lambda e: e.scalar_tensor_tensor(out=cb, in0=m, scalar=c1(8), in1=sl, op0=ALU.mult, op1=ALU.mult), reads=R, writes=W)
            P.op("pe", lambda e: e.transpose(PS[1][0:NE, 0:128], cb, ident[:]), reads=R + [sConst], writes=[sPS[1]])
            P.op("act", lambda e, tcol=tcol: e.copy(out=combT[:, tcol], in_=PS[1][0:NE, 0:128]), reads=[sPS[1]], writes=[sCombT])

    def moe(l, Tq, tag):
        nblk = Tq // 512
        m0 = M.mark()
        hid = M.alloc_at(CB_OFF[0], [128, 8, 1024], BF16)
        s_hid = [Slot(f"hid{tb}") for tb in range(nblk)]
        wg = [M.alloc([128, NCH, 256], BF16) for _ in range(2)]
        wu = [M.alloc([128, NCH, 256], BF16) for _ in range(2)]
        wd = [M.alloc([128, 8, 512], BF16) for _ in range(2)]
        s_wg, s_wu, s_wd = [Slot("wg0"), Slot("wg1")], [Slot("wu0"), Slot("wu1")], [Slot("wd0"), Slot("wd1")]
        d_wg = [P.dsem(f"wg{tag}{i}") for i in range(2)]
        d_wu = [P.dsem(f"wu{tag}{i}") for i in range(2)]
        d_wd = [P.dsem(f"wd{tag}{i}") for i in range(2)]
        ssb = [M.alloc([128, 512], F32) for _ in range(2)]
        tsb = [M.alloc([128, 512], F32) for _ in range(2)]
        s_ssb, s_tsb = [Slot("ssb0"), Slot("ssb1")], [Slot("tsb0"), Slot("tsb1")]

        def load_gu(n):
            e_, fb = divmod(n, 4)
            i = n % 2
            P.dma("pool", wg[i][:], w_gate[l, e_, :, fb * 256:(fb + 1) * 256].rearrange("(c p) n -> p c n", p=128), d_wg[i], writes=[s_wg[i]])
            P.dma("pool", wu[i][:], w_up[l, e_, :, fb * 256:(fb + 1) * 256].rearrange("(c p) n -> p c n", p=128), d_wu[i], writes=[s_wu[i]])

        def load_d(n):
            e_, dq = divmod(n, 4)
            i = n % 2
            P.dma("pool", wd[i][:], w_down[l, e_, :, dq * 512:(dq + 1) * 512].rearrange("(c p) n -> p c n", p=128), d_wd[i], writes=[s_wd[i]])

        load_gu(0); load_gu(1); load_d(0); load_d(1)
        yield
        cnt = 0
        for e_ in range(NE):
            for tb in range(nblk):
                P.op("pe", lambda e, e_=e_, tb=tb: e.matmul(PS[4 + tb][:], lhsT=sel[:, e_, :], rhs=combT[:, tb * 512:(tb + 1) * 512],
                                                           start=True, stop=True),
                     reads=[sCombT, sConst], writes=[sPS[4 + tb]])
            for fb in range(4):
                n = e_ * 4 + fb
                i = n % 2
                for tb in range(nblk):
                    blk = slice(tb * 512, tb * 512 + 512)
                    for fs in range(2):
                        f128 = fb * 2 + fs
                        gb, ub = cnt % 2, 2 + cnt % 2
                        cnt += 1
                        for c in range(NCH):
                            P.op("pe", lambda e, c=c, i=i, fs=fs, gb=gb, blk=blk: e.matmul(
                                PS[gb][:], lhsT=wg[i][:, c, fs * 128:(fs + 1) * 128], rhs=HB[:, c, blk], start=(c == 0), stop=(c == NCH - 1)),
                                reads=[s_wg[i], sHB[tb]], writes=[sPS[gb]], sig=(c == NCH - 1))
                        for c in range(NCH):
                            P.op("pe", lambda e, c=c, i=i, fs=fs, ub=ub, blk=blk: e.matmul(
                                PS[ub][:], lhsT=wu[i][:, c, fs * 128:(fs + 1) * 128], rhs=HB[:, c, blk], start=(c == 0), stop=(c == NCH - 1)),
                                reads=[s_wu[i], sHB[tb]], writes=[sPS[ub]], sig=(c == NCH - 1))
                        k = cnt % 2
                        P.op("act", lambda e, k=k, gb=gb: e.activation(out=ssb[k][:], in_=PS[gb][:], func=AF.Silu),
                             reads=[sPS[gb]], writes=[s_ssb[k]])
                        P.op("dve", lambda e, k=k, ub=ub: e.tensor_tensor(out=tsb[k][:], in0=ssb[k][:], in1=PS[ub][:], op=ALU.mult),
                             reads=[s_ssb[k], sPS[ub]], writes=[s_tsb[k]])
                        P.op("dve", lambda e, k=k, tb=tb, f128=f128, blk=blk: e.tensor_tensor(
                            out=hid[:, f128, blk], in0=tsb[k][:], in1=PS[4 + tb][:], op=ALU.mult),
                            reads=[s_tsb[k], sPS[4 + tb]], writes=[s_hid[tb]])
                if n + 2 < NE * 4:
                    load_gu(n + 2)
            for dq in range(4):
                n = e_ * 4 + dq
                i = n % 2
                for tb in range(nblk):
                    blk = slice(tb * 512, tb * 512 + 512)
                    for ds in range(4):
                        db = dq * 4 + ds
                        bank = 6 + cnt % 2
                        cnt += 1
                        for c in range(8):
                            P.op("pe", lambda e, c=c, i=i, ds=ds, bank=bank, blk=blk: e.matmul(
                                PS[bank][:], lhsT=wd[i][:, c, ds * 128:(ds + 1) * 128], rhs=hid[:, c, blk], start=(c == 0), stop=(c == 7)),
                                reads=[s_wd[i], s_hid[tb]], writes=[sPS[bank]], sig=(c == 7))
                        if e_ == 0:
                            P.op("dve", lambda e, db=db, bank=bank, blk=blk: e.scalar_tensor_tensor(
                                out=X[:, db, blk], in0=X[:, db, blk], scalar=ALPHA, in1=PS[bank][:], op0=ALU.mult, op1=ALU.add),
                                reads=[sPS[bank]], writes=[sX[db][tb]])
                        else:
                            P.op("dve", lambda e, db=db, bank=bank, blk=blk: e.tensor_tensor(
                                out=X[:, db, blk], in0=X[:, db, blk], in1=PS[bank][:], op=ALU.add),
                                reads=[sPS[bank]], writes=[sX[db][tb]])
                if n + 2 < NE * 4:
                    load_d(n + 2)
        M.release(m0)

    def layer(l, Tq, CB, sCB, tag):
        stage(tag + "_in", Tq)
        mixer(l, Tq, CB, sCB, tag)
        stage(tag + "_mix", Tq)
        g = moe(l, Tq, tag)
        next(g)
        ln_fm(X, NCH, Tq, 2 + 4 * l, 3 + 4 * l, X, HB, sX, sHB, 6, 7, ln_tmps())
        stage(tag + "_ln1", Tq)
        router(Tq)
        next(g, None)
        stage(tag + "_moe", Tq)
        ln_fm(X, NCH, Tq, 4 + 4 * l, 5 + 4 * l, X, HB, sX, sHB, 6, 7, ln_tmps())
        P.barrier()
        stage(tag + "_ln2", Tq)

    CB_OFF[0] = M.mark()
    CB = M.alloc([128, NCH, 512], BF16)
    sCB = Slot("CB")
    s_cbsc = Slot("cbsc")
    ARENA2 = M.mark()
    d_cb = P.dsem("cbsc")

    def _main_seq():
        ln_in(0, 4, None, CB, 0, None, [sCB] * 2)
        P.barrier(); M.release(ARENA2)
        ln_in(512, 4, X, HB, 0, sX, sHB)
        P.barrier(); M.release(ARENA2)
        layer(0, 512, CB, sCB, "H")
        P.dma("sp", cbsc.rearrange("p (c t) -> p c t", c=NCH), HB[:, :, 0:512], d_cb, reads=[sHB[0]], writes=[s_cbsc])
        ln_in(512, 4, None, CB, 0, None, [sCB] * 2)
        P.barrier(); M.release(ARENA2)
        ln_in(1024, 8, X, HB, 0, sX, sHB)
        P.barrier(); M.release(ARENA2)
        layer(0, 1024, CB, sCB, "O")
        P.dma("sp", CB[:], cbsc.rearrange("p (c t) -> p c t", c=NCH), d_cb, reads=[s_cbsc], writes=[sCB])
        layer(1, 1024, CB, sCB, "L")

        ot = [M.alloc([128, D], F32), M.alloc([128, D], F32)]
        s_ot = [Slot("ot0"), Slot("ot1")]
        d_out = [P.dsem("out0"), P.dsem("out1")]
        for i in range(8):
            b = i % 2
            for c4 in range(4):
                bank = c4 % 4
                for cc in range(4):
                    c = c4 * 4 + cc
                    P.op("pe", lambda e, c=c, cc=cc, bank=bank, i=i: e.transpose(
                        PS[bank][:, cc * 128:(cc + 1) * 128], X[:, c, i * 128:(i + 1) * 128], ident[:]),
                        reads=[sX[c][i // 4], sConst], writes=[sPS[bank]])
                if c4 % 2:
                    P.op("act", lambda e, b=b, c4=c4, bank=bank: e.copy(out=ot[b][:, c4 * 512:(c4 + 1) * 512], in_=PS[bank][:]),
                         reads=[sPS[bank]], writes=[s_ot[b]])
                else:
                    P.op("dve", lambda e, b=b, c4=c4, bank=bank: e.tensor_copy(out=ot[b][:, c4 * 512:(c4 + 1) * 512], in_=PS[bank][:]),
                         reads=[sPS[bank]], writes=[s_ot[b]])
            P.dma("sp", out[i * 128:(i + 1) * 128, :], ot[b][:], d_out[b], reads=[s_ot[b]], writes=[Slot("o")])

    try:
        _main_seq()
    except _Stop:
        pass
    P.barrier()
    P.emit(nc, st)
    st.close()
    return nc, M.peak


_CACHE = {}


def _rel_index():
    k = np.arange(128)[:, None, None]
    j = np.arange(5)[None, :, None]
    q = np.arange(128)[None, None, :]
    rel = (4 - j) * 128 + q - k
    return (np.clip(rel, -128, 128) + 128).reshape(128, 640)


def kernel(x, ln_in_g, ln_in_b, w_in, rel_bias, sgu_ln_g, sgu_ln_b, sgu_w, sgu_b, w_out, ln1_g, ln1_b,
           router_w, router_b, w_gate, w_up, w_down, ln2_g, ln2_b):
    f = lambda a: np.ascontiguousarray(np.asarray(a, dtype=np.float32))
    x = f(x)
    if "nc" not in _CACHE:
        _CACHE["nc"], _CACHE["peak"] = build_program()
    nc = _CACHE["nc"]
    vecs = [ln_in_g, ln_in_b, ln1_g[0], ln1_b[0], ln2_g[0], ln2_b[0], ln1_g[1], ln1_b[1], ln2_g[1], ln2_b[1]]
    cols = np.concatenate([f(v).reshape(NCH, 128).T for v in vecs], axis=1)
    relT = f(np.asarray(rel_bias)[:, :, _rel_index()])
    shared = {"cols": f(cols), "w_in": f(w_in), "relT": relT, "sgu_ln_g": f(sgu_ln_g), "sgu_ln_b": f(sgu_ln_b),
              "sgu_w": f(sgu_w), "sgu_b": f(sgu_b), "w_out": f(w_out), "router_w": f(router_w), "router_b": f(router_b),
              "w_gate": f(w_gate), "w_up": f(w_up), "w_down": f(w_down)}
    in_maps = []
    for core in range(N_CORES):
        b, half = divmod(core, 2)
        if half == 0:
            xin = np.concatenate([np.zeros((1024, D), np.float32), x[b, 0:1024]], axis=0)
        else:
            xin = x[b]
        hvv = np.full((128, 1), float(half), np.float32)
        in_maps.append({"xin": np.ascontiguousarray(xin), "hv": hvv, **shared})
    res = run_bass_kernel_spmd(nc, in_maps, core_ids=list(range(N_CORES)))
    outp = np.empty((4, 2048, D), np.float32)
    for core in range(N_CORES):
        b, half = divmod(core, 2)
        outp[b, half * 1024:(half + 1) * 1024] = res.results[core]["out"]
    return outp
```

```python
from contextlib import ExitStack

import numpy as np
import concourse.bass as bass
import concourse.mybir as mybir
from concourse.bass_utils import run_bass_kernel_spmd

F32 = mybir.dt.float32
BF16 = mybir.dt.bfloat16
AF = mybir.ActivationFunctionType
ALU = mybir.AluOpType
AX = mybir.AxisListType

ENGS = ("pe", "act", "dve", "pool", "sp")

D = 2048
NCH = 16
DEPTH = 2
ALPHA = (2.0 * DEPTH) ** 0.25
EPS = 1e-5
NE = 16
FF = 1024
N_CORES = 8


class Box:
    __slots__ = ("key", "val")

    def __init__(self, key, val=None):
        self.key = key
        self.val = val


class Slot:
    __slots__ = ("name", "w", "r")

    def __init__(self, name):
        self.name = name
        self.w = None
        self.r = {}


class DSem:
    def __init__(self, prog, name):
        self.name = f"{name}_{len(prog.dsems)}"
        self.count = 0
        self.key = "d:" + self.name
        prog.dsems.append(self)


class _Rec:
    def __init__(self):
        self.call = None

    def __getattr__(self, name):
        def f(*a, **k):
            self.call = (name, a, k)
        return f


class Prog:
    def __init__(self):
        self.ops = {e: [] for e in ENGS}
        self.count = {e: 0 for e in ENGS}
        self.pending = {e: [] for e in ENGS}
        self.seen = {e: {} for e in ENGS}
        self.dsems = []
        self.same_engine_sync = True

    def dsem(self, name):
        return DSem(self, name)

    def _need(self, eng, box, waits):
        if box is None:
            return
        if box.key == eng and (eng == "pe" or not self.same_engine_sync):
            return
        assert box.val is not None, f"dependency on unsignalled op ({box.key})"
        k, v = box.key, box.val
        if self.seen[eng].get(k, 0) >= v:
            return
        self.seen[eng][k] = v
        waits[k] = max(waits.get(k, 0), v)

    def _deps(self, eng, reads, writes):
        waits = {}
        for s in reads:
            self._need(eng, s.w, waits)
        for s in writes:
            self._need(eng, s.w, waits)
            for b in s.r.values():
                self._need(eng, b, waits)
        return waits

    def _mark(self, box, reads, writes):
        for s in reads:
            s.r[box.key] = box
        for s in writes:
            s.w = box
            s.r = {}

    def op(self, eng, fn, reads=(), writes=(), sig=True):
        waits = self._deps(eng, reads, writes)
        box = Box(eng)
        if sig:
            self.count[eng] += 1
            box.val = self.count[eng]
            for b in self.pending[eng]:
                b.val = box.val
            self.pending[eng] = []
        else:
            self.pending[eng].append(box)
        self._mark(box, reads, writes)
        rec = _Rec()
        fn(rec)
        assert rec.call is not None
        self.ops[eng].append(("op", rec.call, waits, sig))
        return box

    def dma(self, eng, out, in_, dsem, reads=(), writes=()):
        waits = self._deps(eng, reads, writes)
        dsem.count += 16
        box = Box(dsem.key, dsem.count)
        self._mark(box, reads, writes)
        self.ops[eng].append(("dma", (out, in_, dsem), waits, True))
        return box

    def barrier(self):
        for e in ENGS:
            assert not self.pending[e], f"barrier with unsignalled ops on {e}"
        for e in ENGS:
            waits = {}
            for k in ENGS:
                if k != e and self.count[k] > self.seen[e].get(k, 0):
                    waits[k] = self.count[k]
                    self.seen[e][k] = self.count[k]
            for d in self.dsems:
                if d.count > self.seen[e].get(d.key, 0):
                    waits[d.key] = d.count
                    self.seen[e][d.key] = d.count
            if waits:
                self.ops[e].append(("wait", None, waits, False))

    def emit(self, nc, stack):
        sems = {}
        for e in ENGS:
            sems[e] = stack.enter_context(nc.semaphore("s_" + e))
        for d in self.dsems:
            sems[d.key] = stack.enter_context(nc.semaphore("d_" + d.name))
        for e in ENGS:
            assert self.count[e] < 60000, (e, self.count[e])
            assert not self.pending[e], f"trailing unsignalled ops on {e}"
        block = stack.enter_context(nc.Block())
        hand = {"pe": block.tensor, "act": block.scalar, "dve": block.vector,
                "pool": block.gpsimd, "sp": block.sync}

        def make(e):
            def body(engine):
                for kind, fn, waits, sig in self.ops[e]:
                    for k, v in waits.items():
                        engine.wait_ge(sems[k], v)
                    if kind == "op":
                        name, a, k = fn
                        ins = getattr(engine, name)(*a, **k)
                        if sig:
                            ins.then_inc(sems[e], 1)
                    elif kind == "dma":
                        out, in_, dsem = fn
                        engine.dma_start(out=out, in_=in_).then_inc(sems[dsem.key], 16)
            return body

        for e in ENGS:
            hand[e](make(e))


class Mem:
    def __init__(self, nc, base=16512, top=229376):
        self.nc, self.p, self.top, self.n = nc, base, top, 0
        self.peak = base

    def alloc(self, shape, dt):
        esz = 4 if dt == F32 else 2
        nbytes = esz * int(np.prod(shape[1:]))
        off = self.p
        self.p += (nbytes + 63) // 64 * 64
        assert self.p <= self.top, f"SBUF overflow: {self.p} > {self.top}"
        self.peak = max(self.peak, self.p)
        self.n += 1
        return self.nc.alloc_sbuf_tensor_at(f"sb{self.n}", list(shape), dt, offset=off)

    def alloc_at(self, off, shape, dt):
        self.n += 1
        return self.nc.alloc_sbuf_tensor_at(f"sb{self.n}", list(shape), dt, offset=off)

    def mark(self):
        return self.p

    def release(self, m):
        self.p = m


class _Stop(Exception):
    pass


def build_program(stop=None):
    nc = bass.Bass("TRN2", target_bir_lowering=False)
    dram = lambda name, shape, dt=F32, kind="ExternalInput": nc.dram_tensor(name, shape, dt, kind=kind).ap()
    xin = dram("xin", [2048, D])
    hvin = dram("hv", [128, 1])
    colsin = dram("cols", [128, 160])
    w_in = dram("w_in", [DEPTH, D, 5120])
    relT = dram("relT", [DEPTH, 8, 128, 640])
    sgu_g = dram("sgu_ln_g", [DEPTH, 1024])
    sgu_bb = dram("sgu_ln_b", [DEPTH, 1024])
    sgu_w = dram("sgu_w", [DEPTH, 8, 128, 128])
    sgu_b = dram("sgu_b", [DEPTH, 8, 128])
    w_out = dram("w_out", [DEPTH, D, D])
    router_w = dram("router_w", [D, NE])
    router_b = dram("router_b", [NE])
    w_gate = dram("w_gate", [DEPTH, NE, D, FF])
    w_up = dram("w_up", [DEPTH, NE, D, FF])
    w_down = dram("w_down", [DEPTH, NE, FF, D])
    out = dram("out", [1024, D], kind="ExternalOutput")
    cbsc = dram("cbsc", [128, NCH * 512], BF16, kind="Internal")

    P = Prog()
    M = Mem(nc)
    st = ExitStack()
    dbg = dram("dbg", [128, NCH, 1024], kind="ExternalOutput") if stop else None

    def stage(name, Tq):
        if stop != name:
            return
        P.barrier()
        dd = P.dsem("dbg")
        P.dma("sp", dbg[:, :, 0:Tq], X[:, :, 0:Tq], dd, writes=[Slot("dbgo")])
        raise _Stop()

    X = M.alloc([128, NCH, 1024], F32)
    HB = M.alloc([128, NCH, 1024], BF16)
    ident = M.alloc([128, 128], F32)
    ones_f = M.alloc([128, 128], F32)
    ones_b = M.alloc([128, 128], BF16)
    sel = M.alloc([16, NE, 128], F32)
    cols = M.alloc([128, 160], F32)
    rw = M.alloc([128, NCH, NE], F32)
    rb = M.alloc([128, NE], F32)
    hv = M.alloc([128, 1], F32)
    epsc = M.alloc([128, 1], F32)
    combT = M.alloc([16, 1024], F32)
    small = M.alloc([128, 256], F32)
    LN_OFF = M.mark()
    LN_T = ([M.alloc([128, 512], F32), M.alloc([128, 512], F32)], M.alloc([128, 512], F32),
            M.alloc([128, 512], F32), M.alloc([128, 512], F32),
            [M.alloc([128, 512], F32), M.alloc([128, 512], F32)])
    LN_S = ([Slot("sq0"), Slot("sq1")], Slot("stat"), [Slot("tmp0"), Slot("tmp1")])
    ARENA = M.mark()

    PS = [st.enter_context(nc.psum_tensor(f"ps{i}", [128, 512], F32)) for i in range(8)]
    sPS = [Slot(f"ps{i}") for i in range(8)]

    sX = [[Slot(f"X{c}_{tb}") for tb in range(2)] for c in range(NCH)]
    sHB = [Slot(f"HB{tb}") for tb in range(2)]
    sConst = Slot("const")
    sCombT = Slot("combT")
    sSmall = Slot("small")

    dconst = P.dsem("const")
    dmisc = P.dsem("misc")

    CB_OFF = [None]
    GCOL = lambda k, c: cols[:, k * 16 + c: k * 16 + c + 1]

    P.dma("sp", cols[:], colsin, dconst, writes=[sConst])
    P.dma("sp", hv[:], hvin, dconst, writes=[sConst])
    P.dma("sp", rw[:], router_w.rearrange("(c p) e -> p c e", p=128), dconst, writes=[sConst])
    P.dma("sp", rb[:], router_b.partition_broadcast(128), dconst, writes=[sConst])
    P.op("pool", lambda e: e.memset(ones_f[:], 1.0), writes=[sConst])
    P.op("pool", lambda e: e.memset(ones_b[:], 1.0), writes=[sConst])
    P.op("pool", lambda e: e.memset(epsc[:], EPS), writes=[sConst])
    P.op("pool", lambda e: e.affine_select(out=ident[:], in_=ones_f[:], pattern=[[-1, 128]],
                                           compare_op=ALU.is_equal, fill=0.0, base=0, channel_multiplier=1),
         reads=[sConst], writes=[sConst])
    P.op("pool", lambda e: e.affine_select(out=sel[:], in_=ones_f[0:16, :].unsqueeze(1).to_broadcast([16, NE, 128]),
                                           pattern=[[-1, NE], [0, 128]], compare_op=ALU.is_equal, fill=0.0,
                                           base=0, channel_multiplier=1),
         reads=[sConst], writes=[sConst])
    P.barrier()

    def ln_fm(src, nch, Tq, gk, bk, out_f32, out_bf, s_src, s_outb, psA, psB, tmps):
        sq, mean, rstd, nmr, tmp = tmps
        s_sq, s_stat, s_tmp = LN_S
        inv = 1.0 / (nch * 128)
        for tb in range(Tq // 512):
            blk = slice(tb * 512, tb * 512 + 512)
            for c in range(nch):
                P.op("act", lambda e, c=c: e.activation(out=sq[c % 2][:], in_=src[:, c, blk], func=AF.Square),
                     reads=[s_src[c][tb]], writes=[s_sq[c % 2]])
                P.op("pe", lambda e, c=c: e.matmul(PS[psA][:], lhsT=ones_f[:], rhs=src[:, c, blk],
                                                   start=(c == 0), stop=(c == nch - 1)),
                     reads=[s_src[c][tb]], writes=[sPS[psA]], sig=(c == nch - 1))
                P.op("pe", lambda e, c=c: e.matmul(PS[psB][:], lhsT=ones_f[:], rhs=sq[c % 2][:],
                                                   start=(c == 0), stop=(c == nch - 1)),
                     reads=[s_sq[c % 2]], writes=[sPS[psB]], sig=True)
            P.op("dve", lambda e: e.tensor_scalar(out=mean[:], in0=PS[psA][:], scalar1=inv, scalar2=None, op0=ALU.mult),
                 reads=[sPS[psA]], writes=[s_stat])
            P.op("dve", lambda e: e.tensor_tensor(out=nmr[:], in0=mean[:], in1=mean[:], op=ALU.mult),
                 reads=[s_stat], writes=[s_stat])
            P.op("dve", lambda e: e.scalar_tensor_tensor(out=rstd[:], in0=PS[psB][:], scalar=inv, in1=nmr[:],
                                                         op0=ALU.mult, op1=ALU.subtract),
                 reads=[sPS[psB], s_stat], writes=[s_stat])
            P.op("act", lambda e: e.activation(out=rstd[:], in_=rstd[:], func=AF.Sqrt, bias=epsc[:], scale=1.0),
                 reads=[s_stat], writes=[s_stat])
            P.op("dve", lambda e: e.reciprocal(out=rstd[:], in_=rstd[:]), reads=[s_stat], writes=[s_stat])
            P.op("dve", lambda e: e.scalar_tensor_tensor(out=nmr[:], in0=mean[:], scalar=-1.0, in1=rstd[:],
                                                         op0=ALU.mult, op1=ALU.mult),
                 reads=[s_stat], writes=[s_stat])
            for c in range(nch):
                t = tmp[c % 2]
                P.op("dve", lambda e, c=c, t=t: e.tensor_tensor(out=t[:], in0=src[:, c, blk], in1=rstd[:], op=ALU.mult),
                     reads=[s_src[c][tb], s_stat], writes=[s_tmp[c % 2]])
                P.op("dve", lambda e, t=t: e.tensor_tensor(out=t[:], in0=t[:], in1=nmr[:], op=ALU.add),
                     reads=[s_stat], writes=[s_tmp[c % 2]])
                if out_f32 is not None:
                    P.op("act", lambda e, c=c, t=t: e.activation(out=out_f32[:, c, blk], in_=t[:], func=AF.Identity,
                                                                 scale=GCOL(gk, c), bias=GCOL(bk, c)),
                         reads=[s_tmp[c % 2]], writes=[s_src[c][tb]])
                P.op("act", lambda e, c=c, t=t: e.activation(out=out_bf[:, c, blk], in_=t[:], func=AF.Identity,
                                                             scale=GCOL(gk, c), bias=GCOL(bk, c)),
                     reads=[s_tmp[c % 2]], writes=[s_outb[tb]])

    def ln_tmps():
        return LN_T

    def ln_in(row0, ntile, dest_f32, dest_bf, tile0, s_dst_f, s_dst_b):
        xt = [M.alloc([128, D], F32), M.alloc([128, D], F32)]
        junk = M.alloc([128, D], BF16)
        st_ = M.alloc([128, 8], F32)
        s_xt = [Slot("xt0"), Slot("xt1")]
        s_junk, s_st = Slot("junk"), Slot("st")
        dx = [P.dsem(f"x{row0}_0"), P.dsem(f"x{row0}_1")]
        for i in range(ntile):
            b = i % 2
            x_t = xt[b]
            P.dma("sp", x_t[:], xin[row0 + i * 128: row0 + (i + 1) * 128, :], dx[b], writes=[s_xt[b]])
            P.op("dve", lambda e, x_t=x_t: e.tensor_reduce(out=st_[:, 0:1], in_=x_t[:], axis=AX.X, op=ALU.add),
                 reads=[s_xt[b]], writes=[s_st])
            P.op("act", lambda e, x_t=x_t: e.activation(out=junk[:], in_=x_t[:], func=AF.Square, accum_out=st_[:, 1:2]),
                 reads=[s_xt[b], s_st], writes=[s_junk, s_st])
            P.op("dve", lambda e: e.tensor_scalar(out=st_[:, 2:3], in0=st_[:, 0:1], scalar1=1.0 / D, scalar2=None, op0=ALU.mult),
                 reads=[s_st], writes=[s_st])
            P.op("dve", lambda e: e.tensor_tensor(out=st_[:, 3:4], in0=st_[:, 2:3], in1=st_[:, 2:3], op=ALU.mult),
                 reads=[s_st], writes=[s_st])
            P.op("dve", lambda e: e.scalar_tensor_tensor(out=st_[:, 4:5], in0=st_[:, 1:2], scalar=1.0 / D, in1=st_[:, 3:4],
                                                         op0=ALU.mult, op1=ALU.subtract),
                 reads=[s_st], writes=[s_st])
            P.op("act", lambda e: e.activation(out=st_[:, 5:6], in_=st_[:, 4:5], func=AF.Sqrt, bias=epsc[:], scale=1.0),
                 reads=[s_st], writes=[s_st])
            P.op("dve", lambda e: e.reciprocal(out=st_[:, 6:7], in_=st_[:, 5:6]), reads=[s_st], writes=[s_st])
            P.op("dve", lambda e, x_t=x_t: e.tensor_scalar(out=x_t[:], in0=x_t[:], scalar1=st_[:, 2:3], scalar2=st_[:, 6:7],
                                                           op0=ALU.subtract, op1=ALU.mult),
                 reads=[s_st], writes=[s_xt[b]])
            tcol = slice((tile0 + i) * 128, (tile0 + i + 1) * 128)
            tb = (tile0 + i) // 4
            for c4 in range(4):
                bank = c4 % 2
                for cc in range(4):
                    c = c4 * 4 + cc
                    P.op("pe", lambda e, c=c, cc=cc, bank=bank, x_t=x_t: e.transpose(
                        PS[bank][:, cc * 128:(cc + 1) * 128], x_t[:, c * 128:(c + 1) * 128], ident[:]),
                        reads=[s_xt[b]], writes=[sPS[bank]])
                for cc in range(4):
                    c = c4 * 4 + cc
                    if dest_f32 is not None:
                        P.op("act", lambda e, c=c, cc=cc, bank=bank: e.activation(
                            out=dest_f32[:, c, tcol], in_=PS[bank][:, cc * 128:(cc + 1) * 128], func=AF.Identity,
                            scale=GCOL(0, c), bias=GCOL(1, c)),
                            reads=[sPS[bank]], writes=[s_dst_f[c][tb]])
                    P.op("act", lambda e, c=c, cc=cc, bank=bank: e.activation(
                        out=dest_bf[:, c, tcol], in_=PS[bank][:, cc * 128:(cc + 1) * 128], func=AF.Identity,
                        scale=GCOL(0, c), bias=GCOL(1, c)),
                        reads=[sPS[bank]], writes=[s_dst_b[tb]])

    def mixer(l, Tq, CB, sCB, tag):
        nblk = Tq // 512
        ntq = Tq // 128
        ntk = ntq + 4
        m0 = M.mark()
        ymix = M.alloc([128, 8, Tq], BF16)
        s_ymix = [[Slot(f"ymix{c}_{tb}") for tb in range(nblk)] for c in range(8)]
        wq = [M.alloc([128, NCH, 128], BF16) for _ in range(3)]
        s_wq = [Slot(f"wq{i}") for i in range(3)]
        d_wq = [P.dsem(f"wq{tag}{i}") for i in range(3)]
        ring = [0]

        def load_cols(c0):
            i = ring[0] % 3
            ring[0] += 1
            P.dma("pool", wq[i][:], w_in[l, :, c0:c0 + 128].rearrange("(c p) n -> p c n", p=128), d_wq[i],
                  writes=[s_wq[i]])
            return wq[i], s_wq[i]

        def tok_src(tbk):
            if tbk == 0:
                return (lambda c: CB[:, c, :]), sCB
            return (lambda c, tbk=tbk: HB[:, c, (tbk - 1) * 512: tbk * 512]), sHB[tbk - 1]

        def tok_tile(j):
            if j < 4:
                return (lambda c: CB[:, c, j * 128:(j + 1) * 128]), sCB
            return (lambda c: HB[:, c, (j - 4) * 128:(j - 3) * 128]), sHB[(j - 4) // 4]

        m1 = M.mark()
        E = M.alloc([128, 8, 640], BF16)
        etmp = [M.alloc([128, 640], F32), M.alloc([128, 640], F32)]
        s_E, s_etmp = Slot("E"), [Slot("et0"), Slot("et1")]
        d_et = [P.dsem(f"et{tag}0"), P.dsem(f"et{tag}1")]
        kT = [M.alloc([128, 512 + Tq], BF16) for _ in range(2)]
        qT = [M.alloc([128, Tq], BF16) for _ in range(2)]
        Vh = [M.alloc([128, ntk, 128], BF16) for _ in range(2)]
        Pt = [M.alloc([128, 640], BF16) for _ in range(2)]
        rec = [M.alloc([128, 128], F32) for _ in range(2)]
        s_kT, s_qT, s_Vh = [Slot("kT0"), Slot("kT1")], [Slot("qT0"), Slot("qT1")], [Slot("Vh0"), Slot("Vh1")]
        s_Pt, s_rec = [Slot("Pt0"), Slot("Pt1")], [Slot("rec0"), Slot("rec1")]
        for h in range(8):
            b = h % 2
            P.dma("sp", etmp[b][:], relT[l, h], d_et[b], writes=[s_etmp[b]])
            P.op("act", lambda e, h=h, b=b: e.activation(out=E[:, h, :], in_=etmp[b][:], func=AF.Exp),
                 reads=[s_etmp[b]], writes=[s_E])
            P.op("pool", lambda e, h=h: e.memset(E[0:64, h, 64:128], 0.0), writes=[s_E])
            P.op("pool", lambda e, h=h: e.memset(E[64:128, h, 512:576], 0.0), writes=[s_E])
        scale = 1.0 / float(np.sqrt(128.0))
        pending = [load_cols(0), load_cols(1024), load_cols(2048)]
        for h in range(8):
            b = h % 2
            (wqh, s_wqh), (wkh, s_wkh), (wvh, s_wvh) = pending
            for tbk in range(nblk + 1):
                src, s_src = tok_src(tbk)
                bank = tbk % 2
                for c in range(NCH):
                    P.op("pe", lambda e, c=c, src=src, bank=bank, wkh=wkh: e.matmul(
                        PS[bank][:], lhsT=wkh[:, c, :], rhs=src(c), start=(c == 0), stop=(c == NCH - 1)),
                        reads=[s_wkh, s_src], writes=[sPS[bank]], sig=(c == NCH - 1))
                P.op("act", lambda e, b=b, bank=bank, tbk=tbk: e.copy(out=kT[b][:, tbk * 512:(tbk + 1) * 512], in_=PS[bank][:]),
                     reads=[sPS[bank]], writes=[s_kT[b]])
            for tb in range(nblk):
                src, s_src = tok_src(tb + 1)
                bank = 2 + tb % 2
                for c in range(NCH):
                    P.op("pe", lambda e, c=c, src=src, bank=bank, wqh=wqh: e.matmul(
                        PS[bank][:], lhsT=wqh[:, c, :], rhs=src(c), start=(c == 0), stop=(c == NCH - 1)),
                        reads=[s_wqh, s_src], writes=[sPS[bank]], sig=(c == NCH - 1))
                P.op("act", lambda e, b=b, bank=bank, tb=tb: e.copy(out=qT[b][:, tb * 512:(tb + 1) * 512], in_=PS[bank][:]),
                     reads=[sPS[bank]], writes=[s_qT[b]])
            for j4 in range(ntk // 4):
                bank = 4 + j4 % 2
                for jj in range(4):
                    j = j4 * 4 + jj
                    src, s_src = tok_tile(j)
                    for c in range(NCH):
                        P.op("pe", lambda e, c=c, src=src, bank=bank, jj=jj, wvh=wvh: e.matmul(
                            PS[bank][:, jj * 128:(jj + 1) * 128], lhsT=src(c), rhs=wvh[:, c, :],
                            start=(c == 0), stop=(c == NCH - 1)),
                            reads=[s_wvh, s_src], writes=[sPS[bank]], sig=(c == NCH - 1))
                P.op("dve", lambda e, b=b, bank=bank, j4=j4: e.tensor_copy(
                    out=Vh[b][:, j4 * 4:(j4 + 1) * 4, :], in_=PS[bank][:].rearrange("p (j d) -> p j d", d=128)),
                    reads=[sPS[bank]], writes=[s_Vh[b]])
            if h < 7:
                pending = [load_cols((h + 1) * 128), load_cols(1024 + (h + 1) * 128), load_cols(2048 + (h + 1) * 128)]
            def scores(i):
                pb = i % 2
                sa, sb_ = (6, 7) if i % 2 == 0 else (0, 1)
                for j in range(5):
                    dst = PS[sa][:, j * 128:(j + 1) * 128] if j < 4 else PS[sb_][:, 0:128]
                    P.op("pe", lambda e: e.matmul(
                        dst, lhsT=kT[b][:, (i + j) * 128:(i + j + 1) * 128], rhs=qT[b][:, i * 128:(i + 1) * 128],
                        start=True, stop=True),
                        reads=[s_kT[b], s_qT[b]], writes=[sPS[sa] if j < 4 else sPS[sb_]], sig=(j >= 3))
                P.op("act", lambda e: e.activation(out=Pt[pb][:, 0:512], in_=PS[sa][:], func=AF.Exp, scale=scale),
                     reads=[sPS[sa]], writes=[s_Pt[pb]])
                P.op("act", lambda e: e.activation(out=Pt[pb][:, 512:640], in_=PS[sb_][:, 0:128], func=AF.Exp, scale=scale),
                     reads=[sPS[sb_]], writes=[s_Pt[pb]])
                nctx = max(0, 4 - i)
                if nctx > 0:
                    P.op("dve", lambda e: e.scalar_tensor_tensor(
                        out=Pt[pb][:, 0:nctx * 128], in0=Pt[pb][:, 0:nctx * 128], scalar=hv[:, 0:1],
                        in1=E[:, h, 0:nctx * 128], op0=ALU.mult, op1=ALU.mult),
                        reads=[s_E, sConst], writes=[s_Pt[pb]])
                P.op("pool", lambda e: e.tensor_tensor(
                    out=Pt[pb][:, nctx * 128:640], in0=Pt[pb][:, nctx * 128:640], in1=E[:, h, nctx * 128:640], op=ALU.mult),
                    reads=[s_E], writes=[s_Pt[pb]])

            def pv(i):
                pb = i % 2
                od = 2 + (i % 2)
                for j in range(5):
                    P.op("pe", lambda e: e.matmul(
                        PS[od][:, 0:128], lhsT=Vh[b][:, i + j, :], rhs=Pt[pb][:, j * 128:(j + 1) * 128],
                        start=(j == 0), stop=(j == 4)),
                        reads=[s_Vh[b], s_Pt[pb]], writes=[sPS[od]], sig=False)
                for j in range(5):
                    P.op("pe", lambda e: e.matmul(
                        PS[od][:, 128:256], lhsT=ones_b[:], rhs=Pt[pb][:, j * 128:(j + 1) * 128],
                        start=(j == 0), stop=(j == 4)),
                        reads=[s_Pt[pb], sConst], writes=[sPS[od]], sig=(j == 4))
                P.op("dve", lambda e: e.reciprocal(out=rec[pb][:], in_=PS[od][:, 128:256]),
                     reads=[sPS[od]], writes=[s_rec[pb]])
                P.op("dve", lambda e: e.tensor_tensor(
                    out=ymix[:, h, i * 128:(i + 1) * 128], in0=PS[od][:, 0:128], in1=rec[pb][:], op=ALU.mult),
                    reads=[sPS[od], s_rec[pb]], writes=[s_ymix[h][i // 4]])

            for i in range(ntq + 1):
                if i < ntq:
                    scores(i)
                if i >= 1:
                    pv(i - 1)
        P.barrier()
        M.release(m1)

        def out_proj(row0, first):
            wo = [M.alloc([128, 8, 128], BF16) for _ in range(3)]
            s_wo = [Slot(f"wo{i}") for i in range(3)]
            d_wo = [P.dsem(f"wo{tag}{row0}_{i}") for i in range(3)]

            def load(db):
                i = db % 3
                P.dma("pool", wo[i][:], w_out[l, row0:row0 + 1024, db * 128:(db + 1) * 128].rearrange("(c p) n -> p c n", p=128),
                      d_wo[i], writes=[s_wo[i]])
            load(0); load(1)
            for db in range(NCH):
                if db + 2 < NCH:
                    load(db + 2)
                i = db % 3
                for tb in range(nblk):
                    bank = (db * nblk + tb) % 4
                    blk = slice(tb * 512, tb * 512 + 512)
                    for c in range(8):
                        P.op("pe", lambda e, c=c, i=i, bank=bank, blk=blk: e.matmul(
                            PS[bank][:], lhsT=wo[i][:, c, :], rhs=ymix[:, c, blk], start=(c == 0), stop=(c == 7)),
                            reads=[s_wo[i], s_ymix[c][tb]], writes=[sPS[bank]], sig=(c == 7))
                    if first:
                        P.op("dve", lambda e, db=db, bank=bank, blk=blk: e.scalar_tensor_tensor(
                            out=X[:, db, blk], in0=X[:, db, blk], scalar=ALPHA, in1=PS[bank][:], op0=ALU.mult, op1=ALU.add),
                            reads=[sPS[bank]], writes=[sX[db][tb]])
                    else:
                        P.op("dve", lambda e, db=db, bank=bank, blk=blk: e.tensor_tensor(
                            out=X[:, db, blk], in0=X[:, db, blk], in1=PS[bank][:], op=ALU.add),
                            reads=[sPS[bank]], writes=[sX[db][tb]])

        m2 = M.mark()
        out_proj(0, True)
        P.barrier()
        M.release(m2)

        gbc = M.alloc([128, 1024], F32)
        bbc = M.alloc([128, 1024], F32)
        bsb = M.alloc([128, 8, 128], F32)
        WmT = M.alloc([128, 8, 128], BF16)
        vln = M.alloc_at(CB_OFF[0], [128, ntq, 1024], BF16)
        wvs = M.alloc([128, NCH, 256], BF16)
        vtmp_off = M.mark()
        vtmp = M.alloc([128, 1024], F32)
        wtmp = M.alloc_at(vtmp_off, [128, 8, 128], F32)
        junk = M.alloc([128, 1024], BF16)
        stt = M.alloc([128, ntq, 12], F32)
        ug = [M.alloc_at(LN_OFF + k * 4096, [128, Tq], F32) for k in range(2)]
        mtmp = [M.alloc([128, 512], F32) for _ in range(2)]
        s_g, s_wtmp, s_WmT, s_wvs, s_junk = Slot("g"), Slot("wtmp"), Slot("WmT"), Slot("wvs"), Slot("junk")
        s_vtmp = s_wtmp
        s_vln = [Slot(f"vln{i}") for i in range(ntq)]
        s_stt = [Slot(f"stt{i}") for i in range(ntq)]
        s_ug, s_mtmp = [Slot("ug0"), Slot("ug1")], [Slot("mt0"), Slot("mt1")]
        d_g, d_wvs = P.dsem(f"sg{tag}"), P.dsem(f"wvs{tag}")
        P.dma("sp", gbc[:], sgu_g[l].partition_broadcast(128), d_g, writes=[s_g])
        P.dma("sp", bbc[:], sgu_bb[l].partition_broadcast(128), d_g, writes=[s_g])
        P.dma("sp", bsb[:].rearrange("p g t -> p (g t)"), sgu_b[l].rearrange("g t -> (g t)").partition_broadcast(128), d_g, writes=[s_g])
        P.dma("sp", wtmp[:], sgu_w[l].rearrange("g t s -> t g s"), d_g, writes=[s_wtmp])
        for g in range(8):
            P.op("pool", lambda e, g=g: e.affine_select(out=wtmp[:, g, :], in_=wtmp[:, g, :], pattern=[[-1, 128]],
                                                        compare_op=ALU.is_ge, fill=0.0, base=0, channel_multiplier=1),
                 reads=[s_g], writes=[s_wtmp])
        for g4 in range(2):
            for gg in range(4):
                g = g4 * 4 + gg
                P.op("pe", lambda e, g=g, gg=gg, g4=g4: e.transpose(PS[g4][:, gg * 128:(gg + 1) * 128], wtmp[:, g, :], ident[:]),
                     reads=[s_wtmp, sConst], writes=[sPS[g4]])
            P.op("act", lambda e, g4=g4: e.copy(out=WmT[:, g4 * 4:(g4 + 1) * 4, :], in_=PS[g4][:].rearrange("p (g t) -> p g t", t=128)),
                 reads=[sPS[g4]], writes=[s_WmT])
        for qt in range(4):
            P.dma("pool", wvs[:], w_in[l, :, 4096 + qt * 256: 4096 + (qt + 1) * 256].rearrange("(c p) n -> p c n", p=128),
                  d_wvs, writes=[s_wvs])
            for i in range(ntq):
                bank = 2 + i % 4
                for c in range(NCH):
                    P.op("pe", lambda e, c=c, i=i, bank=bank: e.matmul(
                        PS[bank][:, 0:256], lhsT=HB[:, c, i * 128:(i + 1) * 128], rhs=wvs[:, c, :], start=(c == 0), stop=(c == NCH - 1)),
                        reads=[s_wvs, sHB[i // 4]], writes=[sPS[bank]], sig=(c == NCH - 1))
                P.op("act", lambda e, i=i, bank=bank, qt=qt: e.activation(
                    out=vln[:, i, qt * 256:(qt + 1) * 256], in_=PS[bank][:, 0:256], func=AF.Gelu, accum_out=stt[:, i, qt:qt + 1]),
                    reads=[sPS[bank]], writes=[s_vln[i], s_stt[i]])
        for i in range(ntq):
            S_ = lambda k, i=i: stt[:, i, k:k + 1]
            P.op("act", lambda e, i=i: e.activation(out=junk[:], in_=vln[:, i, :], func=AF.Square, accum_out=stt[:, i, 4:5]),
                 reads=[s_vln[i]], writes=[s_junk, s_stt[i]])
            P.op("dve", lambda e, S_=S_, i=i: e.tensor_reduce(out=S_(5), in_=stt[:, i, 0:4], axis=AX.X, op=ALU.add), reads=[s_stt[i]], writes=[s_stt[i]])
            P.op("dve", lambda e, S_=S_: e.tensor_scalar(out=S_(5), in0=S_(5), scalar1=1.0 / 1024, scalar2=None, op0=ALU.mult),
                 reads=[s_stt[i]], writes=[s_stt[i]])
            P.op("dve", lambda e, S_=S_: e.tensor_tensor(out=S_(6), in0=S_(5), in1=S_(5), op=ALU.mult), reads=[s_stt[i]], writes=[s_stt[i]])
            P.op("dve", lambda e, S_=S_: e.scalar_tensor_tensor(out=S_(7), in0=S_(4), scalar=1.0 / 1024, in1=S_(6),
                                                                op0=ALU.mult, op1=ALU.subtract), reads=[s_stt[i]], writes=[s_stt[i]])
            P.op("act", lambda e, S_=S_: e.activation(out=S_(8), in_=S_(7), func=AF.Sqrt, bias=epsc[:], scale=1.0),
                 reads=[s_stt[i]], writes=[s_stt[i]])
            P.op("dve", lambda e, S_=S_: e.reciprocal(out=S_(9), in_=S_(8)), reads=[s_stt[i]], writes=[s_stt[i]])
            P.op("dve", lambda e, S_=S_, i=i: e.tensor_scalar(out=vtmp[:], in0=vln[:, i, :], scalar1=S_(5), scalar2=S_(9),
                                                              op0=ALU.subtract, op1=ALU.mult),
                 reads=[s_vln[i], s_stt[i]], writes=[s_vtmp])
            P.op("dve", lambda e: e.tensor_tensor(out=vtmp[:], in0=vtmp[:], in1=gbc[:], op=ALU.mult), reads=[s_g], writes=[s_vtmp])
            P.op("dve", lambda e, i=i: e.tensor_tensor(out=vln[:, i, :], in0=vtmp[:], in1=bbc[:], op=ALU.add),
                 reads=[s_g, s_vtmp], writes=[s_vln[i]])
        pend = load_cols(3072)
        for g in range(8):
            wug, s_wug = pend
            if g < 7:
                pend = load_cols(3072 + (g + 1) * 128)
            ub = g % 2
            for tb in range(nblk):
                bank = tb % 2
                blk = slice(tb * 512, tb * 512 + 512)
                for c in range(NCH):
                    P.op("pe", lambda e, c=c, bank=bank, blk=blk, wug=wug: e.matmul(
                        PS[bank][:], lhsT=wug[:, c, :], rhs=HB[:, c, blk], start=(c == 0), stop=(c == NCH - 1)),
                        reads=[s_wug, sHB[tb]], writes=[sPS[bank]], sig=(c == NCH - 1))
                P.op("act", lambda e, ub=ub, bank=bank, blk=blk: e.activation(out=ug[ub][:, blk], in_=PS[bank][:], func=AF.Gelu),
                     reads=[sPS[bank]], writes=[s_ug[ub]])
            for tb in range(nblk):
                bank = 6 + tb % 2
                blk = slice(tb * 512, tb * 512 + 512)
                for ii in range(4):
                    i = tb * 4 + ii
                    P.op("pe", lambda e, g=g, i=i, ii=ii, bank=bank: e.matmul(
                        PS[bank][:, ii * 128:(ii + 1) * 128], lhsT=vln[:, i, g * 128:(g + 1) * 128], rhs=WmT[:, g, :],
                        start=True, stop=True),
                        reads=[s_vln[i], s_WmT], writes=[sPS[bank]], sig=(ii == 3))
                mt = mtmp[tb % 2]
                P.op("dve", lambda e, g=g, bank=bank, mt=mt: e.tensor_tensor(
                    out=mt[:].rearrange("p (r t) -> p r t", t=128), in0=PS[bank][:].rearrange("p (r t) -> p r t", t=128),
                    in1=bsb[:, g, :].unsqueeze(1).to_broadcast([128, 4, 128]), op=ALU.add),
                    reads=[sPS[bank], s_g], writes=[s_mtmp[tb % 2]])
                P.op("dve", lambda e, g=g, ub=ub, blk=blk, mt=mt: e.tensor_tensor(
                    out=ymix[:, g, blk], in0=mt[:], in1=ug[ub][:, blk], op=ALU.mult),
                    reads=[s_mtmp[tb % 2], s_ug[ub]], writes=[s_ymix[g][tb]])
        P.barrier()
        M.release(m2)
        out_proj(1024, False)
        P.barrier()
        M.release(m0)

    def router(Tq):
        ntq = Tq // 128
        sm = lambda a, b: small[:, a:b]
        for i in range(ntq):
            tcol = slice(i * 128, (i + 1) * 128)
            tb = i // 4
            for c in range(NCH):
                P.op("pe", lambda e, c=c, tcol=tcol: e.matmul(PS[0][:, 0:NE], lhsT=X[:, c, tcol], rhs=rw[:, c, :],
                                                             start=(c == 0), stop=(c == NCH - 1)),
                     reads=[sX[c][tb], sConst], writes=[sPS[0]], sig=(c == NCH - 1))
            R, W = [sSmall], [sSmall]
            lg, ex, pr, gm, m, eq, m2, sl, cb = (sm(0, 16), sm(16, 32), sm(32, 48), sm(48, 52), sm(64, 80), sm(80, 96),
                                                sm(96, 112), sm(112, 128), sm(128, 144))
            pairs, gs = sm(160, 184), sm(184, 188)
            c1 = lambda k: small[:, 200 + k: 201 + k]
            P.op("dve", lambda e: e.tensor_tensor(out=lg, in0=PS[0][:, 0:NE], in1=rb[:], op=ALU.add), reads=[sPS[0], sConst], writes=W)
            P.op("dve", lambda e: e.tensor_reduce(out=c1(0), in_=lg, axis=AX.X, op=ALU.max), reads=R, writes=W)
            P.op("dve", lambda e: e.tensor_scalar(out=c1(1), in0=c1(0), scalar1=-1.0, scalar2=None, op0=ALU.mult), reads=R, writes=W)
            P.op("act", lambda e: e.activation(out=ex, in_=lg, func=AF.Exp, bias=c1(1), scale=1.0, accum_out=c1(2)), reads=R, writes=W)
            P.op("dve", lambda e: e.reciprocal(out=c1(3), in_=c1(2)), reads=R, writes=W)
            P.op("dve", lambda e: e.tensor_scalar(out=pr, in0=ex, scalar1=c1(3), scalar2=None, op0=ALU.mult), reads=R, writes=W)
            pv = pr.rearrange("p (g k) -> p g k", k=4)
            pw = pairs.rearrange("p (g k) -> p g k", k=6)
            for idx, (a, b_) in enumerate([(0, 1), (0, 2), (0, 3), (1, 2), (1, 3), (2, 3)]):
                P.op("dve", lambda e, idx=idx, a=a, b_=b_: e.tensor_tensor(out=pw[:, :, idx:idx + 1], in0=pv[:, :, a:a + 1],
                                                                             in1=pv[:, :, b_:b_ + 1], op=ALU.add), reads=R, writes=W)
            P.op("dve", lambda e: e.tensor_reduce(out=gs, in_=pw, axis=AX.X, op=ALU.max), reads=R, writes=W)
            P.op("dve", lambda e: e.tensor_reduce(out=c1(4), in_=gs, axis=AX.X, op=ALU.max), reads=R, writes=W)
            P.op("dve", lambda e: e.tensor_scalar(out=gm, in0=gs, scalar1=c1(4), scalar2=None, op0=ALU.is_ge), reads=R, writes=W)
            mv = m.rearrange("p (g k) -> p g k", k=4)
            P.op("dve", lambda e: e.tensor_tensor(out=mv, in0=pv, in1=gm.unsqueeze(2).to_broadcast([128, 4, 4]), op=ALU.mult), reads=R, writes=W)
            P.op("dve", lambda e: e.tensor_scalar(out=gs, in0=gm, scalar1=-1.0, scalar2=None, op0=ALU.add), reads=R, writes=W)
            P.op("dve", lambda e: e.tensor_tensor(out=mv, in0=mv, in1=gs.unsqueeze(2).to_broadcast([128, 4, 4]), op=ALU.add), reads=R, writes=W)
            P.op("dve", lambda e: e.tensor_reduce(out=c1(5), in_=m, axis=AX.X, op=ALU.max), reads=R, writes=W)
            P.op("dve", lambda e: e.tensor_scalar(out=eq, in0=m, scalar1=c1(5), scalar2=None, op0=ALU.is_equal), reads=R, writes=W)
            P.op("dve", lambda e: e.scalar_tensor_tensor(out=m2, in0=eq, scalar=-2.0, in1=m, op0=ALU.mult, op1=ALU.add), reads=R, writes=W)
            P.op("dve", lambda e: e.tensor_reduce(out=c1(6), in_=m2, axis=AX.X, op=ALU.max), reads=R, writes=W)
            P.op("dve", lambda e: e.tensor_scalar(out=sl, in0=m, scalar1=c1(6), scalar2=None, op0=ALU.is_ge), reads=R, writes=W)
            P.op("dve", lambda e: e.tensor_tensor(out=c1(7), in0=c1(5), in1=c1(6), op=ALU.add), reads=R, writes=W)
            P.op("dve", lambda e: e.reciprocal(out=c1(8), in_=c1(7)), reads=R, writes=W)
            P.op("dve", lambda e: e.scalar_tensor_tensor(out=cb, in0=m, scalar=c1(8), in1=sl, op0=ALU.mult, op1=ALU.mult), reads=R, writes=W)
            P.op("pe", lambda e: e.transpose(PS[1][0:NE, 0:128], cb, ident[:]), reads=R + [sConst], writes=[sPS[1]])
            P.op("act", lambda e, tcol=tcol: e.copy(out=combT[:, tcol], in_=PS[1][0:NE, 0:128]), reads=[sPS[1]], writes=[sCombT])

    def moe(l, Tq, tag):
        nblk = Tq // 512
        m0 = M.mark()
        hid = M.alloc_at(CB_OFF[0], [128, 8, 1024], BF16)
        s_hid = [Slot(f"hid{tb}") for tb in range(nblk)]
        wg = [M.alloc([128, NCH, 256], BF16) for _ in range(2)]
        wu = [M.alloc([128, NCH, 256], BF16) for _ in range(2)]
        wd = [M.alloc([128, 8, 512], BF16) for _ in range(2)]
        s_wg, s_wu, s_wd = [Slot("wg0"), Slot("wg1")], [Slot("wu0"), Slot("wu1")], [Slot("wd0"), Slot("wd1")]
        d_wg = [P.dsem(f"wg{tag}{i}") for i in range(2)]
        d_wu = [P.dsem(f"wu{tag}{i}") for i in range(2)]
        d_wd = [P.dsem(f"wd{tag}{i}") for i in range(2)]
        ssb = [M.alloc([128, 512], F32) for _ in range(2)]
        tsb = [M.alloc([128, 512], F32) for _ in range(2)]
        s_ssb, s_tsb = [Slot("ssb0"), Slot("ssb1")], [Slot("tsb0"), Slot("tsb1")]

        def load_gu(n):
            e_, fb = divmod(n, 4)
            i = n % 2
            P.dma("pool", wg[i][:], w_gate[l, e_, :, fb * 256:(fb + 1) * 256].rearrange("(c p) n -> p c n", p=128), d_wg[i], writes=[s_wg[i]])
            P.dma("pool", wu[i][:], w_up[l, e_, :, fb * 256:(fb + 1) * 256].rearrange("(c p) n -> p c n", p=128), d_wu[i], writes=[s_wu[i]])

        def load_d(n):
            e_, dq = divmod(n, 4)
            i = n % 2
            P.dma("pool", wd[i][:], w_down[l, e_, :, dq * 512:(dq + 1) * 512].rearrange("(c p) n -> p c n", p=128), d_wd[i], writes=[s_wd[i]])

        load_gu(0); load_gu(1); load_d(0); load_d(1)
        yield
        cnt = 0
        for e_ in range(NE):
            for tb in range(nblk):
                P.op("pe", lambda e, e_=e_, tb=tb: e.matmul(PS[4 + tb][:], lhsT=sel[:, e_, :], rhs=combT[:, tb * 512:(tb + 1) * 512],
                                                           start=True, stop=True),
                     reads=[sCombT, sConst], writes=[sPS[4 + tb]])
            for fb in range(4):
                n = e_ * 4 + fb
                i = n % 2
                for tb in range(nblk):
                    blk = slice(tb * 512, tb * 512 + 512)
                    for fs in range(2):
                        f128 = fb * 2 + fs
                        gb, ub = cnt % 2, 2 + cnt % 2
                        cnt += 1
                        for c in range(NCH):
                            P.op("pe", lambda e, c=c, i=i, fs=fs, gb=gb, blk=blk: e.matmul(
                                PS[gb][:], lhsT=wg[i][:, c, fs * 128:(fs + 1) * 128], rhs=HB[:, c, blk], start=(c == 0), stop=(c == NCH - 1)),
                                reads=[s_wg[i], sHB[tb]], writes=[sPS[gb]], sig=(c == NCH - 1))
                        for c in range(NCH):
                            P.op("pe", lambda e, c=c, i=i, fs=fs, ub=ub, blk=blk: e.matmul(
                                PS[ub][:], lhsT=wu[i][:, c, fs * 128:(fs + 1) * 128], rhs=HB[:, c, blk], start=(c == 0), stop=(c == NCH - 1)),
                                reads=[s_wu[i], sHB[tb]], writes=[sPS[ub]], sig=(c == NCH - 1))
                        k = cnt % 2
                        P.op("act", lambda e, k=k, gb=gb: e.activation(out=ssb[k][:], in_=PS[gb][:], func=AF.Silu),
                             reads=[sPS[gb]], writes=[s_ssb[k]])
                        P.op("dve", lambda e, k=k, ub=ub: e.tensor_tensor(out=tsb[k][:], in0=ssb[k][:], in1=PS[ub][:], op=ALU.mult),
                             reads=[s_ssb[k], sPS[ub]], writes=[s_tsb[k]])
                        P.op("dve", lambda e, k=k, tb=tb, f128=f128, blk=blk: e.tensor_tensor(
                            out=hid[:, f128, blk], in0=tsb[k][:], in1=PS[4 + tb][:], op=ALU.mult),
                            reads=[s_tsb[k], sPS[4 + tb]], writes=[s_hid[tb]])
                if n + 2 < NE * 4:
                    load_gu(n + 2)
            for dq in range(4):
                n = e_ * 4 + dq
                i = n % 2
                for tb in range(nblk):
                    blk = slice(tb * 512, tb * 512 + 512)
                    for ds in range(4):
                        db = dq * 4 + ds
                        bank = 6 + cnt % 2
                        cnt += 1
                        for c in range(8):
                            P.op("pe", lambda e, c=c, i=i, ds=ds, bank=bank, blk=blk: e.matmul(
                                PS[bank][:], lhsT=wd[i][:, c, ds * 128:(ds + 1) * 128], rhs=hid[:, c, blk], start=(c == 0), stop=(c == 7)),
                                reads=[s_wd[i], s_hid[tb]], writes=[sPS[bank]], sig=(c == 7))
                        if e_ == 0:
                            P.op("dve", lambda e, db=db, bank=bank, blk=blk: e.scalar_tensor_tensor(
                                out=X[:, db, blk], in0=X[:, db, blk], scalar=ALPHA, in1=PS[bank][:], op0=ALU.mult, op1=ALU.add),
                                reads=[sPS[bank]], writes=[sX[db][tb]])
                        else:
                            P.op("dve", lambda e, db=db, bank=bank, blk=blk: e.tensor_tensor(
                                out=X[:, db, blk], in0=X[:, db, blk], in1=PS[bank][:], op=ALU.add),
                                reads=[sPS[bank]], writes=[sX[db][tb]])
                if n + 2 < NE * 4:
                    load_d(n + 2)
        M.release(m0)

    def layer(l, Tq, CB, sCB, tag):
        stage(tag + "_in", Tq)
        mixer(l, Tq, CB, sCB, tag)
        stage(tag + "_mix", Tq)
        g = moe(l, Tq, tag)
        next(g)
        ln_fm(X, NCH, Tq, 2 + 4 * l, 3 + 4 * l, X, HB, sX, sHB, 6, 7, ln_tmps())
        stage(tag + "_ln1", Tq)
        router(Tq)
        next(g, None)
        stage(tag + "_moe", Tq)
        ln_fm(X, NCH, Tq, 4 + 4 * l, 5 + 4 * l, X, HB, sX, sHB, 6, 7, ln_tmps())
        P.barrier()
        stage(tag + "_ln2", Tq)

    CB_OFF[0] = M.mark()
    CB = M.alloc([128, NCH, 512], BF16)
    sCB = Slot("CB")
    s_cbsc = Slot("cbsc")
    ARENA2 = M.mark()
    d_cb = P.dsem("cbsc")

    def _main_seq():
        ln_in(0, 4, None, CB, 0, None, [sCB] * 2)
        P.barrier(); M.release(ARENA2)
        ln_in(512, 4, X, HB, 0, sX, sHB)
        P.barrier(); M.release(ARENA2)
        layer(0, 512, CB, sCB, "H")
        P.dma("sp", cbsc.rearrange("p (c t) -> p c t", c=NCH), HB[:, :, 0:512], d_cb, reads=[sHB[0]], writes=[s_cbsc])
        ln_in(512, 4, None, CB, 0, None, [sCB] * 2)
        P.barrier(); M.release(ARENA2)
        ln_in(1024, 8, X, HB, 0, sX, sHB)
        P.barrier(); M.release(ARENA2)
        layer(0, 1024, CB, sCB, "O")
        P.dma("sp", CB[:], cbsc.rearrange("p (c t) -> p c t", c=NCH), d_cb, reads=[s_cbsc], writes=[sCB])
        layer(1, 1024, CB, sCB, "L")

        ot = [M.alloc([128, D], F32), M.alloc([128, D], F32)]
        s_ot = [Slot("ot0"), Slot("ot1")]
        d_out = [P.dsem("out0"), P.dsem("out1")]
        for i in range(8):
            b = i % 2
            for c4 in range(4):
                bank = c4 % 4
                for cc in range(4):
                    c = c4 * 4 + cc
                    P.op("pe", lambda e, c=c, cc=cc, bank=bank, i=i: e.transpose(
                        PS[bank][:, cc * 128:(cc + 1) * 128], X[:, c, i * 128:(i + 1) * 128], ident[:]),
                        reads=[sX[c][i // 4], sConst], writes=[sPS[bank]])
                if c4 % 2:
                    P.op("act", lambda e, b=b, c4=c4, bank=bank: e.copy(out=ot[b][:, c4 * 512:(c4 + 1) * 512], in_=PS[bank][:]),
                         reads=[sPS[bank]], writes=[s_ot[b]])
                else:
                    P.op("dve", lambda e, b=b, c4=c4, bank=bank: e.tensor_copy(out=ot[b][:, c4 * 512:(c4 + 1) * 512], in_=PS[bank][:]),
                         reads=[sPS[bank]], writes=[s_ot[b]])
            P.dma("sp", out[i * 128:(i + 1) * 128, :], ot[b][:], d_out[b], reads=[s_ot[b]], writes=[Slot("o")])

    try:
        _main_seq()
    except _Stop:
        pass
    P.barrier()
    P.emit(nc, st)
    st.close()
    return nc, M.peak


_CACHE = {}


def _rel_index():
    k = np.arange(128)[:, None, None]
    j = np.arange(5)[None, :, None]
    q = np.arange(128)[None, None, :]
    rel = (4 - j) * 128 + q - k
    return (np.clip(rel, -128, 128) + 128).reshape(128, 640)


def kernel(x, ln_in_g, ln_in_b, w_in, rel_bias, sgu_ln_g, sgu_ln_b, sgu_w, sgu_b, w_out, ln1_g, ln1_b,
           router_w, router_b, w_gate, w_up, w_down, ln2_g, ln2_b):
    f = lambda a: np.ascontiguousarray(np.asarray(a, dtype=np.float32))
    x = f(x)
    if "nc" not in _CACHE:
        _CACHE["nc"], _CACHE["peak"] = build_program()
    nc = _CACHE["nc"]
    vecs = [ln_in_g, ln_in_b, ln1_g[0], ln1_b[0], ln2_g[0], ln2_b[0], ln1_g[1], ln1_b[1], ln2_g[1], ln2_b[1]]
    cols = np.concatenate([f(v).reshape(NCH, 128).T for v in vecs], axis=1)
    relT = f(np.asarray(rel_bias)[:, :, _rel_index()])
    shared = {"cols": f(cols), "w_in": f(w_in), "relT": relT, "sgu_ln_g": f(sgu_ln_g), "sgu_ln_b": f(sgu_ln_b),
              "sgu_w": f(sgu_w), "sgu_b": f(sgu_b), "w_out": f(w_out), "router_w": f(router_w), "router_b": f(router_b),
              "w_gate": f(w_gate), "w_up": f(w_up), "w_down": f(w_down)}
    in_maps = []
    for core in range(N_CORES):
        b, half = divmod(core, 2)
        if half == 0:
            xin = np.concatenate([np.zeros((1024, D), np.float32), x[b, 0:1024]], axis=0)
        else:
            xin = x[b]
        hvv = np.full((128, 1), float(half), np.float32)
        in_maps.append({"xin": np.ascontiguousarray(xin), "hv": hvv, **shared})
    res = run_bass_kernel_spmd(nc, in_maps, core_ids=list(range(N_CORES)))
    outp = np.empty((4, 2048, D), np.float32)
    for core in range(N_CORES):
        b, half = divmod(core, 2)
        outp[b, half * 1024:(half + 1) * 1024] = res.results[core]["out"]
    return outp
```

```python
from contextlib import ExitStack

import numpy as np
import concourse.bass as bass
import concourse.mybir as mybir
from concourse.bass_utils import run_bass_kernel_spmd

F32 = mybir.dt.float32
BF16 = mybir.dt.bfloat16
AF = mybir.ActivationFunctionType
ALU = mybir.AluOpType
AX = mybir.AxisListType

ENGS = ("pe", "act", "dve", "pool", "sp")

D = 2048
NCH = 16
DEPTH = 2
ALPHA = (2.0 * DEPTH) ** 0.25
EPS = 1e-5
NE = 16
FF = 1024
N_CORES = 8


class Box:
    __slots__ = ("key", "val")

    def __init__(self, key, val=None):
        self.key = key
        self.val = val


class Slot:
    __slots__ = ("name", "w", "r")

    def __init__(self, name):
        self.name = name
        self.w = None
        self.r = {}


class DSem:
    def __init__(self, prog, name):
        self.name = f"{name}_{len(prog.dsems)}"
        self.count = 0
        self.key = "d:" + self.name
        prog.dsems.append(self)


class _Rec:
    def __init__(self):
        self.call = None

    def __getattr__(self, name):
        def f(*a, **k):
            self.call = (name, a, k)
        return f


class Prog:
    def __init__(self):
        self.ops = {e: [] for e in ENGS}
        self.count = {e: 0 for e in ENGS}
        self.pending = {e: [] for e in ENGS}
        self.seen = {e: {} for e in ENGS}
        self.dsems = []
        self.same_engine_sync = True

    def dsem(self, name):
        return DSem(self, name)

    def _need(self, eng, box, waits):
        if box is None:
            return
        if box.key == eng and (eng == "pe" or not self.same_engine_sync):
            return
        assert box.val is not None, f"dependency on unsignalled op ({box.key})"
        k, v = box.key, box.val
        if self.seen[eng].get(k, 0) >= v:
            return
        self.seen[eng][k] = v
        waits[k] = max(waits.get(k, 0), v)

    def _deps(self, eng, reads, writes):
        waits = {}
        for s in reads:
            self._need(eng, s.w, waits)
        for s in writes:
            self._need(eng, s.w, waits)
            for b in s.r.values():
                self._need(eng, b, waits)
        return waits

    def _mark(self, box, reads, writes):
        for s in reads:
            s.r[box.key] = box
        for s in writes:
            s.w = box
            s.r = {}

    def op(self, eng, fn, reads=(), writes=(), sig=True):
        waits = self._deps(eng, reads, writes)
        box = Box(eng)
        if sig:
            self.count[eng] += 1
            box.val = self.count[eng]
            for b in self.pending[eng]:
                b.val = box.val
            self.pending[eng] = []
        else:
            self.pending[eng].append(box)
        self._mark(box, reads, writes)
        rec = _Rec()
        fn(rec)
        assert rec.call is not None
        self.ops[eng].append(("op", rec.call, waits, sig))
        return box

    def dma(self, eng, out, in_, dsem, reads=(), writes=()):
        waits = self._deps(eng, reads, writes)
        dsem.count += 16
        box = Box(dsem.key, dsem.count)
        self._mark(box, reads, writes)
        self.ops[eng].append(("dma", (out, in_, dsem), waits, True))
        return box

    def barrier(self):
        for e in ENGS:
            assert not self.pending[e], f"barrier with unsignalled ops on {e}"
        for e in ENGS:
            waits = {}
            for k in ENGS:
                if k != e and self.count[k] > self.seen[e].get(k, 0):
                    waits[k] = self.count[k]
                    self.seen[e][k] = self.count[k]
            for d in self.dsems:
                if d.count > self.seen[e].get(d.key, 0):
                    waits[d.key] = d.count
                    self.seen[e][d.key] = d.count
            if waits:
                self.ops[e].append(("wait", None, waits, False))

    def emit(self, nc, stack):
        sems = {}
        for e in ENGS:
            sems[e] = stack.enter_context(nc.semaphore("s_" + e))
        for d in self.dsems:
            sems[d.key] = stack.enter_context(nc.semaphore("d_" + d.name))
        for e in ENGS:
            assert self.count[e] < 60000, (e, self.count[e])
            assert not self.pending[e], f"trailing unsignalled ops on {e}"
        block = stack.enter_context(nc.Block())
        hand = {"pe": block.tensor, "act": block.scalar, "dve": block.vector,
                "pool": block.gpsimd, "sp": block.sync}

        def make(e):
            def body(engine):
                for kind, fn, waits, sig in self.ops[e]:
                    for k, v in waits.items():
                        engine.wait_ge(sems[k], v)
                    if kind == "op":
                        name, a, k = fn
                        ins = getattr(engine, name)(*a, **k)
                        if sig:
                            ins.then_inc(sems[e], 1)
                    elif kind == "dma":
                        out, in_, dsem = fn
                        engine.dma_start(out=out, in_=in_).then_inc(sems[dsem.key], 16)
            return body

        for e in ENGS:
            hand[e](make(e))


class Mem:
    def __init__(self, nc, base=16512, top=229376):
        self.nc, self.p, self.top, self.n = nc, base, top, 0
        self.peak = base

    def alloc(self, shape, dt):
        esz = 4 if dt == F32 else 2
        nbytes = esz * int(np.prod(shape[1:]))
        off = self.p
        self.p += (nbytes + 63) // 64 * 64
        assert self.p <= self.top, f"SBUF overflow: {self.p} > {self.top}"
        self.peak = max(self.peak, self.p)
        self.n += 1
        return self.nc.alloc_sbuf_tensor_at(f"sb{self.n}", list(shape), dt, offset=off)

    def alloc_at(self, off, shape, dt):
        self.n += 1
        return self.nc.alloc_sbuf_tensor_at(f"sb{self.n}", list(shape), dt, offset=off)

    def mark(self):
        return self.p

    def release(self, m):
        self.p = m


class _Stop(Exception):
    pass


def build_program(stop=None):
    nc = bass.Bass("TRN2", target_bir_lowering=False)
    dram = lambda name, shape, dt=F32, kind="ExternalInput": nc.dram_tensor(name, shape, dt, kind=kind).ap()
    xin = dram("xin", [2048, D])
    hvin = dram("hv", [128, 1])
    colsin = dram("cols", [128, 160])
    w_in = dram("w_in", [DEPTH, D, 5120])
    relT = dram("relT", [DEPTH, 8, 128, 640])
    sgu_g = dram("sgu_ln_g", [DEPTH, 1024])
    sgu_bb = dram("sgu_ln_b", [DEPTH, 1024])
    sgu_w = dram("sgu_w", [DEPTH, 8, 128, 128])
    sgu_b = dram("sgu_b", [DEPTH, 8, 128])
    w_out = dram("w_out", [DEPTH, D, D])
    router_w = dram("router_w", [D, NE])
    router_b = dram("router_b", [NE])
    w_gate = dram("w_gate", [DEPTH, NE, D, FF])
    w_up = dram("w_up", [DEPTH, NE, D, FF])
    w_down = dram("w_down", [DEPTH, NE, FF, D])
    out = dram("out", [1024, D], kind="ExternalOutput")
    cbsc = dram("cbsc", [128, NCH * 512], BF16, kind="Internal")

    P = Prog()
    M = Mem(nc)
    st = ExitStack()
    dbg = dram("dbg", [128, NCH, 1024], kind="ExternalOutput") if stop else None

    def stage(name, Tq):
        if stop != name:
            return
        P.barrier()
        dd = P.dsem("dbg")
        P.dma("sp", dbg[:, :, 0:Tq], X[:, :, 0:Tq], dd, writes=[Slot("dbgo")])
        raise _Stop()

    X = M.alloc([128, NCH, 1024], F32)
    HB = M.alloc([128, NCH, 1024], BF16)
    ident = M.alloc([128, 128], F32)
    ones_f = M.alloc([128, 128], F32)
    ones_b = M.alloc([128, 128], BF16)
    sel = M.alloc([16, NE, 128], F32)
    cols = M.alloc([128, 160], F32)
    rw = M.alloc([128, NCH, NE], F32)
    rb = M.alloc([128, NE], F32)
    hv = M.alloc([128, 1], F32)
    epsc = M.alloc([128, 1], F32)
    combT = M.alloc([16, 1024], F32)
    small = M.alloc([128, 256], F32)
    LN_OFF = M.mark()
    LN_T = ([M.alloc([128, 512], F32), M.alloc([128, 512], F32)], M.alloc([128, 512], F32),
            M.alloc([128, 512], F32), M.alloc([128, 512], F32),
            [M.alloc([128, 512], F32), M.alloc([128, 512], F32)])
    LN_S = ([Slot("sq0"), Slot("sq1")], Slot("stat"), [Slot("tmp0"), Slot("tmp1")])
    ARENA = M.mark()

    PS = [st.enter_context(nc.psum_tensor(f"ps{i}", [128, 512], F32)) for i in range(8)]
    sPS = [Slot(f"ps{i}") for i in range(8)]

    sX = [[Slot(f"X{c}_{tb}") for tb in range(2)] for c in range(NCH)]
    sHB = [Slot(f"HB{tb}") for tb in range(2)]
    sConst = Slot("const")
    sCombT = Slot("combT")
    sSmall = Slot("small")

    dconst = P.dsem("const")
    dmisc = P.dsem("misc")

    CB_OFF = [None]
    GCOL = lambda k, c: cols[:, k * 16 + c: k * 16 + c + 1]

    P.dma("sp", cols[:], colsin, dconst, writes=[sConst])
    P.dma("sp", hv[:], hvin, dconst, writes=[sConst])
    P.dma("sp", rw[:], router_w.rearrange("(c p) e -> p c e", p=128), dconst, writes=[sConst])
    P.dma("sp", rb[:], router_b.partition_broadcast(128), dconst, writes=[sConst])
    P.op("pool", lambda e: e.memset(ones_f[:], 1.0), writes=[sConst])
    P.op("pool", lambda e: e.memset(ones_b[:], 1.0), writes=[sConst])
    P.op("pool", lambda e: e.memset(epsc[:], EPS), writes=[sConst])
    P.op("pool", lambda e: e.affine_select(out=ident[:], in_=ones_f[:], pattern=[[-1, 128]],
                                           compare_op=ALU.is_equal, fill=0.0, base=0, channel_multiplier=1),
         reads=[sConst], writes=[sConst])
    P.op("pool", lambda e: e.affine_select(out=sel[:], in_=ones_f[0:16, :].unsqueeze(1).to_broadcast([16, NE, 128]),
                                           pattern=[[-1, NE], [0, 128]], compare_op=ALU.is_equal, fill=0.0,
                                           base=0, channel_multiplier=1),
         reads=[sConst], writes=[sConst])
    P.barrier()

    def ln_fm(src, nch, Tq, gk, bk, out_f32, out_bf, s_src, s_outb, psA, psB, tmps):
        sq, mean, rstd, nmr, tmp = tmps
        s_sq, s_stat, s_tmp = LN_S
        inv = 1.0 / (nch * 128)
        for tb in range(Tq // 512):
            blk = slice(tb * 512, tb * 512 + 512)
            for c in range(nch):
                P.op("act", lambda e, c=c: e.activation(out=sq[c % 2][:], in_=src[:, c, blk], func=AF.Square),
                     reads=[s_src[c][tb]], writes=[s_sq[c % 2]])
                P.op("pe", lambda e, c=c: e.matmul(PS[psA][:], lhsT=ones_f[:], rhs=src[:, c, blk],
                                                   start=(c == 0), stop=(c == nch - 1)),
                     reads=[s_src[c][tb]], writes=[sPS[psA]], sig=(c == nch - 1))
                P.op("pe", lambda e, c=c: e.matmul(PS[psB][:], lhsT=ones_f[:], rhs=sq[c % 2][:],
                                                   start=(c == 0), stop=(c == nch - 1)),
                     reads=[s_sq[c % 2]], writes=[sPS[psB]], sig=True)
            P.op("dve", lambda e: e.tensor_scalar(out=mean[:], in0=PS[psA][:], scalar1=inv, scalar2=None, op0=ALU.mult),
                 reads=[sPS[psA]], writes=[s_stat])
            P.op("dve", lambda e: e.tensor_tensor(out=nmr[:], in0=mean[:], in1=mean[:], op=ALU.mult),
                 reads=[s_stat], writes=[s_stat])
            P.op("dve", lambda e: e.scalar_tensor_tensor(out=rstd[:], in0=PS[psB][:], scalar=inv, in1=nmr[:],
                                                         op0=ALU.mult, op1=ALU.subtract),
                 reads=[sPS[psB], s_stat], writes=[s_stat])
            P.op("act", lambda e: e.activation(out=rstd[:], in_=rstd[:], func=AF.Sqrt, bias=epsc[:], scale=1.0),
                 reads=[s_stat], writes=[s_stat])
            P.op("dve", lambda e: e.reciprocal(out=rstd[:], in_=rstd[:]), reads=[s_stat], writes=[s_stat])
            P.op("dve", lambda e: e.scalar_tensor_tensor(out=nmr[:], in0=mean[:], scalar=-1.0, in1=rstd[:],
                                                         op0=ALU.mult, op1=ALU.mult),
                 reads=[s_stat], writes=[s_stat])
            for c in range(nch):
                t = tmp[c % 2]
                P.op("dve", lambda e, c=c, t=t: e.tensor_tensor(out=t[:], in0=src[:, c, blk], in1=rstd[:], op=ALU.mult),
                     reads=[s_src[c][tb], s_stat], writes=[s_tmp[c % 2]])
                P.op("pool", lambda e, t=t: e.tensor_tensor(out=t[:], in0=t[:], in1=nmr[:], op=ALU.add),
                     reads=[s_stat], writes=[s_tmp[c % 2]])
                if out_f32 is not None:
                    P.op("act", lambda e, c=c, t=t: e.activation(out=out_f32[:, c, blk], in_=t[:], func=AF.Identity,
                                                                 scale=GCOL(gk, c), bias=GCOL(bk, c)),
                         reads=[s_tmp[c % 2]], writes=[s_src[c][tb]])
                P.op("act", lambda e, c=c, t=t: e.activation(out=out_bf[:, c, blk], in_=t[:], func=AF.Identity,
                                                             scale=GCOL(gk, c), bias=GCOL(bk, c)),
                     reads=[s_tmp[c % 2]], writes=[s_outb[tb]])

    def ln_tmps():
        return LN_T

    def ln_in(row0, ntile, dest_f32, dest_bf, tile0, s_dst_f, s_dst_b):
        xt = [M.alloc([128, D], F32), M.alloc([128, D], F32)]
        junk = M.alloc([128, D], BF16)
        st_ = M.alloc([128, 8], F32)
        s_xt = [Slot("xt0"), Slot("xt1")]
        s_junk, s_st = Slot("junk"), Slot("st")
        dx = [P.dsem(f"x{row0}_0"), P.dsem(f"x{row0}_1")]
        for i in range(ntile):
            b = i % 2
            x_t = xt[b]
            P.dma("sp", x_t[:], xin[row0 + i * 128: row0 + (i + 1) * 128, :], dx[b], writes=[s_xt[b]])
            P.op("dve", lambda e, x_t=x_t: e.tensor_reduce(out=st_[:, 0:1], in_=x_t[:], axis=AX.X, op=ALU.add),
                 reads=[s_xt[b]], writes=[s_st])
            P.op("act", lambda e, x_t=x_t: e.activation(out=junk[:], in_=x_t[:], func=AF.Square, accum_out=st_[:, 1:2]),
                 reads=[s_xt[b], s_st], writes=[s_junk, s_st])
            P.op("dve", lambda e: e.tensor_scalar(out=st_[:, 2:3], in0=st_[:, 0:1], scalar1=1.0 / D, scalar2=None, op0=ALU.mult),
                 reads=[s_st], writes=[s_st])
            P.op("dve", lambda e: e.tensor_tensor(out=st_[:, 3:4], in0=st_[:, 2:3], in1=st_[:, 2:3], op=ALU.mult),
                 reads=[s_st], writes=[s_st])
            P.op("dve", lambda e: e.scalar_tensor_tensor(out=st_[:, 4:5], in0=st_[:, 1:2], scalar=1.0 / D, in1=st_[:, 3:4],
                                                         op0=ALU.mult, op1=ALU.subtract),
                 reads=[s_st], writes=[s_st])
            P.op("act", lambda e: e.activation(out=st_[:, 5:6], in_=st_[:, 4:5], func=AF.Sqrt, bias=epsc[:], scale=1.0),
                 reads=[s_st], writes=[s_st])
            P.op("dve", lambda e: e.reciprocal(out=st_[:, 6:7], in_=st_[:, 5:6]), reads=[s_st], writes=[s_st])
            P.op("dve", lambda e, x_t=x_t: e.tensor_scalar(out=x_t[:], in0=x_t[:], scalar1=st_[:, 2:3], scalar2=st_[:, 6:7],
                                                           op0=ALU.subtract, op1=ALU.mult),
                 reads=[s_st], writes=[s_xt[b]])
            tcol = slice((tile0 + i) * 128, (tile0 + i + 1) * 128)
            tb = (tile0 + i) // 4
            for c4 in range(4):
                bank = c4 % 2
                for cc in range(4):
                    c = c4 * 4 + cc
                    P.op("pe", lambda e, c=c, cc=cc, bank=bank, x_t=x_t: e.transpose(
                        PS[bank][:, cc * 128:(cc + 1) * 128], x_t[:, c * 128:(c + 1) * 128], ident[:]),
                        reads=[s_xt[b]], writes=[sPS[bank]])
                for cc in range(4):
                    c = c4 * 4 + cc
                    if dest_f32 is not None:
                        P.op("act", lambda e, c=c, cc=cc, bank=bank: e.activation(
                            out=dest_f32[:, c, tcol], in_=PS[bank][:, cc * 128:(cc + 1) * 128], func=AF.Identity,
                            scale=GCOL(0, c), bias=GCOL(1, c)),
                            reads=[sPS[bank]], writes=[s_dst_f[c][tb]])
                    P.op("act", lambda e, c=c, cc=cc, bank=bank: e.activation(
                        out=dest_bf[:, c, tcol], in_=PS[bank][:, cc * 128:(cc + 1) * 128], func=AF.Identity,
                        scale=GCOL(0, c), bias=GCOL(1, c)),
                        reads=[sPS[bank]], writes=[s_dst_b[tb]])

    def mixer(l, Tq, CB, sCB, tag):
        nblk = Tq // 512
        ntq = Tq // 128
        ntk = ntq + 4
        m0 = M.mark()
        ymix = M.alloc([128, 8, Tq], BF16)
        s_ymix = [[Slot(f"ymix{c}_{tb}") for tb in range(nblk)] for c in range(8)]
        wq = [M.alloc([128, NCH, 128], BF16) for _ in range(3)]
        s_wq = [Slot(f"wq{i}") for i in range(3)]
        d_wq = [P.dsem(f"wq{tag}{i}") for i in range(3)]
        ring = [0]

        def load_cols(c0):
            i = ring[0] % 3
            ring[0] += 1
            P.dma("pool", wq[i][:], w_in[l, :, c0:c0 + 128].rearrange("(c p) n -> p c n", p=128), d_wq[i],
                  writes=[s_wq[i]])
            return wq[i], s_wq[i]

        def tok_src(tbk):
            if tbk == 0:
                return (lambda c: CB[:, c, :]), sCB
            return (lambda c, tbk=tbk: HB[:, c, (tbk - 1) * 512: tbk * 512]), sHB[tbk - 1]

        def tok_tile(j):
            if j < 4:
                return (lambda c: CB[:, c, j * 128:(j + 1) * 128]), sCB
            return (lambda c: HB[:, c, (j - 4) * 128:(j - 3) * 128]), sHB[(j - 4) // 4]

        m1 = M.mark()
        E = M.alloc([128, 8, 640], BF16)
        etmp = [M.alloc([128, 640], F32), M.alloc([128, 640], F32)]
        s_E, s_etmp = Slot("E"), [Slot("et0"), Slot("et1")]
        d_et = [P.dsem(f"et{tag}0"), P.dsem(f"et{tag}1")]
        kT = [M.alloc([128, 512 + Tq], BF16) for _ in range(2)]
        qT = [M.alloc([128, Tq], BF16) for _ in range(2)]
        Vh = [M.alloc([128, ntk, 128], BF16) for _ in range(2)]
        Pt = [M.alloc([128, 640], BF16) for _ in range(2)]
        rec = [M.alloc([128, 128], F32) for _ in range(2)]
        s_kT, s_qT, s_Vh = [Slot("kT0"), Slot("kT1")], [Slot("qT0"), Slot("qT1")], [Slot("Vh0"), Slot("Vh1")]
        s_Pt, s_rec = [Slot("Pt0"), Slot("Pt1")], [Slot("rec0"), Slot("rec1")]
        for h in range(8):
            b = h % 2
            P.dma("sp", etmp[b][:], relT[l, h], d_et[b], writes=[s_etmp[b]])
            P.op("act", lambda e, h=h, b=b: e.activation(out=E[:, h, :], in_=etmp[b][:], func=AF.Exp),
                 reads=[s_etmp[b]], writes=[s_E])
            P.op("pool", lambda e, h=h: e.memset(E[0:64, h, 64:128], 0.0), writes=[s_E])
            P.op("pool", lambda e, h=h: e.memset(E[64:128, h, 512:576], 0.0), writes=[s_E])
        scale = 1.0 / float(np.sqrt(128.0))
        pending = [load_cols(0), load_cols(1024), load_cols(2048)]
        for h in range(8):
            b = h % 2
            (wqh, s_wqh), (wkh, s_wkh), (wvh, s_wvh) = pending
            for tbk in range(nblk + 1):
                src, s_src = tok_src(tbk)
                bank = tbk % 2
                for c in range(NCH):
                    P.op("pe", lambda e, c=c, src=src, bank=bank, wkh=wkh: e.matmul(
                        PS[bank][:], lhsT=wkh[:, c, :], rhs=src(c), start=(c == 0), stop=(c == NCH - 1)),
                        reads=[s_wkh, s_src], writes=[sPS[bank]], sig=(c == NCH - 1))
                P.op("act", lambda e, b=b, bank=bank, tbk=tbk: e.copy(out=kT[b][:, tbk * 512:(tbk + 1) * 512], in_=PS[bank][:]),
                     reads=[sPS[bank]], writes=[s_kT[b]])
            for tb in range(nblk):
                src, s_src = tok_src(tb + 1)
                bank = 2 + tb % 2
                for c in range(NCH):
                    P.op("pe", lambda e, c=c, src=src, bank=bank, wqh=wqh: e.matmul(
                        PS[bank][:], lhsT=wqh[:, c, :], rhs=src(c), start=(c == 0), stop=(c == NCH - 1)),
                        reads=[s_wqh, s_src], writes=[sPS[bank]], sig=(c == NCH - 1))
                P.op("act", lambda e, b=b, bank=bank, tb=tb: e.copy(out=qT[b][:, tb * 512:(tb + 1) * 512], in_=PS[bank][:]),
                     reads=[sPS[bank]], writes=[s_qT[b]])
            for j4 in range(ntk // 4):
                bank = 4 + j4 % 2
                for jj in range(4):
                    j = j4 * 4 + jj
                    src, s_src = tok_tile(j)
                    for c in range(NCH):
                        P.op("pe", lambda e, c=c, src=src, bank=bank, jj=jj, wvh=wvh: e.matmul(
                            PS[bank][:, jj * 128:(jj + 1) * 128], lhsT=src(c), rhs=wvh[:, c, :],
                            start=(c == 0), stop=(c == NCH - 1)),
                            reads=[s_wvh, s_src], writes=[sPS[bank]], sig=(c == NCH - 1))
                P.op("dve", lambda e, b=b, bank=bank, j4=j4: e.tensor_copy(
                    out=Vh[b][:, j4 * 4:(j4 + 1) * 4, :], in_=PS[bank][:].rearrange("p (j d) -> p j d", d=128)),
                    reads=[sPS[bank]], writes=[s_Vh[b]])
            if h < 7:
                pending = [load_cols((h + 1) * 128), load_cols(1024 + (h + 1) * 128), load_cols(2048 + (h + 1) * 128)]
            def scores(i):
                pb = i % 2
                sa, sb_ = (6, 7) if i % 2 == 0 else (0, 1)
                for j in range(5):
                    dst = PS[sa][:, j * 128:(j + 1) * 128] if j < 4 else PS[sb_][:, 0:128]
                    P.op("pe", lambda e: e.matmul(
                        dst, lhsT=kT[b][:, (i + j) * 128:(i + j + 1) * 128], rhs=qT[b][:, i * 128:(i + 1) * 128],
                        start=True, stop=True),
                        reads=[s_kT[b], s_qT[b]], writes=[sPS[sa] if j < 4 else sPS[sb_]], sig=(j >= 3))
                P.op("act", lambda e: e.activation(out=Pt[pb][:, 0:512], in_=PS[sa][:], func=AF.Exp, scale=scale),
                     reads=[sPS[sa]], writes=[s_Pt[pb]])
                P.op("act", lambda e: e.activation(out=Pt[pb][:, 512:640], in_=PS[sb_][:, 0:128], func=AF.Exp, scale=scale),
                     reads=[sPS[sb_]], writes=[s_Pt[pb]])
                nctx = max(0, 4 - i)
                if nctx > 0:
                    P.op("dve", lambda e: e.scalar_tensor_tensor(
                        out=Pt[pb][:, 0:nctx * 128], in0=Pt[pb][:, 0:nctx * 128], scalar=hv[:, 0:1],
                        in1=E[:, h, 0:nctx * 128], op0=ALU.mult, op1=ALU.mult),
                        reads=[s_E, sConst], writes=[s_Pt[pb]])
                P.op("pool", lambda e: e.tensor_tensor(
                    out=Pt[pb][:, nctx * 128:640], in0=Pt[pb][:, nctx * 128:640], in1=E[:, h, nctx * 128:640], op=ALU.mult),
                    reads=[s_E], writes=[s_Pt[pb]])

            def pv(i):
                pb = i % 2
                od = 2 + (i % 2)
                for j in range(5):
                    P.op("pe", lambda e: e.matmul(
                        PS[od][:, 0:128], lhsT=Vh[b][:, i + j, :], rhs=Pt[pb][:, j * 128:(j + 1) * 128],
                        start=(j == 0), stop=(j == 4)),
                        reads=[s_Vh[b], s_Pt[pb]], writes=[sPS[od]], sig=False)
                for j in range(5):
                    P.op("pe", lambda e: e.matmul(
                        PS[od][:, 128:256], lhsT=ones_b[:], rhs=Pt[pb][:, j * 128:(j + 1) * 128],
                        start=(j == 0), stop=(j == 4)),
                        reads=[s_Pt[pb], sConst], writes=[sPS[od]], sig=(j == 4))
                P.op("dve", lambda e: e.reciprocal(out=rec[pb][:], in_=PS[od][:, 128:256]),
                     reads=[sPS[od]], writes=[s_rec[pb]])
                P.op("dve", lambda e: e.tensor_tensor(
                    out=ymix[:, h, i * 128:(i + 1) * 128], in0=PS[od][:, 0:128], in1=rec[pb][:], op=ALU.mult),
                    reads=[sPS[od], s_rec[pb]], writes=[s_ymix[h][i // 4]])

            for i in range(ntq + 1):
                if i < ntq:
                    scores(i)
                if i >= 1:
                    pv(i - 1)
        P.barrier()
        M.release(m1)

        def out_proj(row0, first):
            wo = [M.alloc([128, 8, 128], BF16) for _ in range(3)]
            s_wo = [Slot(f"wo{i}") for i in range(3)]
            d_wo = [P.dsem(f"wo{tag}{row0}_{i}") for i in range(3)]

            def load(db):
                i = db % 3
                P.dma("pool", wo[i][:], w_out[l, row0:row0 + 1024, db * 128:(db + 1) * 128].rearrange("(c p) n -> p c n", p=128),
                      d_wo[i], writes=[s_wo[i]])
            load(0); load(1)
            for db in range(NCH):
                if db + 2 < NCH:
                    load(db + 2)
                i = db % 3
                for tb in range(nblk):
                    bank = (db * nblk + tb) % 4
                    blk = slice(tb * 512, tb * 512 + 512)
                    for c in range(8):
                        P.op("pe", lambda e, c=c, i=i, bank=bank, blk=blk: e.matmul(
                            PS[bank][:], lhsT=wo[i][:, c, :], rhs=ymix[:, c, blk], start=(c == 0), stop=(c == 7)),
                            reads=[s_wo[i], s_ymix[c][tb]], writes=[sPS[bank]], sig=(c == 7))
                    if first:
                        P.op("dve", lambda e, db=db, bank=bank, blk=blk: e.scalar_tensor_tensor(
                            out=X[:, db, blk], in0=X[:, db, blk], scalar=ALPHA, in1=PS[bank][:], op0=ALU.mult, op1=ALU.add),
                            reads=[sPS[bank]], writes=[sX[db][tb]])
                    else:
                        P.op("dve", lambda e, db=db, bank=bank, blk=blk: e.tensor_tensor(
                            out=X[:, db, blk], in0=X[:, db, blk], in1=PS[bank][:], op=ALU.add),
                            reads=[sPS[bank]], writes=[sX[db][tb]])

        m2 = M.mark()
        out_proj(0, True)
        P.barrier()
        M.release(m2)

        gbc = M.alloc([128, 1024], F32)
        bbc = M.alloc([128, 1024], F32)
        bsb = M.alloc([128, 8, 128], F32)
        WmT = M.alloc([128, 8, 128], BF16)
        vln = M.alloc_at(CB_OFF[0], [128, ntq, 1024], BF16)
        wvs = M.alloc([128, NCH, 256], BF16)
        vtmp_off = M.mark()
        vtmp = M.alloc([128, 1024], F32)
        wtmp = M.alloc_at(vtmp_off, [128, 8, 128], F32)
        junk = M.alloc([128, 1024], BF16)
        stt = M.alloc([128, ntq, 12], F32)
        ug = [M.alloc_at(LN_OFF + k * 4096, [128, Tq], F32) for k in range(2)]
        mtmp = [M.alloc([128, 512], F32) for _ in range(2)]
        s_g, s_wtmp, s_WmT, s_wvs, s_junk = Slot("g"), Slot("wtmp"), Slot("WmT"), Slot("wvs"), Slot("junk")
        s_vtmp = s_wtmp
        s_vln = [Slot(f"vln{i}") for i in range(ntq)]
        s_stt = [Slot(f"stt{i}") for i in range(ntq)]
        s_ug, s_mtmp = [Slot("ug0"), Slot("ug1")], [Slot("mt0"), Slot("mt1")]
        d_g, d_wvs = P.dsem(f"sg{tag}"), P.dsem(f"wvs{tag}")
        P.dma("sp", gbc[:], sgu_g[l].partition_broadcast(128), d_g, writes=[s_g])
        P.dma("sp", bbc[:], sgu_bb[l].partition_broadcast(128), d_g, writes=[s_g])
        P.dma("sp", bsb[:].rearrange("p g t -> p (g t)"), sgu_b[l].rearrange("g t -> (g t)").partition_broadcast(128), d_g, writes=[s_g])
        P.dma("sp", wtmp[:], sgu_w[l].rearrange("g t s -> t g s"), P.dsem(f"wt{tag}"), writes=[s_wtmp])
        for g in range(8):
            P.op("pool", lambda e, g=g: e.affine_select(out=wtmp[:, g, :], in_=wtmp[:, g, :], pattern=[[-1, 128]],
                                                        compare_op=ALU.is_ge, fill=0.0, base=0, channel_multiplier=1),
                 reads=[s_g], writes=[s_wtmp])
        for g4 in range(2):
            for gg in range(4):
                g = g4 * 4 + gg
                P.op("pe", lambda e, g=g, gg=gg, g4=g4: e.transpose(PS[g4][:, gg * 128:(gg + 1) * 128], wtmp[:, g, :], ident[:]),
                     reads=[s_wtmp, sConst], writes=[sPS[g4]])
            P.op("act", lambda e, g4=g4: e.copy(out=WmT[:, g4 * 4:(g4 + 1) * 4, :], in_=PS[g4][:].rearrange("p (g t) -> p g t", t=128)),
                 reads=[sPS[g4]], writes=[s_WmT])
        for qt in range(4):
            P.dma("pool", wvs[:], w_in[l, :, 4096 + qt * 256: 4096 + (qt + 1) * 256].rearrange("(c p) n -> p c n", p=128),
                  d_wvs, writes=[s_wvs])
            for i in range(ntq):
                bank = 2 + i % 4
                for c in range(NCH):
                    P.op("pe", lambda e, c=c, i=i, bank=bank: e.matmul(
                        PS[bank][:, 0:256], lhsT=HB[:, c, i * 128:(i + 1) * 128], rhs=wvs[:, c, :], start=(c == 0), stop=(c == NCH - 1)),
                        reads=[s_wvs, sHB[i // 4]], writes=[sPS[bank]], sig=(c == NCH - 1))
                P.op("act", lambda e, i=i, bank=bank, qt=qt: e.activation(
                    out=vln[:, i, qt * 256:(qt + 1) * 256], in_=PS[bank][:, 0:256], func=AF.Gelu, accum_out=stt[:, i, qt:qt + 1]),
                    reads=[sPS[bank]], writes=[s_vln[i], s_stt[i]])
        for i in range(ntq):
            S_ = lambda k, i=i: stt[:, i, k:k + 1]
            P.op("act", lambda e, i=i: e.activation(out=junk[:], in_=vln[:, i, :], func=AF.Square, accum_out=stt[:, i, 4:5]),
                 reads=[s_vln[i]], writes=[s_junk, s_stt[i]])
            P.op("dve", lambda e, S_=S_, i=i: e.tensor_reduce(out=S_(5), in_=stt[:, i, 0:4], axis=AX.X, op=ALU.add), reads=[s_stt[i]], writes=[s_stt[i]])
            P.op("dve", lambda e, S_=S_: e.tensor_scalar(out=S_(5), in0=S_(5), scalar1=1.0 / 1024, scalar2=None, op0=ALU.mult),
                 reads=[s_stt[i]], writes=[s_stt[i]])
            P.op("dve", lambda e, S_=S_: e.tensor_tensor(out=S_(6), in0=S_(5), in1=S_(5), op=ALU.mult), reads=[s_stt[i]], writes=[s_stt[i]])
            P.op("dve", lambda e, S_=S_: e.scalar_tensor_tensor(out=S_(7), in0=S_(4), scalar=1.0 / 1024, in1=S_(6),
                                                                op0=ALU.mult, op1=ALU.subtract), reads=[s_stt[i]], writes=[s_stt[i]])
            P.op("act", lambda e, S_=S_: e.activation(out=S_(8), in_=S_(7), func=AF.Sqrt, bias=epsc[:], scale=1.0),
                 reads=[s_stt[i]], writes=[s_stt[i]])
            P.op("dve", lambda e, S_=S_: e.reciprocal(out=S_(9), in_=S_(8)), reads=[s_stt[i]], writes=[s_stt[i]])
            P.op("dve", lambda e, S_=S_, i=i: e.tensor_scalar(out=vtmp[:], in0=vln[:, i, :], scalar1=S_(5), scalar2=S_(9),
                                                              op0=ALU.subtract, op1=ALU.mult),
                 reads=[s_vln[i], s_stt[i]], writes=[s_vtmp])
            P.op("dve", lambda e: e.tensor_tensor(out=vtmp[:], in0=vtmp[:], in1=gbc[:], op=ALU.mult), reads=[s_g], writes=[s_vtmp])
            P.op("dve", lambda e, i=i: e.tensor_tensor(out=vln[:, i, :], in0=vtmp[:], in1=bbc[:], op=ALU.add),
                 reads=[s_g, s_vtmp], writes=[s_vln[i]])
        pend = load_cols(3072)
        for g in range(8):
            wug, s_wug = pend
            if g < 7:
                pend = load_cols(3072 + (g + 1) * 128)
            ub = g % 2
            for tb in range(nblk):
                bank = tb % 2
                blk = slice(tb * 512, tb * 512 + 512)
                for c in range(NCH):
                    P.op("pe", lambda e, c=c, bank=bank, blk=blk, wug=wug: e.matmul(
                        PS[bank][:], lhsT=wug[:, c, :], rhs=HB[:, c, blk], start=(c == 0), stop=(c == NCH - 1)),
                        reads=[s_wug, sHB[tb]], writes=[sPS[bank]], sig=(c == NCH - 1))
                P.op("act", lambda e, ub=ub, bank=bank, blk=blk: e.activation(out=ug[ub][:, blk], in_=PS[bank][:], func=AF.Gelu),
                     reads=[sPS[bank]], writes=[s_ug[ub]])
            for tb in range(nblk):
                bank = 6 + tb % 2
                blk = slice(tb * 512, tb * 512 + 512)
                for ii in range(4):
                    i = tb * 4 + ii
                    P.op("pe", lambda e, g=g, i=i, ii=ii, bank=bank: e.matmul(
                        PS[bank][:, ii * 128:(ii + 1) * 128], lhsT=vln[:, i, g * 128:(g + 1) * 128], rhs=WmT[:, g, :],
                        start=True, stop=True),
                        reads=[s_vln[i], s_WmT], writes=[sPS[bank]], sig=(ii == 3))
                mt = mtmp[tb % 2]
                P.op("dve", lambda e, g=g, bank=bank, mt=mt: e.tensor_tensor(
                    out=mt[:].rearrange("p (r t) -> p r t", t=128), in0=PS[bank][:].rearrange("p (r t) -> p r t", t=128),
                    in1=bsb[:, g, :].unsqueeze(1).to_broadcast([128, 4, 128]), op=ALU.add),
                    reads=[sPS[bank], s_g], writes=[s_mtmp[tb % 2]])
                P.op("dve", lambda e, g=g, ub=ub, blk=blk, mt=mt: e.tensor_tensor(
                    out=ymix[:, g, blk], in0=mt[:], in1=ug[ub][:, blk], op=ALU.mult),
                    reads=[s_mtmp[tb % 2], s_ug[ub]], writes=[s_ymix[g][tb]])
        P.barrier()
        M.release(m2)
        out_proj(1024, False)
        P.barrier()
        M.release(m0)

    def router(Tq):
        ntq = Tq // 128
        sm = lambda a, b: small[:, a:b]
        for i in range(ntq):
            tcol = slice(i * 128, (i + 1) * 128)
            tb = i // 4
            for c in range(NCH):
                P.op("pe", lambda e, c=c, tcol=tcol: e.matmul(PS[0][:, 0:NE], lhsT=X[:, c, tcol], rhs=rw[:, c, :],
                                                             start=(c == 0), stop=(c == NCH - 1)),
                     reads=[sX[c][tb], sConst], writes=[sPS[0]], sig=(c == NCH - 1))
            R, W = [sSmall], [sSmall]
            lg, ex, pr, gm, m, eq, m2, sl, cb = (sm(0, 16), sm(16, 32), sm(32, 48), sm(48, 52), sm(64, 80), sm(80, 96),
                                                sm(96, 112), sm(112, 128), sm(128, 144))
            pairs, gs = sm(160, 184), sm(184, 188)
            c1 = lambda k: small[:, 200 + k: 201 + k]
            P.op("dve", lambda e: e.tensor_tensor(out=lg, in0=PS[0][:, 0:NE], in1=rb[:], op=ALU.add), reads=[sPS[0], sConst], writes=W)
            P.op("dve", lambda e: e.tensor_reduce(out=c1(0), in_=lg, axis=AX.X, op=ALU.max), reads=R, writes=W)
            P.op("dve", lambda e: e.tensor_scalar(out=c1(1), in0=c1(0), scalar1=-1.0, scalar2=None, op0=ALU.mult), reads=R, writes=W)
            P.op("act", lambda e: e.activation(out=ex, in_=lg, func=AF.Exp, bias=c1(1), scale=1.0, accum_out=c1(2)), reads=R, writes=W)
            P.op("dve", lambda e: e.reciprocal(out=c1(3), in_=c1(2)), reads=R, writes=W)
            P.op("dve", lambda e: e.tensor_scalar(out=pr, in0=ex, scalar1=c1(3), scalar2=None, op0=ALU.mult), reads=R, writes=W)
            pv = pr.rearrange("p (g k) -> p g k", k=4)
            pw = pairs.rearrange("p (g k) -> p g k", k=6)
            for idx, (a, b_) in enumerate([(0, 1), (0, 2), (0, 3), (1, 2), (1, 3), (2, 3)]):
                P.op("dve", lambda e, idx=idx, a=a, b_=b_: e.tensor_tensor(out=pw[:, :, idx:idx + 1], in0=pv[:, :, a:a + 1],
                                                                             in1=pv[:, :, b_:b_ + 1], op=ALU.add), reads=R, writes=W)
            P.op("dve", lambda e: e.tensor_reduce(out=gs, in_=pw, axis=AX.X, op=ALU.max), reads=R, writes=W)
            P.op("dve", lambda e: e.tensor_reduce(out=c1(4), in_=gs, axis=AX.X, op=ALU.max), reads=R, writes=W)
            P.op("dve", lambda e: e.tensor_scalar(out=gm, in0=gs, scalar1=c1(4), scalar2=None, op0=ALU.is_ge), reads=R, writes=W)
            mv = m.rearrange("p (g k) -> p g k", k=4)
            P.op("dve", lambda e: e.tensor_tensor(out=mv, in0=pv, in1=gm.unsqueeze(2).to_broadcast([128, 4, 4]), op=ALU.mult), reads=R, writes=W)
            P.op("dve", lambda e: e.tensor_scalar(out=gs, in0=gm, scalar1=-1.0, scalar2=None, op0=ALU.add), reads=R, writes=W)
            P.op("dve", lambda e: e.tensor_tensor(out=mv, in0=mv, in1=gs.unsqueeze(2).to_broadcast([128, 4, 4]), op=ALU.add), reads=R, writes=W)
            P.op("dve", lambda e: e.tensor_reduce(out=c1(5), in_=m, axis=AX.X, op=ALU.max), reads=R, writes=W)
            P.op("dve", lambda e: e.tensor_scalar(out=eq, in0=m, scalar1=c1(5), scalar2=None, op0=ALU.is_equal), reads=R, writes=W)
            P.op("dve", lambda e: e.scalar_tensor_tensor(out=m2, in0=eq, scalar=-2.0, in1=m, op0=ALU.mult, op1=ALU.add), reads=R, writes=W)
            P.op("dve", lambda e: e.tensor_reduce(out=c1(6), in_=m2, axis=AX.X, op=ALU.max), reads=R, writes=W)
            P.op("dve", lambda e: e.tensor_scalar(out=sl, in0=m, scalar1=c1(6), scalar2=None, op0=ALU.is_ge), reads=R, writes=W)
            P.op("dve", lambda e: e.tensor_tensor(out=c1(7), in0=c1(5), in1=c1(6), op=ALU.add), reads=R, writes=W)
            P.op("dve", lambda e: e.reciprocal(out=c1(8), in_=c1(7)), reads=R, writes=W)
            P.op("dve", lambda e: e.scalar_tensor_tensor(out=cb, in0=m, scalar=c1(8), in1=sl, op0=ALU.mult, op1=ALU.mult), reads=R, writes=W)
            P.op("pe", lambda e: e.transpose(PS[1][0:NE, 0:128], cb, ident[:]), reads=R + [sConst], writes=[sPS[1]])
            P.op("act", lambda e, tcol=tcol: e.copy(out=combT[:, tcol], in_=PS[1][0:NE, 0:128]), reads=[sPS[1]], writes=[sCombT])

    def moe(l, Tq, tag):
        nblk = Tq // 512
        m0 = M.mark()
        hid = M.alloc_at(CB_OFF[0], [128, 8, 1024], BF16)
        s_hid = [Slot(f"hid{tb}") for tb in range(nblk)]
        wg = [M.alloc([128, NCH, 256], BF16) for _ in range(2)]
        wu = [M.alloc([128, NCH, 256], BF16) for _ in range(2)]
        wd = [M.alloc([128, 8, 512], BF16) for _ in range(2)]
        s_wg, s_wu, s_wd = [Slot("wg0"), Slot("wg1")], [Slot("wu0"), Slot("wu1")], [Slot("wd0"), Slot("wd1")]
        d_wg = [P.dsem(f"wg{tag}{i}") for i in range(2)]
        d_wu = [P.dsem(f"wu{tag}{i}") for i in range(2)]
        d_wd = [P.dsem(f"wd{tag}{i}") for i in range(2)]
        ssb = [M.alloc([128, 512], F32) for _ in range(2)]
        tsb = [M.alloc([128, 512], F32) for _ in range(2)]
        s_ssb, s_tsb = [Slot("ssb0"), Slot("ssb1")], [Slot("tsb0"), Slot("tsb1")]

        def load_gu(n):
            e_, fb = divmod(n, 4)
            i = n % 2
            P.dma("pool", wg[i][:], w_gate[l, e_, :, fb * 256:(fb + 1) * 256].rearrange("(c p) n -> p c n", p=128), d_wg[i], writes=[s_wg[i]])
            P.dma("pool", wu[i][:], w_up[l, e_, :, fb * 256:(fb + 1) * 256].rearrange("(c p) n -> p c n", p=128), d_wu[i], writes=[s_wu[i]])

        def load_d(n):
            e_, dq = divmod(n, 4)
            i = n % 2
            P.dma("pool", wd[i][:], w_down[l, e_, :, dq * 512:(dq + 1) * 512].rearrange("(c p) n -> p c n", p=128), d_wd[i], writes=[s_wd[i]])

        load_gu(0); load_gu(1); load_d(0); load_d(1)
        yield
        cnt = 0
        for e_ in range(NE):
            for tb in range(nblk):
                P.op("pe", lambda e, e_=e_, tb=tb: e.matmul(PS[4 + tb][:], lhsT=sel[:, e_, :], rhs=combT[:, tb * 512:(tb + 1) * 512],
                                                           start=True, stop=True),
                     reads=[sCombT, sConst], writes=[sPS[4 + tb]])
            for fb in range(4):
                n = e_ * 4 + fb
                i = n % 2
                for tb in range(nblk):
                    blk = slice(tb * 512, tb * 512 + 512)
                    for fs in range(2):
                        f128 = fb * 2 + fs
                        gb, ub = cnt % 2, 2 + cnt % 2
                        cnt += 1
                        for c in range(NCH):
                            P.op("pe", lambda e, c=c, i=i, fs=fs, gb=gb, blk=blk: e.matmul(
                                PS[gb][:], lhsT=wg[i][:, c, fs * 128:(fs + 1) * 128], rhs=HB[:, c, blk], start=(c == 0), stop=(c == NCH - 1)),
                                reads=[s_wg[i], sHB[tb]], writes=[sPS[gb]], sig=(c == NCH - 1))
                        for c in range(NCH):
                            P.op("pe", lambda e, c=c, i=i, fs=fs, ub=ub, blk=blk: e.matmul(
                                PS[ub][:], lhsT=wu[i][:, c, fs * 128:(fs + 1) * 128], rhs=HB[:, c, blk], start=(c == 0), stop=(c == NCH - 1)),
                                reads=[s_wu[i], sHB[tb]], writes=[sPS[ub]], sig=(c == NCH - 1))
                        k = cnt % 2
                        P.op("act", lambda e, k=k, gb=gb: e.activation(out=ssb[k][:], in_=PS[gb][:], func=AF.Silu),
                             reads=[sPS[gb]], writes=[s_ssb[k]])
                        P.op("dve", lambda e, k=k, ub=ub: e.tensor_tensor(out=tsb[k][:], in0=ssb[k][:], in1=PS[ub][:], op=ALU.mult),
                             reads=[s_ssb[k], sPS[ub]], writes=[s_tsb[k]])
                        P.op("dve", lambda e, k=k, tb=tb, f128=f128, blk=blk: e.tensor_tensor(
                            out=hid[:, f128, blk], in0=tsb[k][:], in1=PS[4 + tb][:], op=ALU.mult),
                            reads=[s_tsb[k], sPS[4 + tb]], writes=[s_hid[tb]])
                if n + 2 < NE * 4:
                    load_gu(n + 2)
            for dq in range(4):
                n = e_ * 4 + dq
                i = n % 2
                for tb in range(nblk):
                    blk = slice(tb * 512, tb * 512 + 512)
                    for ds in range(4):
                        db = dq * 4 + ds
                        bank = 6 + cnt % 2
                        cnt += 1
                        for c in range(8):
                            P.op("pe", lambda e, c=c, i=i, ds=ds, bank=bank, blk=blk: e.matmul(
                                PS[bank][:], lhsT=wd[i][:, c, ds * 128:(ds + 1) * 128], rhs=hid[:, c, blk], start=(c == 0), stop=(c == 7)),
                                reads=[s_wd[i], s_hid[tb]], writes=[sPS[bank]], sig=(c == 7))
                        if e_ == 0:
                            P.op("dve", lambda e, db=db, bank=bank, blk=blk: e.scalar_tensor_tensor(
                                out=X[:, db, blk], in0=X[:, db, blk], scalar=ALPHA, in1=PS[bank][:], op0=ALU.mult, op1=ALU.add),
                                reads=[sPS[bank]], writes=[sX[db][tb]])
                        else:
                            P.op("dve", lambda e, db=db, bank=bank, blk=blk: e.tensor_tensor(
                                out=X[:, db, blk], in0=X[:, db, blk], in1=PS[bank][:], op=ALU.add),
                                reads=[sPS[bank]], writes=[sX[db][tb]])
                if n + 2 < NE * 4:
                    load_d(n + 2)
        M.release(m0)

    def layer(l, Tq, CB, sCB, tag):
        stage(tag + "_in", Tq)
        mixer(l, Tq, CB, sCB, tag)
        stage(tag + "_mix", Tq)
        g = moe(l, Tq, tag)
        next(g)
        ln_fm(X, NCH, Tq, 2 + 4 * l, 3 + 4 * l, X, HB, sX, sHB, 6, 7, ln_tmps())
        stage(tag + "_ln1", Tq)
        router(Tq)
        next(g, None)
        stage(tag + "_moe", Tq)
        ln_fm(X, NCH, Tq, 4 + 4 * l, 5 + 4 * l, X, HB, sX, sHB, 6, 7, ln_tmps())
        P.barrier()
        stage(tag + "_ln2", Tq)

    CB_OFF[0] = M.mark()
    CB = M.alloc([128, NCH, 512], BF16)
    sCB = Slot("CB")
    s_cbsc = Slot("cbsc")
    ARENA2 = M.mark()
    d_cb = P.dsem("cbsc")

    def _main_seq():
        ln_in(0, 4, None, CB, 0, None, [sCB] * 2)
        P.barrier(); M.release(ARENA2)
        ln_in(512, 4, X, HB, 0, sX, sHB)
        P.barrier(); M.release(ARENA2)
        layer(0, 512, CB, sCB, "H")
        P.dma("sp", cbsc.rearrange("p (c t) -> p c t", c=NCH), HB[:, :, 0:512], d_cb, reads=[sHB[0]], writes=[s_cbsc])
        ln_in(512, 4, None, CB, 0, None, [sCB] * 2)
        P.barrier(); M.release(ARENA2)
        ln_in(1024, 8, X, HB, 0, sX, sHB)
        P.barrier(); M.release(ARENA2)
        layer(0, 1024, CB, sCB, "O")
        P.dma("sp", CB[:], cbsc.rearrange("p (c t) -> p c t", c=NCH), d_cb, reads=[s_cbsc], writes=[sCB])
        layer(1, 1024, CB, sCB, "L")

        ot = [M.alloc([128, D], F32), M.alloc([128, D], F32)]
        s_ot = [Slot("ot0"), Slot("ot1")]
        d_out = [P.dsem("out0"), P.dsem("out1")]
        for i in range(8):
            b = i % 2
            for c4 in range(4):
                bank = c4 % 4
                for cc in range(4):
                    c = c4 * 4 + cc
                    P.op("pe", lambda e, c=c, cc=cc, bank=bank, i=i: e.transpose(
                        PS[bank][:, cc * 128:(cc + 1) * 128], X[:, c, i * 128:(i + 1) * 128], ident[:]),
                        reads=[sX[c][i // 4], sConst], writes=[sPS[bank]])
                if c4 % 2:
                    P.op("act", lambda e, b=b, c4=c4, bank=bank: e.copy(out=ot[b][:, c4 * 512:(c4 + 1) * 512], in_=PS[bank][:]),
                         reads=[sPS[bank]], writes=[s_ot[b]])
                else:
                    P.op("dve", lambda e, b=b, c4=c4, bank=bank: e.tensor_copy(out=ot[b][:, c4 * 512:(c4 + 1) * 512], in_=PS[bank][:]),
                         reads=[sPS[bank]], writes=[s_ot[b]])
            P.dma("sp", out[i * 128:(i + 1) * 128, :], ot[b][:], d_out[b], reads=[s_ot[b]], writes=[Slot("o")])

    try:
        _main_seq()
    except _Stop:
        pass
    P.barrier()
    P.emit(nc, st)
    st.close()
    return nc, M.peak


_CACHE = {}


def _rel_index():
    k = np.arange(128)[:, None, None]
    j = np.arange(5)[None, :, None]
    q = np.arange(128)[None, None, :]
    rel = (4 - j) * 128 + q - k
    return (np.clip(rel, -128, 128) + 128).reshape(128, 640)


def kernel(x, ln_in_g, ln_in_b, w_in, rel_bias, sgu_ln_g, sgu_ln_b, sgu_w, sgu_b, w_out, ln1_g, ln1_b,
           router_w, router_b, w_gate, w_up, w_down, ln2_g, ln2_b):
    f = lambda a: np.ascontiguousarray(np.asarray(a, dtype=np.float32))
    x = f(x)
    if "nc" not in _CACHE:
        _CACHE["nc"], _CACHE["peak"] = build_program()
    nc = _CACHE["nc"]
    vecs = [ln_in_g, ln_in_b, ln1_g[0], ln1_b[0], ln2_g[0], ln2_b[0], ln1_g[1], ln1_b[1], ln2_g[1], ln2_b[1]]
    cols = np.concatenate([f(v).reshape(NCH, 128).T for v in vecs], axis=1)
    relT = f(np.asarray(rel_bias)[:, :, _rel_index()])
    shared = {"cols": f(cols), "w_in": f(w_in), "relT": relT, "sgu_ln_g": f(sgu_ln_g), "sgu_ln_b": f(sgu_ln_b),
              "sgu_w": f(sgu_w), "sgu_b": f(sgu_b), "w_out": f(w_out), "router_w": f(router_w), "router_b": f(router_b),
              "w_gate": f(w_gate), "w_up": f(w_up), "w_down": f(w_down)}
    in_maps = []
    for core in range(N_CORES):
        b, half = divmod(core, 2)
        if half == 0:
            xin = np.concatenate([np.zeros((1024, D), np.float32), x[b, 0:1024]], axis=0)
        else:
            xin = x[b]
        hvv = np.full((128, 1), float(half), np.float32)
        in_maps.append({"xin": np.ascontiguousarray(xin), "hv": hvv, **shared})
    res = run_bass_kernel_spmd(nc, in_maps, core_ids=list(range(N_CORES)))
    outp = np.empty((4, 2048, D), np.float32)
    for core in range(N_CORES):
        b, half = divmod(core, 2)
        outp[b, half * 1024:(half + 1) * 1024] = res.results[core]["out"]
    return outp
```

```python
from contextlib import ExitStack

import numpy as np
import concourse.bass as bass
import concourse.mybir as mybir
from concourse.bass_utils import run_bass_kernel_spmd

F32 = mybir.dt.float32
BF16 = mybir.dt.bfloat16
AF = mybir.ActivationFunctionType
ALU = mybir.AluOpType
AX = mybir.AxisListType

ENGS = ("pe", "act", "dve", "pool", "sp")

D = 2048
NCH = 16
DEPTH = 2
ALPHA = (2.0 * DEPTH) ** 0.25
EPS = 1e-5
NE = 16
FF = 1024
N_CORES = 8


class Box:
    __slots__ = ("key", "val")

    def __init__(self, key, val=None):
        self.key = key
        self.val = val


class Slot:
    __slots__ = ("name", "w", "r")

    def __init__(self, name):
        self.name = name
        self.w = None
        self.r = {}


class DSem:
    def __init__(self, prog, name):
        self.name = f"{name}_{len(prog.dsems)}"
        self.count = 0
        self.key = "d:" + self.name
        prog.dsems.append(self)


class _Rec:
    def __init__(self):
        self.call = None

    def __getattr__(self, name):
        def f(*a, **k):
            self.call = (name, a, k)
        return f


class Prog:
    def __init__(self):
        self.ops = {e: [] for e in ENGS}
        self.count = {e: 0 for e in ENGS}
        self.pending = {e: [] for e in ENGS}
        self.seen = {e: {} for e in ENGS}
        self.dsems = []
        self.same_engine_sync = True

    def dsem(self, name):
        return DSem(self, name)

    def _need(self, eng, box, waits):
        if box is None:
            return
        if box.key == eng and (eng == "pe" or not self.same_engine_sync):
            return
        assert box.val is not None, f"dependency on unsignalled op ({box.key})"
        k, v = box.key, box.val
        if self.seen[eng].get(k, 0) >= v:
            return
        self.seen[eng][k] = v
        waits[k] = max(waits.get(k, 0), v)

    def _deps(self, eng, reads, writes):
        waits = {}
        for s in reads:
            self._need(eng, s.w, waits)
        for s in writes:
            self._need(eng, s.w, waits)
            for b in s.r.values():
                self._need(eng, b, waits)
        return waits

    def _mark(self, box, reads, writes):
        for s in reads:
            s.r[box.key] = box
        for s in writes:
            s.w = box
            s.r = {}

    def op(self, eng, fn, reads=(), writes=(), sig=True):
        waits = self._deps(eng, reads, writes)
        box = Box(eng)
        if sig:
            self.count[eng] += 1
            box.val = self.count[eng]
            for b in self.pending[eng]:
                b.val = box.val
            self.pending[eng] = []
        else:
            self.pending[eng].append(box)
        self._mark(box, reads, writes)
        rec = _Rec()
        fn(rec)
        assert rec.call is not None
        self.ops[eng].append(("op", rec.call, waits, sig))
        return box

    def dma(self, eng, out, in_, dsem, reads=(), writes=()):
        waits = self._deps(eng, reads, writes)
        dsem.count += 16
        box = Box(dsem.key, dsem.count)
        self._mark(box, reads, writes)
        self.ops[eng].append(("dma", (out, in_, dsem), waits, True))
        return box

    def barrier(self):
        for e in ENGS:
            assert not self.pending[e], f"barrier with unsignalled ops on {e}"
        for e in ENGS:
            waits = {}
            for k in ENGS:
                if k != e and self.count[k] > self.seen[e].get(k, 0):
                    waits[k] = self.count[k]
                    self.seen[e][k] = self.count[k]
            for d in self.dsems:
                if d.count > self.seen[e].get(d.key, 0):
                    waits[d.key] = d.count
                    self.seen[e][d.key] = d.count
            if waits:
                self.ops[e].append(("wait", None, waits, False))

    def emit(self, nc, stack):
        sems = {}
        for e in ENGS:
            sems[e] = stack.enter_context(nc.semaphore("s_" + e))
        for d in self.dsems:
            sems[d.key] = stack.enter_context(nc.semaphore("d_" + d.name))
        for e in ENGS:
            assert self.count[e] < 60000, (e, self.count[e])
            assert not self.pending[e], f"trailing unsignalled ops on {e}"
        block = stack.enter_context(nc.Block())
        hand = {"pe": block.tensor, "act": block.scalar, "dve": block.vector,
                "pool": block.gpsimd, "sp": block.sync}

        def make(e):
            def body(engine):
                for kind, fn, waits, sig in self.ops[e]:
                    for k, v in waits.items():
                        engine.wait_ge(sems[k], v)
                    if kind == "op":
                        name, a, k = fn
                        ins = getattr(engine, name)(*a, **k)
                        if sig:
                            ins.then_inc(sems[e], 1)
                    elif kind == "dma":
                        out, in_, dsem = fn
                        engine.dma_start(out=out, in_=in_).then_inc(sems[dsem.key], 16)
            return body

        for e in ENGS:
            hand[e](make(e))


class Mem:
    def __init__(self, nc, base=16512, top=229376):
        self.nc, self.p, self.top, self.n = nc, base, top, 0
        self.peak = base

    def alloc(self, shape, dt):
        esz = 4 if dt == F32 else 2
        nbytes = esz * int(np.prod(shape[1:]))
        off = self.p
        self.p += (nbytes + 63) // 64 * 64
        assert self.p <= self.top, f"SBUF overflow: {self.p} > {self.top}"
        self.peak = max(self.peak, self.p)
        self.n += 1
        return self.nc.alloc_sbuf_tensor_at(f"sb{self.n}", list(shape), dt, offset=off)

    def alloc_at(self, off, shape, dt):
        self.n += 1
        return self.nc.alloc_sbuf_tensor_at(f"sb{self.n}", list(shape), dt, offset=off)

    def mark(self):
        return self.p

    def release(self, m):
        self.p = m


class _Stop(Exception):
    pass


def build_program(stop=None):
    nc = bass.Bass("TRN2", target_bir_lowering=False)
    dram = lambda name, shape, dt=F32, kind="ExternalInput": nc.dram_tensor(name, shape, dt, kind=kind).ap()
    xin = dram("xin", [2048, D])
    hvin = dram("hv", [128, 1])
    colsin = dram("cols", [128, 160])
    w_in = dram("w_in", [DEPTH, D, 5120])
    relT = dram("relT", [DEPTH, 8, 128, 640])
    sgu_g = dram("sgu_ln_g", [DEPTH, 1024])
    sgu_bb = dram("sgu_ln_b", [DEPTH, 1024])
    sgu_w = dram("sgu_w", [DEPTH, 8, 128, 128])
    sgu_b = dram("sgu_b", [DEPTH, 8, 128])
    w_out = dram("w_out", [DEPTH, D, D])
    router_w = dram("router_w", [D, NE])
    router_b = dram("router_b", [NE])
    w_gate = dram("w_gate", [DEPTH, NE, D, FF])
    w_up = dram("w_up", [DEPTH, NE, D, FF])
    w_down = dram("w_down", [DEPTH, NE, FF, D])
    out = dram("out", [1024, D], kind="ExternalOutput")
    cbsc = dram("cbsc", [128, NCH * 512], BF16, kind="Internal")

    P = Prog()
    M = Mem(nc)
    st = ExitStack()
    dbg = dram("dbg", [128, NCH, 1024], kind="ExternalOutput") if stop else None

    def stage(name, Tq):
        if stop != name:
            return
        P.barrier()
        dd = P.dsem("dbg")
        P.dma("sp", dbg[:, :, 0:Tq], X[:, :, 0:Tq], dd, writes=[Slot("dbgo")])
        raise _Stop()

    X = M.alloc([128, NCH, 1024], F32)
    HB = M.alloc([128, NCH, 1024], BF16)
    ident = M.alloc([128, 128], F32)
    ones_f = M.alloc([128, 128], F32)
    ones_b = M.alloc([128, 128], BF16)
    sel = M.alloc([16, NE, 128], F32)
    cols = M.alloc([128, 160], F32)
    rw = M.alloc([128, NCH, NE], F32)
    rb = M.alloc([128, NE], F32)
    hv = M.alloc([128, 1], F32)
    epsc = M.alloc([128, 1], F32)
    combT = M.alloc([16, 1024], F32)
    small = M.alloc([128, 256], F32)
    LN_OFF = M.mark()
    LN_T = ([M.alloc([128, 512], F32), M.alloc([128, 512], F32)], M.alloc([128, 512], F32),
            M.alloc([128, 512], F32), M.alloc([128, 512], F32),
            [M.alloc([128, 512], F32), M.alloc([128, 512], F32)])
    LN_S = ([Slot("sq0"), Slot("sq1")], Slot("stat"), [Slot("tmp0"), Slot("tmp1")])
    ARENA = M.mark()

    PS = [st.enter_context(nc.psum_tensor(f"ps{i}", [128, 512], F32)) for i in range(8)]
    sPS = [Slot(f"ps{i}") for i in range(8)]

    sX = [[Slot(f"X{c}_{tb}") for tb in range(2)] for c in range(NCH)]
    sHB = [Slot(f"HB{tb}") for tb in range(2)]
    sConst = Slot("const")
    sCombT = Slot("combT")
    sSmall = Slot("small")

    dconst = P.dsem("const")
    dmisc = P.dsem("misc")

    CB_OFF = [None]
    GCOL = lambda k, c: cols[:, k * 16 + c: k * 16 + c + 1]

    P.dma("sp", cols[:], colsin, dconst, writes=[sConst])
    P.dma("sp", hv[:], hvin, dconst, writes=[sConst])
    P.dma("sp", rw[:], router_w.rearrange("(c p) e -> p c e", p=128), dconst, writes=[sConst])
    P.dma("sp", rb[:], router_b.partition_broadcast(128), dconst, writes=[sConst])
    P.op("pool", lambda e: e.memset(ones_f[:], 1.0), writes=[sConst])
    P.op("pool", lambda e: e.memset(ones_b[:], 1.0), writes=[sConst])
    P.op("pool", lambda e: e.memset(epsc[:], EPS), writes=[sConst])
    P.op("pool", lambda e: e.affine_select(out=ident[:], in_=ones_f[:], pattern=[[-1, 128]],
                                           compare_op=ALU.is_equal, fill=0.0, base=0, channel_multiplier=1),
         reads=[sConst], writes=[sConst])
    P.op("pool", lambda e: e.affine_select(out=sel[:], in_=ones_f[0:16, :].unsqueeze(1).to_broadcast([16, NE, 128]),
                                           pattern=[[-1, NE], [0, 128]], compare_op=ALU.is_equal, fill=0.0,
                                           base=0, channel_multiplier=1),
         reads=[sConst], writes=[sConst])
    P.barrier()

    def ln_fm(src, nch, Tq, gk, bk, out_f32, out_bf, s_src, s_outb, psA, psB, tmps):
        sq, mean, rstd, nmr, tmp = tmps
        s_sq, s_stat, s_tmp = LN_S
        inv = 1.0 / (nch * 128)
        for tb in range(Tq // 512):
            blk = slice(tb * 512, tb * 512 + 512)
            for c in range(nch):
                P.op("act", lambda e, c=c: e.activation(out=sq[c % 2][:], in_=src[:, c, blk], func=AF.Square),
                     reads=[s_src[c][tb]], writes=[s_sq[c % 2]])
                P.op("pe", lambda e, c=c: e.matmul(PS[psA][:], lhsT=ones_f[:], rhs=src[:, c, blk],
                                                   start=(c == 0), stop=(c == nch - 1)),
                     reads=[s_src[c][tb]], writes=[sPS[psA]], sig=(c == nch - 1))
                P.op("pe", lambda e, c=c: e.matmul(PS[psB][:], lhsT=ones_f[:], rhs=sq[c % 2][:],
                                                   start=(c == 0), stop=(c == nch - 1)),
                     reads=[s_sq[c % 2]], writes=[sPS[psB]], sig=True)
            P.op("dve", lambda e: e.tensor_scalar(out=mean[:], in0=PS[psA][:], scalar1=inv, scalar2=None, op0=ALU.mult),
                 reads=[sPS[psA]], writes=[s_stat])
            P.op("dve", lambda e: e.tensor_tensor(out=nmr[:], in0=mean[:], in1=mean[:], op=ALU.mult),
                 reads=[s_stat], writes=[s_stat])
            P.op("dve", lambda e: e.scalar_tensor_tensor(out=rstd[:], in0=PS[psB][:], scalar=inv, in1=nmr[:],
                                                         op0=ALU.mult, op1=ALU.subtract),
                 reads=[sPS[psB], s_stat], writes=[s_stat])
            P.op("act", lambda e: e.activation(out=rstd[:], in_=rstd[:], func=AF.Sqrt, bias=epsc[:], scale=1.0),
                 reads=[s_stat], writes=[s_stat])
            P.op("dve", lambda e: e.reciprocal(out=rstd[:], in_=rstd[:]), reads=[s_stat], writes=[s_stat])
            P.op("dve", lambda e: e.scalar_tensor_tensor(out=nmr[:], in0=mean[:], scalar=-1.0, in1=rstd[:],
                                                         op0=ALU.mult, op1=ALU.mult),
                 reads=[s_stat], writes=[s_stat])
            for c in range(nch):
                t = tmp[c % 2]
                P.op("dve", lambda e, c=c, t=t: e.tensor_tensor(out=t[:], in0=src[:, c, blk], in1=rstd[:], op=ALU.mult),
                     reads=[s_src[c][tb], s_stat], writes=[s_tmp[c % 2]])
                P.op("dve", lambda e, t=t: e.tensor_tensor(out=t[:], in0=t[:], in1=nmr[:], op=ALU.add),
                     reads=[s_stat], writes=[s_tmp[c % 2]])
                if out_f32 is not None:
                    P.op("act", lambda e, c=c, t=t: e.activation(out=out_f32[:, c, blk], in_=t[:], func=AF.Identity,
                                                                 scale=GCOL(gk, c), bias=GCOL(bk, c)),
                         reads=[s_tmp[c % 2]], writes=[s_src[c][tb]])
                P.op("act", lambda e, c=c, t=t: e.activation(out=out_bf[:, c, blk], in_=t[:], func=AF.Identity,
                                                             scale=GCOL(gk, c), bias=GCOL(bk, c)),
                     reads=[s_tmp[c % 2]], writes=[s_outb[tb]])

    def ln_tmps():
        return LN_T

    def ln_in(row0, ntile, dest_f32, dest_bf, tile0, s_dst_f, s_dst_b):
        xt = [M.alloc([128, D], F32), M.alloc([128, D], F32)]
        junk = M.alloc([128, D], BF16)
        st_ = M.alloc([128, 8], F32)
        s_xt = [Slot("xt0"), Slot("xt1")]
        s_junk, s_st = Slot("junk"), Slot("st")
        dx = [P.dsem(f"x{row0}_0"), P.dsem(f"x{row0}_1")]
        for i in range(ntile):
            b = i % 2
            x_t = xt[b]
            P.dma("sp", x_t[:], xin[row0 + i * 128: row0 + (i + 1) * 128, :], dx[b], writes=[s_xt[b]])
            P.op("dve", lambda e, x_t=x_t: e.tensor_reduce(out=st_[:, 0:1], in_=x_t[:], axis=AX.X, op=ALU.add),
                 reads=[s_xt[b]], writes=[s_st])
            P.op("act", lambda e, x_t=x_t: e.activation(out=junk[:], in_=x_t[:], func=AF.Square, accum_out=st_[:, 1:2]),
                 reads=[s_xt[b], s_st], writes=[s_junk, s_st])
            P.op("dve", lambda e: e.tensor_scalar(out=st_[:, 2:3], in0=st_[:, 0:1], scalar1=1.0 / D, scalar2=None, op0=ALU.mult),
                 reads=[s_st], writes=[s_st])
            P.op("dve", lambda e: e.tensor_tensor(out=st_[:, 3:4], in0=st_[:, 2:3], in1=st_[:, 2:3], op=ALU.mult),
                 reads=[s_st], writes=[s_st])
            P.op("dve", lambda e: e.scalar_tensor_tensor(out=st_[:, 4:5], in0=st_[:, 1:2], scalar=1.0 / D, in1=st_[:, 3:4],
                                                         op0=ALU.mult, op1=ALU.subtract),
                 reads=[s_st], writes=[s_st])
            P.op("act", lambda e: e.activation(out=st_[:, 5:6], in_=st_[:, 4:5], func=AF.Sqrt, bias=epsc[:], scale=1.0),
                 reads=[s_st], writes=[s_st])
            P.op("dve", lambda e: e.reciprocal(out=st_[:, 6:7], in_=st_[:, 5:6]), reads=[s_st], writes=[s_st])
            P.op("dve", lambda e, x_t=x_t: e.tensor_scalar(out=x_t[:], in0=x_t[:], scalar1=st_[:, 2:3], scalar2=st_[:, 6:7],
                                                           op0=ALU.subtract, op1=ALU.mult),
                 reads=[s_st], writes=[s_xt[b]])
            tcol = slice((tile0 + i) * 128, (tile0 + i + 1) * 128)
            tb = (tile0 + i) // 4
            for c4 in range(4):
                bank = c4 % 2
                for cc in range(4):
                    c = c4 * 4 + cc
                    P.op("pe", lambda e, c=c, cc=cc, bank=bank, x_t=x_t: e.transpose(
                        PS[bank][:, cc * 128:(cc + 1) * 128], x_t[:, c * 128:(c + 1) * 128], ident[:]),
                        reads=[s_xt[b]], writes=[sPS[bank]])
                for cc in range(4):
                    c = c4 * 4 + cc
                    if dest_f32 is not None:
                        P.op("act", lambda e, c=c, cc=cc, bank=bank: e.activation(
                            out=dest_f32[:, c, tcol], in_=PS[bank][:, cc * 128:(cc + 1) * 128], func=AF.Identity,
                            scale=GCOL(0, c), bias=GCOL(1, c)),
                            reads=[sPS[bank]], writes=[s_dst_f[c][tb]])
                    P.op("act", lambda e, c=c, cc=cc, bank=bank: e.activation(
                        out=dest_bf[:, c, tcol], in_=PS[bank][:, cc * 128:(cc + 1) * 128], func=AF.Identity,
                        scale=GCOL(0, c), bias=GCOL(1, c)),
                        reads=[sPS[bank]], writes=[s_dst_b[tb]])

    def mixer(l, Tq, CB, sCB, tag):
        nblk = Tq // 512
        ntq = Tq // 128
        ntk = ntq + 4
        m0 = M.mark()
        ymix = M.alloc([128, 8, Tq], BF16)
        s_ymix = [[Slot(f"ymix{c}_{tb}") for tb in range(nblk)] for c in range(8)]
        wq = [M.alloc([128, NCH, 128], BF16) for _ in range(3)]
        s_wq = [Slot(f"wq{i}") for i in range(3)]
        d_wq = [P.dsem(f"wq{tag}{i}") for i in range(3)]
        ring = [0]

        def load_cols(c0):
            i = ring[0] % 3
            ring[0] += 1
            P.dma("pool", wq[i][:], w_in[l, :, c0:c0 + 128].rearrange("(c p) n -> p c n", p=128), d_wq[i],
                  writes=[s_wq[i]])
            return wq[i], s_wq[i]

        def tok_src(tbk):
            if tbk == 0:
                return (lambda c: CB[:, c, :]), sCB
            return (lambda c, tbk=tbk: HB[:, c, (tbk - 1) * 512: tbk * 512]), sHB[tbk - 1]

        def tok_tile(j):
            if j < 4:
                return (lambda c: CB[:, c, j * 128:(j + 1) * 128]), sCB
            return (lambda c: HB[:, c, (j - 4) * 128:(j - 3) * 128]), sHB[(j - 4) // 4]

        m1 = M.mark()
        E = M.alloc([128, 8, 640], BF16)
        etmp = [M.alloc([128, 640], F32), M.alloc([128, 640], F32)]
        s_E, s_etmp = Slot("E"), [Slot("et0"), Slot("et1")]
        d_et = [P.dsem(f"et{tag}0"), P.dsem(f"et{tag}1")]
        kT = [M.alloc([128, 512 + Tq], BF16) for _ in range(2)]
        qT = [M.alloc([128, Tq], BF16) for _ in range(2)]
        Vh = [M.alloc([128, ntk, 128], BF16) for _ in range(2)]
        Pt = [M.alloc([128, 640], BF16) for _ in range(2)]
        rec = [M.alloc([128, 128], F32) for _ in range(2)]
        s_kT, s_qT, s_Vh = [Slot("kT0"), Slot("kT1")], [Slot("qT0"), Slot("qT1")], [Slot("Vh0"), Slot("Vh1")]
        s_Pt, s_rec = [Slot("Pt0"), Slot("Pt1")], [Slot("rec0"), Slot("rec1")]
        for h in range(8):
            b = h % 2
            P.dma("sp", etmp[b][:], relT[l, h], d_et[b], writes=[s_etmp[b]])
            P.op("act", lambda e, h=h, b=b: e.activation(out=E[:, h, :], in_=etmp[b][:], func=AF.Exp),
                 reads=[s_etmp[b]], writes=[s_E])
            P.op("pool", lambda e, h=h: e.memset(E[0:64, h, 64:128], 0.0), writes=[s_E])
            P.op("pool", lambda e, h=h: e.memset(E[64:128, h, 512:576], 0.0), writes=[s_E])
        scale = 1.0 / float(np.sqrt(128.0))
        pending = [load_cols(0), load_cols(1024), load_cols(2048)]
        for h in range(8):
            b = h % 2
            (wqh, s_wqh), (wkh, s_wkh), (wvh, s_wvh) = pending
            for tbk in range(nblk + 1):
                src, s_src = tok_src(tbk)
                bank = tbk % 2
                for c in range(NCH):
                    P.op("pe", lambda e, c=c, src=src, bank=bank, wkh=wkh: e.matmul(
                        PS[bank][:], lhsT=wkh[:, c, :], rhs=src(c), start=(c == 0), stop=(c == NCH - 1)),
                        reads=[s_wkh, s_src], writes=[sPS[bank]], sig=(c == NCH - 1))
                P.op("act", lambda e, b=b, bank=bank, tbk=tbk: e.copy(out=kT[b][:, tbk * 512:(tbk + 1) * 512], in_=PS[bank][:]),
                     reads=[sPS[bank]], writes=[s_kT[b]])
            for tb in range(nblk):
                src, s_src = tok_src(tb + 1)
                bank = 2 + tb % 2
                for c in range(NCH):
                    P.op("pe", lambda e, c=c, src=src, bank=bank, wqh=wqh: e.matmul(
                        PS[bank][:], lhsT=wqh[:, c, :], rhs=src(c), start=(c == 0), stop=(c == NCH - 1)),
                        reads=[s_wqh, s_src], writes=[sPS[bank]], sig=(c == NCH - 1))
                P.op("act", lambda e, b=b, bank=bank, tb=tb: e.copy(out=qT[b][:, tb * 512:(tb + 1) * 512], in_=PS[bank][:]),
                     reads=[sPS[bank]], writes=[s_qT[b]])
            for j4 in range(ntk // 4):
                bank = 4 + j4 % 2
                for jj in range(4):
                    j = j4 * 4 + jj
                    src, s_src = tok_tile(j)
                    for c in range(NCH):
                        P.op("pe", lambda e, c=c, src=src, bank=bank, jj=jj, wvh=wvh: e.matmul(
                            PS[bank][:, jj * 128:(jj + 1) * 128], lhsT=src(c), rhs=wvh[:, c, :],
                            start=(c == 0), stop=(c == NCH - 1)),
                            reads=[s_wvh, s_src], writes=[sPS[bank]], sig=(c == NCH - 1))
                P.op("dve", lambda e, b=b, bank=bank, j4=j4: e.tensor_copy(
                    out=Vh[b][:, j4 * 4:(j4 + 1) * 4, :], in_=PS[bank][:].rearrange("p (j d) -> p j d", d=128)),
                    reads=[sPS[bank]], writes=[s_Vh[b]])
            if h < 7:
                pending = [load_cols((h + 1) * 128), load_cols(1024 + (h + 1) * 128), load_cols(2048 + (h + 1) * 128)]
            def scores(i):
                pb = i % 2
                sa, sb_ = (6, 7) if i % 2 == 0 else (0, 1)
                for j in range(5):
                    dst = PS[sa][:, j * 128:(j + 1) * 128] if j < 4 else PS[sb_][:, 0:128]
                    P.op("pe", lambda e: e.matmul(
                        dst, lhsT=kT[b][:, (i + j) * 128:(i + j + 1) * 128], rhs=qT[b][:, i * 128:(i + 1) * 128],
                        start=True, stop=True),
                        reads=[s_kT[b], s_qT[b]], writes=[sPS[sa] if j < 4 else sPS[sb_]], sig=(j >= 3))
                P.op("act", lambda e: e.activation(out=Pt[pb][:, 0:512], in_=PS[sa][:], func=AF.Exp, scale=scale),
                     reads=[sPS[sa]], writes=[s_Pt[pb]])
                P.op("act", lambda e: e.activation(out=Pt[pb][:, 512:640], in_=PS[sb_][:, 0:128], func=AF.Exp, scale=scale),
                     reads=[sPS[sb_]], writes=[s_Pt[pb]])
                nctx = max(0, 4 - i)
                if nctx > 0:
                    P.op("dve", lambda e: e.scalar_tensor_tensor(
                        out=Pt[pb][:, 0:nctx * 128], in0=Pt[pb][:, 0:nctx * 128], scalar=hv[:, 0:1],
                        in1=E[:, h, 0:nctx * 128], op0=ALU.mult, op1=ALU.mult),
                        reads=[s_E, sConst], writes=[s_Pt[pb]])
                P.op("pool", lambda e: e.tensor_tensor(
                    out=Pt[pb][:, nctx * 128:640], in0=Pt[pb][:, nctx * 128:640], in1=E[:, h, nctx * 128:640], op=ALU.mult),
                    reads=[s_E], writes=[s_Pt[pb]])

            def pv(i):
                pb = i % 2
                od = 2 + (i % 2)
                for j in range(5):
                    P.op("pe", lambda e: e.matmul(
                        PS[od][:, 0:128], lhsT=Vh[b][:, i + j, :], rhs=Pt[pb][:, j * 128:(j + 1) * 128],
                        start=(j == 0), stop=(j == 4)),
                        reads=[s_Vh[b], s_Pt[pb]], writes=[sPS[od]], sig=False)
                for j in range(5):
                    P.op("pe", lambda e: e.matmul(
                        PS[od][:, 128:256], lhsT=ones_b[:], rhs=Pt[pb][:, j * 128:(j + 1) * 128],
                        start=(j == 0), stop=(j == 4)),
                        reads=[s_Pt[pb], sConst], writes=[sPS[od]], sig=(j == 4))
                P.op("dve", lambda e: e.reciprocal(out=rec[pb][:], in_=PS[od][:, 128:256]),
                     reads=[sPS[od]], writes=[s_rec[pb]])
                P.op("dve", lambda e: e.tensor_tensor(
                    out=ymix[:, h, i * 128:(i + 1) * 128], in0=PS[od][:, 0:128], in1=rec[pb][:], op=ALU.mult),
                    reads=[sPS[od], s_rec[pb]], writes=[s_ymix[h][i // 4]])

            for i in range(ntq + 1):
                if i < ntq:
                    scores(i)
                if i >= 1:
                    pv(i - 1)
        P.barrier()
        M.release(m1)

        def out_proj(row0, first):
            wo = [M.alloc([128, 8, 128], BF16) for _ in range(3)]
            s_wo = [Slot(f"wo{i}") for i in range(3)]
            d_wo = [P.dsem(f"wo{tag}{row0}_{i}") for i in range(3)]

            def load(db):
                i = db % 3
                P.dma("pool", wo[i][:], w_out[l, row0:row0 + 1024, db * 128:(db + 1) * 128].rearrange("(c p) n -> p c n", p=128),
                      d_wo[i], writes=[s_wo[i]])
            load(0); load(1)
            for db in range(NCH):
                if db + 2 < NCH:
                    load(db + 2)
                i = db % 3
                for tb in range(nblk):
                    bank = (db * nblk + tb) % 4
                    blk = slice(tb * 512, tb * 512 + 512)
                    for c in range(8):
                        P.op("pe", lambda e, c=c, i=i, bank=bank, blk=blk: e.matmul(
                            PS[bank][:], lhsT=wo[i][:, c, :], rhs=ymix[:, c, blk], start=(c == 0), stop=(c == 7)),
                            reads=[s_wo[i], s_ymix[c][tb]], writes=[sPS[bank]], sig=(c == 7))
                    if first:
                        P.op("dve", lambda e, db=db, bank=bank, blk=blk: e.scalar_tensor_tensor(
                            out=X[:, db, blk], in0=X[:, db, blk], scalar=ALPHA, in1=PS[bank][:], op0=ALU.mult, op1=ALU.add),
                            reads=[sPS[bank]], writes=[sX[db][tb]])
                    else:
                        P.op("dve", lambda e, db=db, bank=bank, blk=blk: e.tensor_tensor(
                            out=X[:, db, blk], in0=X[:, db, blk], in1=PS[bank][:], op=ALU.add),
                            reads=[sPS[bank]], writes=[sX[db][tb]])

        m2 = M.mark()
        out_proj(0, True)
        P.barrier()
        M.release(m2)

        gbc = M.alloc([128, 1024], F32)
        bbc = M.alloc([128, 1024], F32)
        bsb = M.alloc([128, 8, 128], F32)
        WmT = M.alloc([128, 8, 128], BF16)
        vln = M.alloc_at(CB_OFF[0], [128, ntq, 1024], BF16)
        wvs = M.alloc([128, NCH, 256], BF16)
        vtmp_off = M.mark()
        vtmp = M.alloc([128, 1024], F32)
        wtmp = M.alloc_at(vtmp_off, [128, 8, 128], F32)
        junk = M.alloc([128, 1024], BF16)
        stt = M.alloc([128, ntq, 12], F32)
        ug = [M.alloc_at(LN_OFF + k * 4096, [128, Tq], F32) for k in range(2)]
        mtmp = [M.alloc([128, 512], F32) for _ in range(2)]
        s_g, s_wtmp, s_WmT, s_wvs, s_junk = Slot("g"), Slot("wtmp"), Slot("WmT"), Slot("wvs"), Slot("junk")
        s_vtmp = s_wtmp
        s_vln = [Slot(f"vln{i}") for i in range(ntq)]
        s_stt = [Slot(f"stt{i}") for i in range(ntq)]
        s_ug, s_mtmp = [Slot("ug0"), Slot("ug1")], [Slot("mt0"), Slot("mt1")]
        d_g, d_wvs = P.dsem(f"sg{tag}"), P.dsem(f"wvs{tag}")
        P.dma("sp", gbc[:], sgu_g[l].partition_broadcast(128), d_g, writes=[s_g])
        P.dma("sp", bbc[:], sgu_bb[l].partition_broadcast(128), d_g, writes=[s_g])
        P.dma("sp", bsb[:].rearrange("p g t -> p (g t)"), sgu_b[l].rearrange("g t -> (g t)").partition_broadcast(128), d_g, writes=[s_g])
        P.dma("sp", wtmp[:], sgu_w[l].rearrange("g t s -> t g s"), P.dsem(f"wt{tag}"), writes=[s_wtmp])
        for g in range(8):
            P.op("pool", lambda e, g=g: e.affine_select(out=wtmp[:, g, :], in_=wtmp[:, g, :], pattern=[[-1, 128]],
                                                        compare_op=ALU.is_ge, fill=0.0, base=0, channel_multiplier=1),
                 reads=[s_g], writes=[s_wtmp])
        for g4 in range(2):
            for gg in range(4):
                g = g4 * 4 + gg
                P.op("pe", lambda e, g=g, gg=gg, g4=g4: e.transpose(PS[g4][:, gg * 128:(gg + 1) * 128], wtmp[:, g, :], ident[:]),
                     reads=[s_wtmp, sConst], writes=[sPS[g4]])
            P.op("act", lambda e, g4=g4: e.copy(out=WmT[:, g4 * 4:(g4 + 1) * 4, :], in_=PS[g4][:].rearrange("p (g t) -> p g t", t=128)),
                 reads=[sPS[g4]], writes=[s_WmT])
        for qt in range(4):
            P.dma("pool", wvs[:], w_in[l, :, 4096 + qt * 256: 4096 + (qt + 1) * 256].rearrange("(c p) n -> p c n", p=128),
                  d_wvs, writes=[s_wvs])
            for i in range(ntq):
                bank = 2 + i % 4
                for c in range(NCH):
                    P.op("pe", lambda e, c=c, i=i, bank=bank: e.matmul(
                        PS[bank][:, 0:256], lhsT=HB[:, c, i * 128:(i + 1) * 128], rhs=wvs[:, c, :], start=(c == 0), stop=(c == NCH - 1)),
                        reads=[s_wvs, sHB[i // 4]], writes=[sPS[bank]], sig=(c == NCH - 1))
                P.op("act", lambda e, i=i, bank=bank, qt=qt: e.activation(
                    out=vln[:, i, qt * 256:(qt + 1) * 256], in_=PS[bank][:, 0:256], func=AF.Gelu, accum_out=stt[:, i, qt:qt + 1]),
                    reads=[sPS[bank]], writes=[s_vln[i], s_stt[i]])
        for i in range(ntq):
            S_ = lambda k, i=i: stt[:, i, k:k + 1]
            P.op("act", lambda e, i=i: e.activation(out=junk[:], in_=vln[:, i, :], func=AF.Square, accum_out=stt[:, i, 4:5]),
                 reads=[s_vln[i]], writes=[s_junk, s_stt[i]])
            P.op("dve", lambda e, S_=S_, i=i: e.tensor_reduce(out=S_(5), in_=stt[:, i, 0:4], axis=AX.X, op=ALU.add), reads=[s_stt[i]], writes=[s_stt[i]])
            P.op("dve", lambda e, S_=S_: e.tensor_scalar(out=S_(5), in0=S_(5), scalar1=1.0 / 1024, scalar2=None, op0=ALU.mult),
                 reads=[s_stt[i]], writes=[s_stt[i]])
            P.op("dve", lambda e, S_=S_: e.tensor_tensor(out=S_(6), in0=S_(5), in1=S_(5), op=ALU.mult), reads=[s_stt[i]], writes=[s_stt[i]])
            P.op("dve", lambda e, S_=S_: e.scalar_tensor_tensor(out=S_(7), in0=S_(4), scalar=1.0 / 1024, in1=S_(6),
                                                                op0=ALU.mult, op1=ALU.subtract), reads=[s_stt[i]], writes=[s_stt[i]])
            P.op("act", lambda e, S_=S_: e.activation(out=S_(8), in_=S_(7), func=AF.Sqrt, bias=epsc[:], scale=1.0),
                 reads=[s_stt[i]], writes=[s_stt[i]])
            P.op("dve", lambda e, S_=S_: e.reciprocal(out=S_(9), in_=S_(8)), reads=[s_stt[i]], writes=[s_stt[i]])
            P.op("dve", lambda e, S_=S_, i=i: e.tensor_scalar(out=vtmp[:], in0=vln[:, i, :], scalar1=S_(5), scalar2=S_(9),
                                                              op0=ALU.subtract, op1=ALU.mult),
                 reads=[s_vln[i], s_stt[i]], writes=[s_vtmp])
            P.op("dve", lambda e: e.tensor_tensor(out=vtmp[:], in0=vtmp[:], in1=gbc[:], op=ALU.mult), reads=[s_g], writes=[s_vtmp])
            P.op("dve", lambda e, i=i: e.tensor_tensor(out=vln[:, i, :], in0=vtmp[:], in1=bbc[:], op=ALU.add),
                 reads=[s_g, s_vtmp], writes=[s_vln[i]])
        pend = load_cols(3072)
        for g in range(8):
            wug, s_wug = pend
            if g < 7:
                pend = load_cols(3072 + (g + 1) * 128)
            ub = g % 2
            for tb in range(nblk):
                bank = tb % 2
                blk = slice(tb * 512, tb * 512 + 512)
                for c in range(NCH):
                    P.op("pe", lambda e, c=c, bank=bank, blk=blk, wug=wug: e.matmul(
                        PS[bank][:], lhsT=wug[:, c, :], rhs=HB[:, c, blk], start=(c == 0), stop=(c == NCH - 1)),
                        reads=[s_wug, sHB[tb]], writes=[sPS[bank]], sig=(c == NCH - 1))
                P.op("act", lambda e, ub=ub, bank=bank, blk=blk: e.activation(out=ug[ub][:, blk], in_=PS[bank][:], func=AF.Gelu),
                     reads=[sPS[bank]], writes=[s_ug[ub]])
            for tb in range(nblk):
                bank = 6 + tb % 2
                blk = slice(tb * 512, tb * 512 + 512)
                for ii in range(4):
                    i = tb * 4 + ii
                    P.op("pe", lambda e, g=g, i=i, ii=ii, bank=bank: e.matmul(
                        PS[bank][:, ii * 128:(ii + 1) * 128], lhsT=vln[:, i, g * 128:(g + 1) * 128], rhs=WmT[:, g, :],
                        start=True, stop=True),
                        reads=[s_vln[i], s_WmT], writes=[sPS[bank]], sig=(ii == 3))
                mt = mtmp[tb % 2]
                P.op("dve", lambda e, g=g, bank=bank, mt=mt: e.tensor_tensor(
                    out=mt[:].rearrange("p (r t) -> p r t", t=128), in0=PS[bank][:].rearrange("p (r t) -> p r t", t=128),
                    in1=bsb[:, g, :].unsqueeze(1).to_broadcast([128, 4, 128]), op=ALU.add),
                    reads=[sPS[bank], s_g], writes=[s_mtmp[tb % 2]])
                P.op("dve", lambda e, g=g, ub=ub, blk=blk, mt=mt: e.tensor_tensor(
                    out=ymix[:, g, blk], in0=mt[:], in1=ug[ub][:, blk], op=ALU.mult),
                    reads=[s_mtmp[tb % 2], s_ug[ub]], writes=[s_ymix[g][tb]])
        P.barrier()
        M.release(m2)
        out_proj(1024, False)
        P.barrier()
        M.release(m0)

    def router(Tq):
        ntq = Tq // 128
        sm = lambda a, b: small[:, a:b]
        for i in range(ntq):
            tcol = slice(i * 128, (i + 1) * 128)
            tb = i // 4
            for c in range(NCH):
                P.op("pe", lambda e, c=c, tcol=tcol: e.matmul(PS[0][:, 0:NE], lhsT=X[:, c, tcol], rhs=rw[:, c, :],
                                                             start=(c == 0), stop=(c == NCH - 1)),
                     reads=[sX[c][tb], sConst], writes=[sPS[0]], sig=(c == NCH - 1))
            R, W = [sSmall], [sSmall]
            lg, ex, pr, gm, m, eq, m2, sl, cb = (sm(0, 16), sm(16, 32), sm(32, 48), sm(48, 52), sm(64, 80), sm(80, 96),
                                                sm(96, 112), sm(112, 128), sm(128, 144))
            pairs, gs = sm(160, 184), sm(184, 188)
            c1 = lambda k: small[:, 200 + k: 201 + k]
            P.op("dve", lambda e: e.tensor_tensor(out=lg, in0=PS[0][:, 0:NE], in1=rb[:], op=ALU.add), reads=[sPS[0], sConst], writes=W)
            P.op("dve", lambda e: e.tensor_reduce(out=c1(0), in_=lg, axis=AX.X, op=ALU.max), reads=R, writes=W)
            P.op("dve", lambda e: e.tensor_scalar(out=c1(1), in0=c1(0), scalar1=-1.0, scalar2=None, op0=ALU.mult), reads=R, writes=W)
            P.op("act", lambda e: e.activation(out=ex, in_=lg, func=AF.Exp, bias=c1(1), scale=1.0, accum_out=c1(2)), reads=R, writes=W)
            P.op("dve", lambda e: e.reciprocal(out=c1(3), in_=c1(2)), reads=R, writes=W)
            P.op("dve", lambda e: e.tensor_scalar(out=pr, in0=ex, scalar1=c1(3), scalar2=None, op0=ALU.mult), reads=R, writes=W)
            pv = pr.rearrange("p (g k) -> p g k", k=4)
            pw = pairs.rearrange("p (g k) -> p g k", k=6)
            for idx, (a, b_) in enumerate([(0, 1), (0, 2), (0, 3), (1, 2), (1, 3), (2, 3)]):
                P.op("dve", lambda e, idx=idx, a=a, b_=b_: e.tensor_tensor(out=pw[:, :, idx:idx + 1], in0=pv[:, :, a:a + 1],
                                                                             in1=pv[:, :, b_:b_ + 1], op=ALU.add), reads=R, writes=W)
            P.op("dve", lambda e: e.tensor_reduce(out=gs, in_=pw, axis=AX.X, op=ALU.max), reads=R, writes=W)
            P.op("dve", lambda e: e.tensor_reduce(out=c1(4), in_=gs, axis=AX.X, op=ALU.max), reads=R, writes=W)
            P.op("dve", lambda e: e.tensor_scalar(out=gm, in0=gs, scalar1=c1(4), scalar2=None, op0=ALU.is_ge), reads=R, writes=W)
            mv = m.rearrange("p (g k) -> p g k", k=4)
            P.op("dve", lambda e: e.tensor_tensor(out=mv, in0=pv, in1=gm.unsqueeze(2).to_broadcast([128, 4, 4]), op=ALU.mult), reads=R, writes=W)
            P.op("dve", lambda e: e.tensor_scalar(out=gs, in0=gm, scalar1=-1.0, scalar2=None, op0=ALU.add), reads=R, writes=W)
            P.op("dve", lambda e: e.tensor_tensor(out=mv, in0=mv, in1=gs.unsqueeze(2).to_broadcast([128, 4, 4]), op=ALU.add), reads=R, writes=W)
            P.op("dve", lambda e: e.tensor_reduce(out=c1(5), in_=m, axis=AX.X, op=ALU.max), reads=R, writes=W)
            P.op("dve", lambda e: e.tensor_scalar(out=eq, in0=m, scalar1=c1(5), scalar2=None, op0=ALU.is_equal), reads=R, writes=W)
            P.op("dve", lambda e: e.scalar_tensor_tensor(out=m2, in0=eq, scalar=-2.0, in1=m, op0=ALU.mult, op1=ALU.add), reads=R, writes=W)
            P.op("dve", lambda e: e.tensor_reduce(out=c1(6), in_=m2, axis=AX.X, op=ALU.max), reads=R, writes=W)
            P.op("dve", lambda e: e.tensor_scalar(out=sl, in0=m, scalar1=c1(6), scalar2=None, op0=ALU.is_ge), reads=R, writes=W)
            P.op("dve", lambda e: e.tensor_tensor(out=c1(7), in0=c1(5), in1=c1(6), op=ALU.add), reads=R, writes=W)
            P.op("dve", lambda e: e.reciprocal(out=c1(8), in_=c1(7)), reads=R, writes=W)
            P.op("dve", lambda e: e.scalar_tensor_tensor(out=cb, in0=m, scalar=c1(8), in1=sl, op0=ALU.mult, op1=ALU.mult), reads=R, writes=W)
            P.op("pe", lambda e: e.transpose(PS[1][0:NE, 0:128], cb, ident[:]), reads=R + [sConst], writes=[sPS[1]])
            P.op("act", lambda e, tcol=tcol: e.copy(out=combT[:, tcol], in_=PS[1][0:NE, 0:128]), reads=[sPS[1]], writes=[sCombT])

    def moe(l, Tq, tag):
        nblk = Tq // 512
        m0 = M.mark()
        hid = M.alloc_at(CB_OFF[0], [128, 8, 1024], BF16)
        s_hid = [Slot(f"hid{tb}") for tb in range(nblk)]
        wg = [M.alloc([128, NCH, 256], BF16) for _ in range(2)]
        wu = [M.alloc([128, NCH, 256], BF16) for _ in range(2)]
        wd = [M.alloc([128, 8, 512], BF16) for _ in range(2)]
        s_wg, s_wu, s_wd = [Slot("wg0"), Slot("wg1")], [Slot("wu0"), Slot("wu1")], [Slot("wd0"), Slot("wd1")]
        d_wg = [P.dsem(f"wg{tag}{i}") for i in range(2)]
        d_wu = [P.dsem(f"wu{tag}{i}") for i in range(2)]
        d_wd = [P.dsem(f"wd{tag}{i}") for i in range(2)]
        ssb = [M.alloc([128, 512], F32) for _ in range(2)]
        tsb = [M.alloc([128, 512], F32) for _ in range(2)]
        s_ssb, s_tsb = [Slot("ssb0"), Slot("ssb1")], [Slot("tsb0"), Slot("tsb1")]

        def load_gu(n):
            e_, fb = divmod(n, 4)
            i = n % 2
            P.dma("pool", wg[i][:], w_gate[l, e_, :, fb * 256:(fb + 1) * 256].rearrange("(c p) n -> p c n", p=128), d_wg[i], writes=[s_wg[i]])
            P.dma("pool", wu[i][:], w_up[l, e_, :, fb * 256:(fb + 1) * 256].rearrange("(c p) n -> p c n", p=128), d_wu[i], writes=[s_wu[i]])

        def load_d(n):
            e_, dq = divmod(n, 4)
            i = n % 2
            P.dma("pool", wd[i][:], w_down[l, e_, :, dq * 512:(dq + 1) * 512].rearrange("(c p) n -> p c n", p=128), d_wd[i], writes=[s_wd[i]])

        load_gu(0); load_gu(1); load_d(0); load_d(1)
        yield
        cnt = 0
        for e_ in range(NE):
            for tb in range(nblk):
                P.op("pe", lambda e, e_=e_, tb=tb: e.matmul(PS[4 + tb][:], lhsT=sel[:, e_, :], rhs=combT[:, tb * 512:(tb + 1) * 512],
                                                           start=True, stop=True),
                     reads=[sCombT, sConst], writes=[sPS[4 + tb]])
            for fb in range(4):
                n = e_ * 4 + fb
                i = n % 2
                for tb in range(nblk):
                    blk = slice(tb * 512, tb * 512 + 512)
                    for fs in range(2):
                        f128 = fb * 2 + fs
                        gb, ub = cnt % 2, 2 + cnt % 2
                        cnt += 1
                        for c in range(NCH):
                            P.op("pe", lambda e, c=c, i=i, fs=fs, gb=gb, blk=blk: e.matmul(
                                PS[gb][:], lhsT=wg[i][:, c, fs * 128:(fs + 1) * 128], rhs=HB[:, c, blk], start=(c == 0), stop=(c == NCH - 1)),
                                reads=[s_wg[i], sHB[tb]], writes=[sPS[gb]], sig=(c == NCH - 1))
                        for c in range(NCH):
                            P.op("pe", lambda e, c=c, i=i, fs=fs, ub=ub, blk=blk: e.matmul(
                                PS[ub][:], lhsT=wu[i][:, c, fs * 128:(fs + 1) * 128], rhs=HB[:, c, blk], start=(c == 0), stop=(c == NCH - 1)),
                                reads=[s_wu[i], sHB[tb]], writes=[sPS[ub]], sig=(c == NCH - 1))
                        k = cnt % 2
                        P.op("act", lambda e, k=k, gb=gb: e.activation(out=ssb[k][:], in_=PS[gb][:], func=AF.Silu),
                             reads=[sPS[gb]], writes=[s_ssb[k]])
                        P.op("dve", lambda e, k=k, ub=ub: e.tensor_tensor(out=tsb[k][:], in0=ssb[k][:], in1=PS[ub][:], op=ALU.mult),
                             reads=[s_ssb[k], sPS[ub]], writes=[s_tsb[k]])
                        P.op("dve", lambda e, k=k, tb=tb, f128=f128, blk=blk: e.tensor_tensor(
                            out=hid[:, f128, blk], in0=tsb[k][:], in1=PS[4 + tb][:], op=ALU.mult),
                            reads=[s_tsb[k], sPS[4 + tb]], writes=[s_hid[tb]])
                if n + 2 < NE * 4:
                    load_gu(n + 2)
            for dq in range(4):
                n = e_ * 4 + dq
                i = n % 2
                for tb in range(nblk):
                    blk = slice(tb * 512, tb * 512 + 512)
                    for ds in range(4):
                        db = dq * 4 + ds
                        bank = 6 + cnt % 2
                        cnt += 1
                        for c in range(8):
                            P.op("pe", lambda e, c=c, i=i, ds=ds, bank=bank, blk=blk: e.matmul(
                                PS[bank][:], lhsT=wd[i][:, c, ds * 128:(ds + 1) * 128], rhs=hid[:, c, blk], start=(c == 0), stop=(c == 7)),
                                reads=[s_wd[i], s_hid[tb]], writes=[sPS[bank]], sig=(c == 7))
                        if e_ == 0:
                            P.op("dve", lambda e, db=db, bank=bank, blk=blk: e.scalar_tensor_tensor(
                                out=X[:, db, blk], in0=X[:, db, blk], scalar=ALPHA, in1=PS[bank][:], op0=ALU.mult, op1=ALU.add),
                                reads=[sPS[bank]], writes=[sX[db][tb]])
                        else:
                            P.op("dve", lambda e, db=db, bank=bank, blk=blk: e.tensor_tensor(
                                out=X[:, db, blk], in0=X[:, db, blk], in1=PS[bank][:], op=ALU.add),
                                reads=[sPS[bank]], writes=[sX[db][tb]])
                if n + 2 < NE * 4:
                    load_d(n + 2)
        M.release(m0)

    def layer(l, Tq, CB, sCB, tag):
        stage(tag + "_in", Tq)
        mixer(l, Tq, CB, sCB, tag)
        stage(tag + "_mix", Tq)
        g = moe(l, Tq, tag)
        next(g)
        ln_fm(X, NCH, Tq, 2 + 4 * l, 3 + 4 * l, X, HB, sX, sHB, 6, 7, ln_tmps())
        stage(tag + "_ln1", Tq)
        router(Tq)
        next(g, None)
        stage(tag + "_moe", Tq)
        ln_fm(X, NCH, Tq, 4 + 4 * l, 5 + 4 * l, X, HB, sX, sHB, 6, 7, ln_tmps())
        P.barrier()
        stage(tag + "_ln2", Tq)

    CB_OFF[0] = M.mark()
    CB = M.alloc([128, NCH, 512], BF16)
    sCB = Slot("CB")
    s_cbsc = Slot("cbsc")
    ARENA2 = M.mark()
    d_cb = P.dsem("cbsc")

    def _main_seq():
        ln_in(0, 4, None, CB, 0, None, [sCB] * 2)
        P.barrier(); M.release(ARENA2)
        ln_in(512, 4, X, HB, 0, sX, sHB)
        P.barrier(); M.release(ARENA2)
        layer(0, 512, CB, sCB, "H")
        P.dma("sp", cbsc.rearrange("p (c t) -> p c t", c=NCH), HB[:, :, 0:512], d_cb, reads=[sHB[0]], writes=[s_cbsc])
        ln_in(512, 4, None, CB, 0, None, [sCB] * 2)
        P.barrier(); M.release(ARENA2)
        ln_in(1024, 8, X, HB, 0, sX, sHB)
        P.barrier(); M.release(ARENA2)
        layer(0, 1024, CB, sCB, "O")
        P.dma("sp", CB[:], cbsc.rearrange("p (c t) -> p c t", c=NCH), d_cb, reads=[s_cbsc], writes=[sCB])
        layer(1, 1024, CB, sCB, "L")

        ot = [M.alloc([128, D], F32), M.alloc([128, D], F32)]
        s_ot = [Slot("ot0"), Slot("ot1")]
        d_out = [P.dsem("out0"), P.dsem("out1")]
        for i in range(8):
            b = i % 2
            for c4 in range(4):
                bank = c4 % 4
                for cc in range(4):
                    c = c4 * 4 + cc
                    P.op("pe", lambda e, c=c, cc=cc, bank=bank, i=i: e.transpose(
                        PS[bank][:, cc * 128:(cc + 1) * 128], X[:, c, i * 128:(i + 1) * 128], ident[:]),
                        reads=[sX[c][i // 4], sConst], writes=[sPS[bank]])
                if c4 % 2:
                    P.op("act", lambda e, b=b, c4=c4, bank=bank: e.copy(out=ot[b][:, c4 * 512:(c4 + 1) * 512], in_=PS[bank][:]),
                         reads=[sPS[bank]], writes=[s_ot[b]])
                else:
                    P.op("dve", lambda e, b=b, c4=c4, bank=bank: e.tensor_copy(out=ot[b][:, c4 * 512:(c4 + 1) * 512], in_=PS[bank][:]),
                         reads=[sPS[bank]], writes=[s_ot[b]])
            P.dma("sp", out[i * 128:(i + 1) * 128, :], ot[b][:], d_out[b], reads=[s_ot[b]], writes=[Slot("o")])

    try:
        _main_seq()
    except _Stop:
        pass
    P.barrier()
    P.emit(nc, st)
    st.close()
    return nc, M.peak


_CACHE = {}


def _rel_index():
    k = np.arange(128)[:, None, None]
    j = np.arange(5)[None, :, None]
    q = np.arange(128)[None, None, :]
    rel = (4 - j) * 128 + q - k
    return (np.clip(rel, -128, 128) + 128).reshape(128, 640)


def kernel(x, ln_in_g, ln_in_b, w_in, rel_bias, sgu_ln_g, sgu_ln_b, sgu_w, sgu_b, w_out, ln1_g, ln1_b,
           router_w, router_b, w_gate, w_up, w_down, ln2_g, ln2_b):
    f = lambda a: np.ascontiguousarray(np.asarray(a, dtype=np.float32))
    x = f(x)
    if "nc" not in _CACHE:
        _CACHE["nc"], _CACHE["peak"] = build_program()
    nc = _CACHE["nc"]
    vecs = [ln_in_g, ln_in_b, ln1_g[0], ln1_b[0], ln2_g[0], ln2_b[0], ln1_g[1], ln1_b[1], ln2_g[1], ln2_b[1]]
    cols = np.concatenate([f(v).reshape(NCH, 128).T for v in vecs], axis=1)
    relT = f(np.asarray(rel_bias)[:, :, _rel_index()])
    shared = {"cols": f(cols), "w_in": f(w_in), "relT": relT, "sgu_ln_g": f(sgu_ln_g), "sgu_ln_b": f(sgu_ln_b),
              "sgu_w": f(sgu_w), "sgu_b": f(sgu_b), "w_out": f(w_out), "router_w": f(router_w), "router_b": f(router_b),
              "w_gate": f(w_gate), "w_up": f(w_up), "w_down": f(w_down)}
    in_maps = []
    for core in range(N_CORES):
        b, half = divmod(core, 2)
        if half == 0:
            xin = np.concatenate([np.zeros((1024, D), np.float32), x[b, 0:1024]], axis=0)
        else:
            xin = x[b]
        hvv = np.full((128, 1), float(half), np.float32)
        in_maps.append({"xin": np.ascontiguousarray(xin), "hv": hvv, **shared})
    res = run_bass_kernel_spmd(nc, in_maps, core_ids=list(range(N_CORES)))
    outp = np.empty((4, 2048, D), np.float32)
    for core in range(N_CORES):
        b, half = divmod(core, 2)
        outp[b, half * 1024:(half + 1) * 1024] = res.results[core]["out"]
    return outp
```
